# Optimizing a Trainium2 kernel written in Bass

```python
import math
import jax
import jax.numpy as jnp
from jax import lax
import numpy as np

D_MODEL = 1024
BATCH = 8
SEQ = 8192
DEPTH = 2

CTX_LEN = 256
GRID_W = 64
SSD_HEAD_DIM = 64
SSD_HEADS = D_MODEL // SSD_HEAD_DIM
D_SSD = SSD_HEADS * SSD_HEAD_DIM
SSD_GROUPS = 2
SSD_HPG = SSD_HEADS // SSD_GROUPS
SSD_STATE = 128
SSD_CONV = 5
SSD_CHUNK = 128
XBC_DIM = D_SSD + 2 * SSD_GROUPS * SSD_STATE
NA_HEAD_DIM = 64
NA_HEADS = D_MODEL // NA_HEAD_DIM
D_NA = NA_HEADS * NA_HEAD_DIM
NA_ROWS_MAX = 8
NA_COLS = 16
ROPE_BASE = 10000.0
D_FF = 256 * ((8 * D_MODEL // 3 + 255) // 256)
N_EXPERTS = 8
TOP_K = 2
D_FF_EXPERT = 7 * D_MODEL // 2
MOE_BLOCK = 256
EPS = 1e-6
NEG_INF = -1e30
IN_SPLITS = (D_MODEL, 2 * D_MODEL, 2 * D_MODEL + D_SSD, 2 * D_MODEL + D_SSD + XBC_DIM,
             2 * D_MODEL + D_SSD + XBC_DIM + 2 * SSD_HEADS,
             2 * D_MODEL + D_SSD + XBC_DIM + 2 * SSD_HEADS + D_NA,
             2 * D_MODEL + D_SSD + XBC_DIM + 2 * SSD_HEADS + 2 * D_NA)
IN_DIM = IN_SPLITS[-1] + D_NA
F32 = jnp.float32

kernel_name = 'hybrid_ssd_natten_moe_dit'


def rms_norm(x, w):
    xf = x.astype(F32)
    y = xf * lax.rsqrt(jnp.mean(xf * xf, axis=-1, keepdims=True) + EPS)
    return (y * w.astype(F32)).astype(x.dtype)


def flip(a):
    return jnp.flip(a, axis=1)


def depthwise_conv(x, w, b):
    y = lax.conv_general_dilated(x, w[:, None, :].astype(x.dtype), window_strides=(1,),
                                 padding=[(SSD_CONV // 2, SSD_CONV // 2)],
                                 dimension_numbers=('NWC', 'WIO', 'NWC'),
                                 feature_group_count=x.shape[-1])
    return y + b.astype(x.dtype)


def ssd_inputs(xbc_raw, dt_raw, conv_w, conv_b, dt_bias):
    bsz, seqlen, _ = xbc_raw.shape
    xbc = jax.nn.silu(depthwise_conv(xbc_raw, conv_w, conv_b))
    xs, bs, cs = jnp.split(xbc, (D_SSD, D_SSD + SSD_GROUPS * SSD_STATE), axis=-1)
    xs = xs.reshape(bsz, seqlen, SSD_GROUPS, SSD_HPG, SSD_HEAD_DIM)
    bs = bs.reshape(bsz, seqlen, SSD_GROUPS, SSD_STATE)
    cs = cs.reshape(bsz, seqlen, SSD_GROUPS, SSD_STATE)
    dt = jax.nn.softplus(dt_raw.astype(F32) + dt_bias.reshape(-1).astype(F32))
    dt = dt.reshape(bsz, seqlen, 2, SSD_GROUPS, SSD_HPG)
    return xs, bs, cs, dt[:, :, 0], dt[:, :, 1]


def ssd_chunked(x, dt, a, b_in, c_in, h0, return_y=True):
    bsz, seqlen, g, r, p = x.shape
    n = b_in.shape[-1]
    nc = seqlen // SSD_CHUNK
    dtf = dt.astype(F32)
    xdt = (x.astype(F32) * dtf[..., None]).reshape(bsz, nc, SSD_CHUNK, g, r, p)
    log_a = (dtf * a.astype(F32)).reshape(bsz, nc, SSD_CHUNK, g, r)
    bc = b_in.astype(F32).reshape(bsz, nc, SSD_CHUNK, g, n)
    cc = c_in.astype(F32).reshape(bsz, nc, SSD_CHUNK, g, n)
    a_cs = jnp.cumsum(log_a, axis=2)
    a_last = a_cs[:, :, -1]
    states = jnp.einsum('bcsgn,bcsgr,bcsgrp->bcgrpn', bc, jnp.exp(a_last[:, :, None] - a_cs), xdt)

    def step(h, inp):
        s_c, d_c = inp
        return h * d_c[..., None, None] + s_c, h

    h_fin, h_in = lax.scan(step, h0.astype(F32),
                           (jnp.moveaxis(states, 1, 0), jnp.moveaxis(jnp.exp(a_last), 1, 0)))
    if not return_y:
        return None, h_fin
    h_in = jnp.moveaxis(h_in, 0, 1)
    tri = jnp.tril(jnp.ones((SSD_CHUNK, SSD_CHUNK), bool))
    seg = a_cs[:, :, :, None] - a_cs[:, :, None, :]
    decay = jnp.exp(jnp.where(tri[:, :, None, None], seg, -jnp.inf))
    cb = jnp.einsum('bcqgn,bcsgn->bcqsg', cc, bc)
    y_diag = jnp.einsum('bcqsgr,bcsgrp->bcqgrp', cb[..., None] * decay, xdt)
    y_off = jnp.einsum('bcqgn,bcgrpn->bcqgrp', cc, h_in) * jnp.exp(a_cs)[..., None]
    y = (y_diag + y_off).reshape(bsz, seqlen, g, r, p)
    return y.astype(x.dtype), h_fin


def gated_group_rmsnorm(y, z, w):
    shp = y.shape
    u = y.astype(F32) * jax.nn.silu(z.astype(F32))
    u = u.reshape(*shp[:-1], SSD_GROUPS, shp[-1] // SSD_GROUPS)
    u = u * lax.rsqrt(jnp.mean(u * u, axis=-1, keepdims=True) + EPS)
    return (u.reshape(shp) * w.astype(F32)).astype(y.dtype)


def axial_rope(x, pos_r, pos_c):
    hd = x.shape[-1]
    quarter = hd // 4
    inv = ROPE_BASE ** (-jnp.arange(quarter, dtype=F32) / quarter)

    def rot(xh, pos):
        ang = pos[:, None] * inv[None, :]
        cos = jnp.cos(ang)[None, :, None, :].astype(x.dtype)
        sin = jnp.sin(ang)[None, :, None, :].astype(x.dtype)
        x1, x2 = jnp.split(xh, 2, axis=-1)
        return jnp.concatenate([x1 * cos - x2 * sin, x1 * sin + x2 * cos], axis=-1)

    return jnp.concatenate([rot(x[..., :hd // 2], pos_r), rot(x[..., hd // 2:], pos_c)], axis=-1)


def na_latent(q, k, v, kc, vc, rpb):
    bsz, seqlen, h, hd = q.shape
    rows = seqlen // GRID_W
    kr = min(NA_ROWS_MAX, rows)
    scale = hd ** -0.5
    qg = q.reshape(bsz, rows, GRID_W, h, hd)
    kg = k.reshape(bsz, rows, GRID_W, h, hd)
    vg = v.reshape(bsz, rows, GRID_W, h, hd)
    col = jnp.arange(GRID_W)
    col_start = jnp.clip(col - NA_COLS // 2, 0, GRID_W - NA_COLS)
    col_ok = (col[None, :] >= col_start[:, None]) & (col[None, :] < col_start[:, None] + NA_COLS)
    col_idx = jnp.clip(col[None, :] - col[:, None], -(NA_COLS - 1), NA_COLS - 1) + NA_COLS - 1
    rpb_c = rpb.astype(F32)[:, :, col_idx]

    def one_row(args):
        r, q_r = args
        r0 = jnp.clip(r - kr // 2, 0, rows - kr)
        k_band = lax.dynamic_slice_in_dim(kg, r0, kr, axis=1)
        v_band = lax.dynamic_slice_in_dim(vg, r0, kr, axis=1)
        dr_idx = r0 + jnp.arange(kr) - r + NA_ROWS_MAX - 1
        bias = jnp.take(rpb_c, dr_idx, axis=1)
        bias = jnp.where(col_ok[None, None], bias, NEG_INF).transpose(0, 2, 1, 3)
        s_win = jnp.einsum('bqhd,biwhd->bhqiw', q_r, k_band).astype(F32) * scale + bias[None]
        s_ctx = jnp.einsum('bqhd,bchd->bhqc', q_r, kc).astype(F32) * scale
        s = jnp.concatenate([s_win.reshape(bsz, h, GRID_W, kr * GRID_W), s_ctx], axis=-1)
        pr = jax.nn.softmax(s, axis=-1).astype(v.dtype)
        p_win = pr[..., :kr * GRID_W].reshape(bsz, h, GRID_W, kr, GRID_W)
        p_ctx = pr[..., kr * GRID_W:]
        return (jnp.einsum('bhqiw,biwhd->bqhd', p_win, v_band)
                + jnp.einsum('bhqc,bchd->bqhd', p_ctx, vc))

    out = lax.map(one_row, (jnp.arange(rows), jnp.moveaxis(qg, 1, 0)))
    return jnp.moveaxis(out, 0, 1).reshape(bsz, seqlen, h * hd)


def ctx_attention(q, k, v):
    bsz, clen, h, hd = q.shape
    s = jnp.einsum('bqhd,bkhd->bhqk', q, k).astype(F32) * hd ** -0.5
    pr = jax.nn.softmax(s, axis=-1).astype(v.dtype)
    return jnp.einsum('bhqk,bkhd->bqhd', pr, v).reshape(bsz, clen, h * hd)


def merge_branches(g_ssd, g_na, y_ssd, y_na, w_br_ssd, w_br_na, w_out):
    mixed = jax.nn.sigmoid(g_ssd) * (y_ssd @ w_br_ssd) + jax.nn.sigmoid(g_na) * (y_na @ w_br_na)
    return mixed @ w_out


def token_mixer(a_lat, a_ctx, pos_r, pos_c, w_in, conv_w, conv_b, dt_bias, a_log, d_skip,
                ssd_norm_w, q_norm_w, k_norm_w, rpb, w_br_ssd, w_br_na, w_out, need_ctx):
    bsz, seqlen, _ = a_lat.shape
    clen = a_ctx.shape[1]
    g_ssd_l, g_na_l, z_l, xbc_l, dt_l, q_l, k_l, v_l = jnp.split(a_lat @ w_in, IN_SPLITS, axis=-1)
    g_ssd_c, g_na_c, z_c, xbc_c, dt_c, q_c, k_c, v_c = jnp.split(a_ctx @ w_in, IN_SPLITS, axis=-1)

    a_dir = -jnp.exp(a_log.astype(F32)).reshape(2, SSD_GROUPS, SSD_HPG)
    d_sum = (d_skip[0] + d_skip[1]).reshape(SSD_GROUPS, SSD_HPG)[..., None]
    xs_l, b_l, c_l, dtf_l, dtb_l = ssd_inputs(xbc_l, dt_l, conv_w, conv_b, dt_bias)
    xs_c, b_c, c_c, dtf_c, dtb_c = ssd_inputs(xbc_c, dt_c, conv_w, conv_b, dt_bias)
    h0 = jnp.zeros((bsz, SSD_GROUPS, SSD_HPG, SSD_HEAD_DIM, SSD_STATE), F32)
    yf_c, sf_c = ssd_chunked(xs_c, dtf_c, a_dir[0], b_c, c_c, h0, need_ctx)
    yb_c, sb_c = ssd_chunked(flip(xs_c), flip(dtb_c), a_dir[1], flip(b_c), flip(c_c), h0, need_ctx)
    yf_l, _ = ssd_chunked(xs_l, dtf_l, a_dir[0], b_l, c_l, sf_c)
    yb_l, _ = ssd_chunked(flip(xs_l), flip(dtb_l), a_dir[1], flip(b_l), flip(c_l), sb_c)
    y_ssd_l = (yf_l + flip(yb_l) + xs_l * d_sum.astype(xs_l.dtype)).reshape(bsz, seqlen, D_SSD)
    y_ssd_l = gated_group_rmsnorm(y_ssd_l, z_l, ssd_norm_w)

    k_c = rms_norm(k_c.reshape(bsz, clen, NA_HEADS, NA_HEAD_DIM), k_norm_w)
    v_c = v_c.reshape(bsz, clen, NA_HEADS, NA_HEAD_DIM)
    q_l = axial_rope(rms_norm(q_l.reshape(bsz, seqlen, NA_HEADS, NA_HEAD_DIM), q_norm_w), pos_r, pos_c)
    k_l = axial_rope(rms_norm(k_l.reshape(bsz, seqlen, NA_HEADS, NA_HEAD_DIM), k_norm_w), pos_r, pos_c)
    v_l = v_l.reshape(bsz, seqlen, NA_HEADS, NA_HEAD_DIM)
    y_na_l = na_latent(q_l, k_l, v_l, k_c, v_c, rpb)

    out_l = merge_branches(g_ssd_l, g_na_l, y_ssd_l, y_na_l, w_br_ssd, w_br_na, w_out)
    if not need_ctx:
        return out_l, None
    y_ssd_c = (yf_c + flip(yb_c) + xs_c * d_sum.astype(xs_c.dtype)).reshape(bsz, clen, D_SSD)
    y_ssd_c = gated_group_rmsnorm(y_ssd_c, z_c, ssd_norm_w)
    q_c = rms_norm(q_c.reshape(bsz, clen, NA_HEADS, NA_HEAD_DIM), q_norm_w)
    y_na_c = ctx_attention(q_c, k_c, v_c)
    out_c = merge_branches(g_ssd_c, g_na_c, y_ssd_c, y_na_c, w_br_ssd, w_br_na, w_out)
    return out_l, out_c


def swiglu(h, w1, w3, w2):
    return (jax.nn.silu(h @ w1) * (h @ w3)) @ w2


def moe_swiglu(h, w_router, w1, w3, w2):
    n_tok, d = h.shape
    n_assign = n_tok * TOP_K
    logits = (h @ w_router).astype(F32)
    top_v, top_i = lax.top_k(logits, TOP_K)
    gates = jax.nn.softmax(top_v, axis=-1)
    flat_e = top_i.reshape(-1)
    order = jnp.argsort(flat_e)
    sorted_e = flat_e[order]
    tok = order // TOP_K
    counts = jnp.bincount(flat_e, length=N_EXPERTS)
    padded = (counts + MOE_BLOCK - 1) // MOE_BLOCK * MOE_BLOCK
    pad_end = jnp.cumsum(padded)
    pad_start = pad_end - padded
    start = jnp.cumsum(counts) - counts
    dest = pad_start[sorted_e] + jnp.arange(n_assign) - start[sorted_e]
    n_blocks = -(-n_assign // MOE_BLOCK) + N_EXPERTS
    rows = jnp.zeros((n_blocks * MOE_BLOCK, d), h.dtype).at[dest].set(h[tok])
    block_e = jnp.minimum(jnp.searchsorted(pad_end, jnp.arange(n_blocks) * MOE_BLOCK, side='right'),
                          N_EXPERTS - 1)

    def run_block(args):
        xb, e = args
        return swiglu(xb, w1[e], w3[e], w2[e])

    y_rows = lax.map(run_block, (rows.reshape(n_blocks, MOE_BLOCK, d), block_e)).reshape(-1, d)
    contrib = y_rows[dest] * gates.reshape(-1)[order][:, None].astype(h.dtype)
    return jnp.zeros_like(h).at[tok].add(contrib)


def setup_inputs(seed: int = 0) -> dict:
    key = jax.random.key(seed)
    ks = iter(jax.random.split(key, 40))

    def nrm(shape, scale):
        return jax.random.normal(next(ks), shape, F32) * scale

    L = DEPTH
    le = (DEPTH + 1) // 2
    lo = DEPTH // 2
    D = D_MODEL
    dt0 = jnp.exp(jax.random.uniform(next(ks), (L, 2, SSD_HEADS), F32, math.log(1e-3), math.log(1e-1)))
    dt_bias = dt0 + jnp.log(-jnp.expm1(-dt0))
    a_log = jnp.log(jax.random.uniform(next(ks), (L, 2, SSD_HEADS), F32, 1.0, 16.0))
    return {
        'x': nrm((BATCH, SEQ, D), 1.0),
        'c': nrm((BATCH, D), 1.0),
        'ctx': nrm((BATCH, CTX_LEN, D), 1.0),
        'c_ctx': nrm((D,), 1.0),
        'w_mod': nrm((L, D, 6 * D), 0.5 * D ** -0.5),
        'b_mod': nrm((L, 6 * D), 0.02),
        'norm1_w': 1.0 + nrm((L, D), 0.02),
        'norm2_w': 1.0 + nrm((L, D), 0.02),
        'w_in': nrm((L, D, IN_DIM), D ** -0.5),
        'conv_w': nrm((L, SSD_CONV, XBC_DIM), SSD_CONV ** -0.5),
        'conv_b': nrm((L, XBC_DIM), 0.02),
        'dt_bias': dt_bias,
        'a_log': a_log,
        'd_skip': 1.0 + nrm((L, 2, SSD_HEADS), 0.02),
        'ssd_norm_w': 1.0 + nrm((L, D_SSD), 0.02),
        'q_norm_w': 1.0 + nrm((L, NA_HEAD_DIM), 0.02),
        'k_norm_w': 1.0 + nrm((L, NA_HEAD_DIM), 0.02),
        'rpb': nrm((L, NA_HEADS, 2 * NA_ROWS_MAX - 1, 2 * NA_COLS - 1), 0.1),
        'w_br_ssd': nrm((L, D_SSD, D), D_SSD ** -0.5),
        'w_br_na': nrm((L, D_NA, D), D_NA ** -0.5),
        'w_out': nrm((L, D, D), D ** -0.5),
        'w_ff1': nrm((le, D, D_FF), D ** -0.5),
        'w_ff3': nrm((le, D, D_FF), D ** -0.5),
        'w_ff2': nrm((le, D_FF, D), D_FF ** -0.5),
        'w_router': nrm((lo, D, N_EXPERTS), D ** -0.5),
        'w_e1': nrm((lo, N_EXPERTS, D, D_FF_EXPERT), D ** -0.5),
        'w_e3': nrm((lo, N_EXPERTS, D, D_FF_EXPERT), D ** -0.5),
        'w_e2': nrm((lo, N_EXPERTS, D_FF_EXPERT, D), D_FF_EXPERT ** -0.5),
    }


def reference(x, c, ctx, c_ctx, w_mod, b_mod, norm1_w, norm2_w, w_in, conv_w, conv_b, dt_bias,
              a_log, d_skip, ssd_norm_w, q_norm_w, k_norm_w, rpb, w_br_ssd, w_br_na, w_out,
              w_ff1, w_ff3, w_ff2, w_router, w_e1, w_e3, w_e2):
    bsz, seqlen, d = x.shape
    t = jnp.arange(seqlen)
    pos_r = (t // GRID_W).astype(F32)
    pos_c = (t % GRID_W).astype(F32)
    cond_lat = jax.nn.silu(c)
    cond_ctx = jax.nn.silu(c_ctx)[None]
    h_lat, h_ctx = x, ctx
    n_ctx_tok = ctx.shape[0] * ctx.shape[1]
    for i in range(DEPTH):
        last = i == DEPTH - 1
        sh1, sc1, g1, sh2, sc2, g2 = jnp.split((cond_lat @ w_mod[i] + b_mod[i])[:, None, :], 6, axis=-1)
        csh1, csc1, cg1, csh2, csc2, cg2 = jnp.split((cond_ctx @ w_mod[i] + b_mod[i])[:, None, :], 6, axis=-1)
        a_lat = rms_norm(h_lat, norm1_w[i]) * (1.0 + sc1) + sh1
        a_ctx = rms_norm(h_ctx, norm1_w[i]) * (1.0 + csc1) + csh1
        m_lat, m_ctx = token_mixer(a_lat, a_ctx, pos_r, pos_c, w_in[i], conv_w[i], conv_b[i], dt_bias[i],
                                   a_log[i], d_skip[i], ssd_norm_w[i], q_norm_w[i], k_norm_w[i], rpb[i],
                                   w_br_ssd[i], w_br_na[i], w_out[i], not last)
        h_lat = h_lat + g1 * m_lat
        f_lat = rms_norm(h_lat, norm2_w[i]) * (1.0 + sc2) + sh2
        if last:
            tokens = f_lat.reshape(-1, d)
        else:
            h_ctx = h_ctx + cg1 * m_ctx
            f_ctx = rms_norm(h_ctx, norm2_w[i]) * (1.0 + csc2) + csh2
            tokens = jnp.concatenate([f_ctx.reshape(-1, d), f_lat.reshape(-1, d)], axis=0)
        fi = i // 2
        if i % 2 == 0:
            f_out = swiglu(tokens, w_ff1[fi], w_ff3[fi], w_ff2[fi])
        else:
            f_out = moe_swiglu(tokens, w_router[fi], w_e1[fi], w_e3[fi], w_e2[fi])
        if last:
            h_lat = h_lat + g2 * f_out.reshape(bsz, seqlen, d)
        else:
            h_ctx = h_ctx + cg2 * f_out[:n_ctx_tok].reshape(h_ctx.shape)
            h_lat = h_lat + g2 * f_out[n_ctx_tok:].reshape(bsz, seqlen, d)
    return h_lat
```

```python
import numpy as np
from contextlib import ExitStack
import concourse.bass as bass
import concourse.mybir as mybir
from concourse.bass_utils import run_bass_kernel_spmd

F32 = mybir.dt.float32
BF16 = mybir.dt.bfloat16
AF = mybir.ActivationFunctionType
ALU = mybir.AluOpType
AX = mybir.AxisListType

D = 1024
SEQ = 8192
CTX = 256
T = SEQ + CTX
NT = T // 128
IN_DIM = 7712
C_Z, C_XBC, C_DT, C_Q, C_K, C_V = 2048, 3072, 4608, 4640, 5664, 6688
D_FF = 2816
D_FFE = 3584
NEXP = 8
EPS = 1e-6
NEGM = -30000.0
ENG = ("pe", "act", "dve", "pool", "sp")


class Prog:
    def __init__(self, nc, ndma=28):
        self.nc = nc
        self.ndma = ndma
        self.nsp = 16
        self.rr = {"sp": 0, "pool": 0}
        self.sems = {}
        self.ops = {e: [] for e in ENG}
        self.cnt = {}
        self.known = {e: {} for e in ENG}
        self.last_w = {}
        self.readers = {}
        self.dma_rr = 0
        self.dma_last = [None] * ndma
        self.nops = 0

    def setup_sems(self, st):
        for e in ENG:
            self.sems[e] = st.enter_context(self.nc.semaphore("s_" + e))
        for i in range(self.ndma):
            self.sems[("d", i)] = st.enter_context(self.nc.semaphore("s_d%d" % i))

    def _wait(self, eng, tok):
        k, v = tok
        if self.known[eng].get(k, 0) >= v:
            return
        self.known[eng][k] = v
        self.ops[eng].append(("wait", k, v))

    def _deps(self, reads, writes):
        toks = []
        for b in reads:
            t = self.last_w.get(b)
            if t:
                toks.append(t)
        for b in writes:
            t = self.last_w.get(b)
            if t:
                toks.append(t)
            toks.extend(self.readers.get(b, {}).items())
        return toks

    def _record(self, tok, reads, writes):
        for b in writes:
            self.last_w[b] = tok
            self.readers[b] = {}
        for b in reads:
            r = self.readers.setdefault(b, {})
            if r.get(tok[0], 0) < tok[1]:
                r[tok[0]] = tok[1]

    def op(self, eng, fn, reads=(), writes=(), inc=True):
        cur = self.cnt.get(eng, 0)
        for t in self._deps(reads, writes):
            if t[0] == eng and (t[1] < cur - 1 or t[1] > cur):
                continue
            self._wait(eng, t)
        self.ops[eng].append(("op", fn, inc))
        self.nops += 1
        if inc:
            cur += 1
            self.cnt[eng] = cur
            tok = (eng, cur)
        else:
            tok = (eng, cur + 1)
        self._record(tok, reads, writes)
        return tok

    def _slot(self, eng):
        if eng == "sp":
            sl = self.rr["sp"] % self.nsp
        else:
            sl = self.nsp + self.rr["pool"] % (self.ndma - self.nsp)
        self.rr["sp" if eng == "sp" else "pool"] += 1
        return sl

    def dma(self, out, in_, reads=(), writes=(), eng="sp"):
        slot = self._slot(eng)
        if self.dma_last[slot]:
            self._wait(eng, self.dma_last[slot])
        for t in self._deps(reads, writes):
            self._wait(eng, t)
        k = ("d", slot)
        v = self.cnt.get(k, 0) + 16
        self.cnt[k] = v
        self.ops[eng].append(("dma", out, in_, k))
        self.nops += 1
        tok = (k, v)
        self.dma_last[slot] = tok
        self._record(tok, reads, writes)
        return tok

    def idma(self, out, out_off, in_, in_off, reads=(), writes=()):
        eng = "pool"
        slot = self._slot(eng)
        if self.dma_last[slot]:
            self._wait(eng, self.dma_last[slot])
        for t in self._deps(reads, writes):
            self._wait(eng, t)
        k = ("d", slot)
        v = self.cnt.get(k, 0) + 16
        self.cnt[k] = v
        self.ops[eng].append(("idma", out, out_off, in_, in_off, k))
        self.nops += 1
        tok = (k, v)
        self.dma_last[slot] = tok
        self._record(tok, reads, writes)
        return tok

    def barrier(self):
        for e in ENG:
            for k, v in self.cnt.items():
                if k != e:
                    self._wait(e, (k, v))

    def check(self):
        if not hasattr(self, "simval"):
            self.simval = {}
        val = self.simval
        pos = {e: 0 for e in ENG}
        prog = True
        while prog:
            prog = False
            for e in ENG:
                lst = self.ops[e]
                while pos[e] < len(lst):
                    it = lst[pos[e]]
                    if it[0] == "wait":
                        if val.get(it[1], 0) < it[2]:
                            break
                    elif it[0] == "op":
                        if it[2]:
                            val[e] = val.get(e, 0) + 1
                    elif it[0] == "dma":
                        val[it[3]] = val.get(it[3], 0) + 16
                    else:
                        val[it[5]] = val.get(it[5], 0) + 16
                    pos[e] += 1
                    prog = True
        for e in ENG:
            if pos[e] < len(self.ops[e]):
                raise RuntimeError("deadlock: engine %s stuck at %d/%d on %r (vals %r)" % (
                    e, pos[e], len(self.ops[e]), self.ops[e][pos[e]][:3], val))

    def flush(self, final=False):
        self.barrier()
        self.check()
        nc = self.nc
        sems = self.sems

        def run(e, name):
            for it in self.ops[name]:
                if it[0] == "wait":
                    e.wait_ge(sems[it[1]], it[2])
                elif it[0] == "op":
                    ins = it[1](e)
                    if it[2]:
                        ins.then_inc(sems[name], 1)
                elif it[0] == "dma":
                    e.dma_start(out=it[1], in_=it[2]).then_inc(sems[it[3]], 16)
                else:
                    oo = bass.IndirectOffsetOnAxis(ap=it[2], axis=0) if it[2] is not None else None
                    io = bass.IndirectOffsetOnAxis(ap=it[4], axis=0) if it[4] is not None else None
                    e.indirect_dma_start(out=it[1], out_offset=oo, in_=it[3], in_offset=io).then_inc(sems[it[5]], 16)
            self.ops[name] = []

        with nc.Block() as block:
            @block.tensor
            def _(e):
                run(e, "pe")

            @block.scalar
            def _(e):
                run(e, "act")

            @block.vector
            def _(e):
                run(e, "dve")

            @block.gpsimd
            def _(e):
                run(e, "pool")

            @block.sync
            def _(e):
                run(e, "sp")

    def mm(self, out, lhsT, rhs, start=True, stop=True, inc=True, reads=(), writes=()):
        return self.op("pe", lambda e: e.matmul(out, lhsT, rhs, start=start, stop=stop),
                       reads, writes, inc)

    def tr(self, out, in_, ident, inc=True, reads=(), writes=()):
        return self.op("pe", lambda e: e.transpose(out, in_, ident), reads, writes, inc)

    def act(self, out, in_, func, reads=(), writes=(), eng="act", **kw):
        return self.op("act", lambda e: e.activation(out, in_, func, **kw), reads, writes)

    def tt(self, eng, out, in0, in1, op, reads=(), writes=()):
        return self.op(eng, lambda e: e.tensor_tensor(out, in0, in1, op), reads, writes)

    def ts(self, eng, out, in0, s1, s2, op0, op1=None, reads=(), writes=()):
        if op1 is None:
            return self.op(eng, lambda e: e.tensor_scalar(out, in0, s1, None, op0), reads, writes)
        return self.op(eng, lambda e: e.tensor_scalar(out, in0, s1, s2, op0, op1), reads, writes)

    def stt(self, eng, out, in0, scalar, in1, op0, op1, reads=(), writes=()):
        return self.op(eng, lambda e: e.scalar_tensor_tensor(out, in0, scalar, in1, op0, op1),
                       reads, writes)

    def copy(self, eng, out, in_, reads=(), writes=()):
        if eng == "act":
            return self.op("act", lambda e: e.copy(out, in_), reads, writes)
        return self.op(eng, lambda e: e.tensor_copy(out, in_), reads, writes)

    def memset(self, eng, ap, val, writes=()):
        return self.op(eng, lambda e: e.memset(ap, val), (), writes)


class Ctx:
    pass


def host_consts(rpb):
    c = {}
    s = np.arange(128)[:, None]
    q = np.arange(128)[None, :]
    c["ident"] = np.eye(128, dtype=np.float32)
    c["trif"] = (s <= q).astype(np.float32)
    c["trib"] = (s >= q).astype(np.float32)
    c["ones"] = np.ones((128, 128), np.float32)
    mf = np.where(q < s, NEGM, 0.0).astype(np.float32)
    mb = np.where(q > s, NEGM, 0.0).astype(np.float32)
    c["maskf"] = np.tile(mf, (1, 4))
    c["maskb"] = np.tile(mb, (1, 4))
    sel = np.zeros((16, 16 * 128), np.float32)
    for h in range(16):
        sel[h, h * 128:(h + 1) * 128] = 1.0
    c["sel"] = sel
    c["stri"] = (s < q).astype(np.float32)
    p = np.arange(128)[:, None]
    c["thr"] = np.tile((np.arange(16) * 1024.0)[None, :], (128, 8)).astype(np.float32)
    c["thr2"] = np.repeat((np.arange(24) * 1024.0), 8)[None, :].repeat(128, 0).astype(np.float32)
    kq = np.zeros((128, 4, 8), np.float32)
    for qd in range(4):
        for k in range(8):
            kq[:, qd, k] = (k * 128 + np.arange(128)) * 4 + qd
    c["kpq"] = kq.reshape(128, 32)
    k2 = np.zeros((128, 4, 7), np.float32)
    for qd in range(4):
        for k in range(7):
            k2[:, qd, k] = qd * 896 + k * 128 + np.arange(128)
    c["kp2"] = k2.reshape(128, 28)
    t = np.arange(SEQ)
    pr = (t // 64).astype(np.float32)
    pc = (t % 64).astype(np.float32)
    inv = (np.float32(10000.0) ** (-np.arange(16, dtype=np.float32) / np.float32(16))).astype(np.float32)
    ar = (pr[:, None] * inv[None, :]).astype(np.float32)
    ac = (pc[:, None] * inv[None, :]).astype(np.float32)
    c["ropec"] = np.concatenate([np.cos(ar), np.cos(ar), np.cos(ac), np.cos(ac)], 1).astype(np.float32)
    c["ropes"] = np.concatenate([-np.sin(ar), np.sin(ar), -np.sin(ac), np.sin(ac)], 1).astype(np.float32)
    L = rpb.shape[0]
    tab = np.empty((L, 5, 16, 128, 5, 128), np.float32)
    for ci, jq in enumerate([2, 0, 1, 62, 63]):
        cb = min(max(jq - 2, 0), 59)
        kc = np.arange(5)[:, None, None]
        kk = np.arange(128)[None, :, None]
        qq = np.arange(128)[None, None, :]
        rk = 2 * (cb + kc) + kk // 64
        ck = kk % 64 + 0 * kc
        r = 2 * jq + qq // 64
        cq = qq % 64
        r0 = np.clip(r - 4, 0, 120)
        cs = np.clip(cq - 8, 0, 48)
        valid = (rk >= r0) & (rk < r0 + 8) & (ck >= cs) & (ck < cs + 16)
        dr = np.clip(rk - r + 7, 0, 14)
        dc = np.clip(ck - cq, -15, 15) + 15
        dr, dc, valid = np.broadcast_arrays(dr, dc, valid)
        vals = rpb[:, :, dr, dc]
        vals = np.where(valid[None, None], vals, np.float32(NEGM))
        tab[:, ci] = vals.transpose(0, 1, 3, 2, 4)
    c["nab"] = tab.reshape(L, 5, 16, 128, 640)
    return c


CONST_SHAPES = {
    "ident": [128, 128], "trif": [128, 128], "trib": [128, 128], "ones": [128, 128],
    "maskf": [128, 512], "maskb": [128, 512], "sel": [16, 2048],
    "stri": [128, 128], "thr": [128, 128], "thr2": [128, 192], "kpq": [128, 32], "kp2": [128, 28],
    "ropec": [SEQ, 64], "ropes": [SEQ, 64], "nab": [2, 5, 16, 128, 640],
}

WEIGHT_SHAPES = {
    "w_mod": [2, 1024, 6144], "b_mod": [2, 6144], "norm1_w": [2, 1024], "norm2_w": [2, 1024],
    "w_in": [2, 1024, 7712], "conv_w": [2, 5, 1536], "conv_b": [2, 1536], "dt_bias": [2, 2, 16],
    "a_log": [2, 2, 16], "d_skip": [2, 2, 16], "ssd_norm_w": [2, 1024], "q_norm_w": [2, 64],
    "k_norm_w": [2, 64], "w_br_ssd": [2, 1024, 1024], "w_br_na": [2, 1024, 1024],
    "w_out": [2, 1024, 1024], "w_ff1": [1, 1024, 2816], "w_ff3": [1, 1024, 2816],
    "w_ff2": [1, 2816, 1024], "w_router": [1, 1024, 8], "w_e1": [1, 8, 1024, 3584],
    "w_e3": [1, 8, 1024, 3584], "w_e2": [1, 8, 3584, 1024],
}


def rowb(ap1d_row, n, parts=128):
    return ap1d_row.broadcast_to([parts, n])


def build(stages, dbg=()):
    nc = bass.Bass("TRN2", target_bir_lowering=False)
    g = Ctx()
    g.nc = nc
    g.dbg = dbg
    I = {}
    I["x"] = nc.dram_tensor("x", [SEQ, D], F32, kind="ExternalInput").ap()
    I["ctx"] = nc.dram_tensor("ctx", [CTX, D], F32, kind="ExternalInput").ap()
    I["ct"] = nc.dram_tensor("ct", [128, 16], F32, kind="ExternalInput").ap()
    for k, s in WEIGHT_SHAPES.items():
        I[k] = nc.dram_tensor(k, s, F32, kind="ExternalInput").ap()
    for k, s in CONST_SHAPES.items():
        I[k] = nc.dram_tensor(k, s, F32, kind="ExternalInput").ap()
    g.I = I
    g.out = nc.dram_tensor("out", [SEQ, D], F32, kind="ExternalOutput").ap()

    def scratch(name, shape, dt):
        kind = "ExternalOutput" if name in dbg else "Internal"
        return nc.dram_tensor(name, shape, dt, kind=kind).ap()

    g.MOD = scratch("MOD", [2, 2, 6144], F32)
    g.P = scratch("P", [T, IN_DIM], F32)
    g.YF = scratch("YF", [T, D], F32)
    g.XS = scratch("XS", [T, 1536], F32)
    g.YS = scratch("YS", [T, D], F32)
    g.YN = scratch("YN", [T, D], F32)
    g.H = scratch("H", [T, D], F32)
    g.QT = scratch("QT", [NT, 128, 8, 128], BF16)
    g.KT = scratch("KT", [NT, 128, 8, 128], BF16)
    g.FT = scratch("FT", [64, 128, 8, 128], BF16)
    g.GT = scratch("GT", [64, 128, 8], F32)
    I32 = mybir.dt.int32
    g.F = scratch("F", [SEQ, D], BF16)
    g.ROWS = scratch("ROWS", [24576, D], BF16)
    g.YROWS = scratch("YROWS", [24576, D], F32)
    g.GG = scratch("GG", [128, 128], F32)
    g.DEST = scratch("DEST", [128, 128], I32)
    g.BED = scratch("BED", [128, 24], F32)

    with ExitStack() as st:
        pr = Prog(nc)
        pr.setup_sems(st)
        g.pr = pr
        for (l, ph) in stages:
            PHASES[ph](g, l)
            pr.flush()
    return nc


_UID = [0]


def sbt(g, st, name, shape, dt):
    _UID[0] += 1
    return st.enter_context(g.nc.sbuf_tensor("sb%d_%s" % (_UID[0], name), shape, dt))


def pst(g, st, name, shape, dt):
    _UID[0] += 1
    return st.enter_context(g.nc.psum_tensor("ps%d_%s" % (_UID[0], name), shape, dt))


def load_consts(g, st, names):
    pr, I = g.pr, g.I
    out = {}
    for n in names:
        base = n[:-2] if n.endswith("_b") else n
        shp = CONST_SHAPES[base]
        if n.endswith("_b"):
            t = sbt(g, st, "c_" + n, shp, BF16)
            pr.dma(t[:], I[base][:, :], writes=[n], eng="pool")
        else:
            t = sbt(g, st, "c_" + n, shp, F32)
            pr.dma(t[:], I[n][:, :], writes=[n])
        out[n] = t
    return out


def src_rows(g, l, t):
    if l == 0:
        if t < 2:
            return g.I["ctx"][t * 128:(t + 1) * 128, :]
        return g.I["x"][(t - 2) * 128:(t - 1) * 128, :]
    return g.H[t * 128:(t + 1) * 128, :]


def phase_M(g, l):
    pr, I, nc = g.pr, g.I, g.nc
    with ExitStack() as st:
        ct = sbt(g, st, "ct", [128, 16], F32)
        cs = sbt(g, st, "cs", [128, 16], F32)
        cb = sbt(g, st, "cb", [128, 16, 128], F32)
        wst = [sbt(g, st, "wst%d" % i, [128, 8, 512], F32) for i in range(2)]
        bst = [sbt(g, st, "bst%d" % i, [128, 512], F32) for i in range(2)]
        res = [sbt(g, st, "res%d" % i, [128, 512], F32) for i in range(2)]
        pp = [pst(g, st, "pp%d" % i, [128, 512], F32) for i in range(4)]
        pr.dma(ct[:], I["ct"][:, :], writes=["ct"])
        pr.act(cs[:], ct[:], AF.Silu, reads=["ct"], writes=["cs"])
        pr.copy("dve", cb[:], cs[:].unsqueeze(2).broadcast_to([128, 16, 128]), reads=["cs"], writes=["cb"])
        i = 0
        for cg in range(12):
            c0 = cg * 512
            w = wst[cg % 2]
            pr.dma(w[:], I["w_mod"][l, :, c0:c0 + 512].rearrange("(k p) n -> p k n", p=128),
                   writes=["wst%d" % (cg % 2)])
            pr.dma(bst[cg % 2][:], rowb(I["b_mod"][l:l + 1, c0:c0 + 512], 512), writes=["bst%d" % (cg % 2)])
            for v in range(2):
                p = pp[i % 4]
                pk = "pp%d" % (i % 4)
                for k in range(8):
                    col = (8 + k) if v == 0 else k
                    pr.mm(p[:], cb[:, col, :], w[:, k, :], start=(k == 0), stop=(k == 7), inc=(k == 7),
                          reads=["cb", "wst%d" % (cg % 2)], writes=[pk])
                r = res[i % 2]
                rk = "res%d" % (i % 2)
                pr.tt("dve", r[:], p[:], bst[cg % 2][:], ALU.add, reads=[pk, "bst%d" % (cg % 2)], writes=[rk])
                pr.dma(g.MOD[l, v:v + 1, c0:c0 + 512], r[0:1, :], reads=[rk], eng="pool")
                i += 1


def load_mod(g, st, l, which, name):
    pr = g.pr
    out = []
    for v in range(2):
        t = sbt(g, st, "%s%d" % (name, v), [128, 1024], F32)
        pr.dma(t[:], rowb(g.MOD[l, v:v + 1, which * 1024:(which + 1) * 1024], 1024), writes=["%s%d" % (name, v)])
        out.append(t)
    return out


def make_scale(g, st, l, which, wname, name):
    pr, I = g.pr, g.I
    sc = load_mod(g, st, l, which, name)
    nw = sbt(g, st, name + "nw", [128, 1024], F32)
    pr.dma(nw[:], rowb(I[wname][l:l + 1, :], 1024), writes=[name + "nw"])
    for v in range(2):
        k = "%s%d" % (name, v)
        pr.stt("dve", sc[v][:], sc[v][:], 1.0, nw[:], ALU.add, ALU.mult, reads=[k, name + "nw"], writes=[k])
    return sc


def rms_rstd(g, sq_junk, x_ap, ssq, rs, n, keys_r, key_ssq, key_rs, junk_key):
    pr = g.pr
    pr.act(sq_junk, x_ap, AF.Square, reads=keys_r, writes=[junk_key, key_ssq], accum_out=ssq)
    pr.ts("dve", rs, ssq, 1.0 / n, EPS, ALU.mult, ALU.add, reads=[key_ssq], writes=[key_rs])
    pr.act(rs, rs, AF.Sqrt, reads=[key_rs], writes=[key_rs])
    pr.op("dve", lambda e: e.reciprocal(rs, rs), reads=[key_rs], writes=[key_rs])


def phase_A(g, l):
    pr, I, nc = g.pr, g.I, g.nc
    with ExitStack() as st:
        W = sbt(g, st, "winb", [128, 8, IN_DIM], BF16)
        for k in range(8):
            for pc in range(4):
                c0 = pc * 2048
                c1 = min(IN_DIM, c0 + 2048)
                pr.dma(W[:, k, c0:c1], I["w_in"][l, k * 128:(k + 1) * 128, c0:c1], writes=["W"], eng="pool")
        cst = load_consts(g, st, ["ident_b"])
        G1 = make_scale(g, st, l, 1, "norm1_w", "G1")
        SH1 = load_mod(g, st, l, 0, "SH1")
        hb = [sbt(g, st, "hb%d" % i, [128, 1024], F32) for i in range(2)]
        junk = sbt(g, st, "junk", [128, 1024], F32)
        tmp = sbt(g, st, "tmpa", [128, 1024], F32)
        abf = sbt(g, st, "abf", [128, 1024], BF16)
        aT = [sbt(g, st, "aT%d" % i, [128, 8, 128], BF16) for i in range(2)]
        ssq = sbt(g, st, "ssq", [128, 2], F32)
        rs = sbt(g, st, "rs", [128, 2], F32)
        ostg = [sbt(g, st, "ostg%d" % i, [128, 2048], F32) for i in range(2)]
        ptr = pst(g, st, "ptr", [128, 1024], BF16)
        pp = [pst(g, st, "ppa%d" % i, [128, 512], F32) for i in range(5)]
        abf2 = [abf, sbt(g, st, "abf1", [128, 1024], BF16)]
        pr.dma(hb[0][:], src_rows(g, l, 0), writes=["hb0"])
        pr.dma(hb[1][:], src_rows(g, l, 1), writes=["hb1"])
        oi = 0
        ei = 0

        def normA(t):
            v = 0 if t < 2 else 1
            h = hb[t % 2]
            hk = "hb%d" % (t % 2)
            ab, abk = abf2[t % 2], "abf%d" % (t % 2)
            rms_rstd(g, junk[:], h[:], ssq[:, 0:1], rs[:, 0:1], 1024, [hk], "ssq", "rs", "junk")
            pr.stt("dve", tmp[:], h[:], rs[:, 0:1], G1[v][:], ALU.mult, ALU.mult,
                   reads=[hk, "rs", "G1%d" % v], writes=["tmpa"])
            pr.tt("dve", ab[:], tmp[:], SH1[v][:], ALU.add, reads=["tmpa", "SH1%d" % v], writes=[abk])
            if t + 2 < NT:
                pr.dma(h[:], src_rows(g, l, t + 2), reads=[], writes=[hk])

        def transA(t):
            ab, abk = abf2[t % 2], "abf%d" % (t % 2)
            for k in range(8):
                pr.tr(ptr[:, k * 128:(k + 1) * 128], ab[:, k * 128:(k + 1) * 128], cst["ident_b"][:],
                      inc=(k == 7), reads=[abk, "ident_b"], writes=["ptr"])
            a = aT[t % 2]
            ak = "aT%d" % (t % 2)
            pr.copy("act", a[:], ptr[:].rearrange("p (k n) -> p k n", k=8), reads=["ptr"], writes=[ak])

        normA(0)
        transA(0)
        for t in range(NT):
            a = aT[t % 2]
            ak = "aT%d" % (t % 2)
            if t + 1 < NT:
                normA(t + 1)
            for cg in range(16):
                c0 = cg * 512
                n = min(512, IN_DIM - c0)
                p = pp[cg % 5]
                pk = "ppa%d" % (cg % 5)
                for k in range(8):
                    pr.mm(p[:, :n], a[:, k, :], W[:, k, c0:c0 + n], start=(k == 0), stop=(k == 7),
                          inc=(k == 7), reads=[ak, "W"], writes=[pk])
                o = ostg[oi % 2]
                ok = "ostg%d" % (oi % 2)
                oc = (cg % 4) * 512
                pr.copy("act" if ei % 2 == 0 else "dve", o[:, oc:oc + n], p[:, :n], reads=[pk], writes=[ok])
                ei += 1
                if cg % 4 == 3:
                    b0 = (cg // 4) * 2048
                    b1 = min(IN_DIM, b0 + 2048)
                    pr.dma(g.P[t * 128:(t + 1) * 128, b0:b1], o[:, :b1 - b0], reads=[ok], eng="pool")
                    oi += 1
                if cg == 11 and t + 1 < NT:
                    transA(t + 1)


def phase_SSD(g, l, d):
    pr, I, nc = g.pr, g.I, g.nc
    with ExitStack() as st:
        CK = ["ident_b", "trib" if d else "trif", "ones", "sel", "maskb_b" if d else "maskf_b"]
        cst = load_consts(g, st, CK)
        identb, tri, ones, sel, maskb = [cst[k] for k in CK]
        if d == 0:
            convw = sbt(g, st, "convw", [128, 5, 1536], F32)
            pr.dma(convw[:].rearrange("p k c -> p (k c)"),
                   rowb(I["conv_w"][l:l + 1].rearrange("a k c -> a (k c)"), 7680), writes=["convw"])
            convb = sbt(g, st, "convb", [128, 1536], F32)
            pr.dma(convb[:], rowb(I["conv_b"][l:l + 1, :], 1536), writes=["convb"])
            xb = [sbt(g, st, "xb%d" % i, [128, 1536], F32) for i in range(3)]
            ctmp = [sbt(g, st, "ctmp%d" % i, [128, 1536], F32) for i in range(2)]
            acc = sbt(g, st, "cacc", [128, 1536], F32)
        dtb = sbt(g, st, "dtb", [128, 32], F32)
        pr.dma(dtb[:], rowb(I["dt_bias"][l:l + 1].rearrange("a x h -> a (x h)"), 32), writes=["dtb"])
        aneg = sbt(g, st, "aneg", [128, 32], F32)
        pr.dma(aneg[:], rowb(I["a_log"][l:l + 1].rearrange("a x h -> a (x h)"), 32), writes=["aneg"])
        pr.act(aneg[:], aneg[:], AF.Exp, reads=["aneg"], writes=["aneg"])
        pr.ts("dve", aneg[:], aneg[:], -1.0, None, ALU.mult, reads=["aneg"], writes=["aneg"])
        if d:
            dsk = sbt(g, st, "dsk", [128, 32], F32)
            pr.dma(dsk[:], rowb(I["d_skip"][l:l + 1].rearrange("a x h -> a (x h)"), 32), writes=["dsk"])
            dsum = sbt(g, st, "dsum", [128, 16], F32)
            pr.tt("dve", dsum[:], dsk[:, 0:16], dsk[:, 16:32], ALU.add, reads=["dsk"], writes=["dsum"])
            ssdw = sbt(g, st, "ssdw", [128, 1024], F32)
            pr.dma(ssdw[:], rowb(I["ssd_norm_w"][l:l + 1, :], 1024), writes=["ssdw"])
            yfb = [sbt(g, st, "yfb%d" % i, [128, 1024], F32) for i in range(2)]
            zb = [sbt(g, st, "zb%d" % i, [128, 1024], F32) for i in range(2)]
            ub = sbt(g, st, "ub", [128, 1024], F32)
            ms = sbt(g, st, "ms", [128, 4], F32)
        SR = [sbt(g, st, "S%d" % i, [128, 1024], F32) for i in range(2)]
        SbfR = [sbt(g, st, "Sbf%d" % i, [128, 1024], BF16) for i in range(2)]
        pr.memset("dve", SR[0][:], 0.0, writes=["S0"])
        pr.memset("dve", SbfR[0][:], 0.0, writes=["Sbf0"])
        def ring(name, n, shape, dt_):
            return [sbt(g, st, "%s%d" % (name, i), shape, dt_) for i in range(n)]

        xbcR = ring("xbc", 4, [128, 1536], F32)
        bcbfR = ring("bcbf", 3, [128, 512], BF16)
        bcTR = ring("bcT", 3, [128, 4, 128], BF16)
        cbtR = ring("cbt", 2, [128, 2, 128], BF16)
        dtrR = ring("dtr", 2, [128, 32], F32)
        dtR = ring("dt", 3, [128, 32], F32)
        smR = ring("sm", 3, [128, 128], F32)
        acsTR = ring("acsT", 2, [16, 128], F32)
        nacsTR = ring("nacsT", 2, [16, 128], F32)
        BmR = ring("Bm", 2, [16, 2048], F32)
        xdtR = ring("xdt", 3, [128, 1024], BF16)
        xwR = ring("xw", 3, [128, 1024], BF16)
        mtR = ring("mt", 2, [128, 2048], BF16)
        ebf = ring("ebf", 2, [128, 512], BF16)
        ytmp = sbt(g, st, "ytmp", [128, 1024], F32)
        ysb = ring("ysb", 2, [128, 1024], F32)
        pmA = pst(g, st, "pmA", [128, 512], F32)
        ptb = pst(g, st, "ptb", [128, 1024], BF16)
        pseg = [pst(g, st, "pseg%d" % i, [128, 512], F32) for i in range(2)]
        pyd1 = pst(g, st, "pyd", [128, 512], F32)
        pyo1 = pst(g, st, "pyo", [128, 512], F32)
        psd = [pst(g, st, "psd%d" % i, [128, 512], F32) for i in range(2)]
        order = list(range(NT)) if d == 0 else [1, 0] + list(range(NT - 1, 1, -1))
        xi = [0]

        def R(ringl, name, c):
            i = c % len(ringl)
            return ringl[i], "%s%d" % (name, i)

        def smv(sm):
            return (sm[:, 0:16], sm[:, 16:32], sm[:, 32:48], sm[:, 64:80], sm[:, 80:96], sm[:, 96:112], sm[:, 112:128])

        def s0(c):
            t = order[c]
            t0 = t * 128
            lo, hi = (0, 256) if t < 2 else (256, T)
            xbc, xk_ = R(xbcR, "xbc", c)
            dtr, dtrk = R(dtrR, "dtr", c)
            dt, dtk = R(dtR, "dt", c)
            pr.dma(dtr[:], g.P[t0:t0 + 128, C_DT:C_DT + 32], writes=[dtrk])
            if d:
                pr.dma(xbc[:], g.XS[t0:t0 + 128, :], writes=[xk_])
            else:
                for k in range(5):
                    x = xb[xi[0] % 3]
                    xk = "xb%d" % (xi[0] % 3)
                    xi[0] += 1
                    r0 = t0 + k - 2
                    i_lo = max(0, lo - r0)
                    i_hi = min(128, hi - r0)
                    if i_lo > 0 or i_hi < 128:
                        pr.memset("dve", x[:], 0.0, writes=[xk])
                    pr.dma(x[i_lo:i_hi, :], g.P[r0 + i_lo:r0 + i_hi, C_XBC:C_XBC + 1536], writes=[xk],
                           eng=("pool" if k in (1, 3) else "sp"))
                    ct_ = ctmp[k % 2]
                    ck = "ctmp%d" % (k % 2)
                    pr.tt("pool" if k in (0, 2) else "dve", ct_[:], x[:], convw[:, k, :], ALU.mult,
                          reads=[xk, "convw"], writes=[ck])
                    pr.tt("dve", acc[:], ct_[:], convb[:] if k == 0 else acc[:], ALU.add,
                          reads=[ck, "convb", "cacc"], writes=["cacc"])
                pr.act(xbc[:], acc[:], AF.Silu, reads=["cacc"], writes=[xk_])
                pr.dma(g.XS[t0:t0 + 128, :], xbc[:], reads=[xk_], eng="pool")
            pr.tt("dve", dt[:], dtr[:], dtb[:], ALU.add, reads=[dtrk, "dtb"], writes=[dtk])
            pr.act(dt[:], dt[:], AF.Exp, reads=[dtk], writes=[dtk])
            pr.act(dt[:], dt[:], AF.Ln, reads=[dtk], writes=[dtk], bias=1.0)

        def s1a(c):
            xbc, xk_ = R(xbcR, "xbc", c)
            dt, dtk = R(dtR, "dt", c)
            sm, smk = R(smR, "sm", c)
            bcbf, bcbfk = R(bcbfR, "bcbf", c)
            bcT, bcTk = R(bcTR, "bcT", c)
            acsT, acsTk = R(acsTR, "acsT", c)
            nacsT, nacsTk = R(nacsTR, "nacsT", c)
            xdt, xdtk = R(xdtR, "xdt", c)
            la, acs, tot, eacs, etot, wdec, dtw = smv(sm)
            dtd = dt[:, 16 * d:16 * d + 16]
            pr.copy("dve", bcbf[:], xbc[:, 1024:1536], reads=[xk_], writes=[bcbfk])
            pr.tt("dve", la, dtd, aneg[:, 16 * d:16 * d + 16], ALU.mult, reads=[dtk, "aneg"], writes=[smk + "la"])
            for j in range(4):
                pr.tr(ptb[:, j * 128:(j + 1) * 128], bcbf[:, j * 128:(j + 1) * 128], identb[:], inc=(j == 3),
                      reads=[bcbfk, "ident_b"], writes=["ptb"])
            tk = CK[1]
            pr.mm(pmA[:, 256:272], tri[:], la, reads=[tk, smk + "la"], writes=["pmA"], inc=False)
            pr.mm(pmA[:, 272:288], ones[:], la, reads=["ones", smk + "la"], writes=["pmA"], inc=False)
            pr.mm(pmA[0:16, 320:448], la, tri[:], reads=[tk, smk + "la"], writes=["pmA"])
            xs3 = xbc[:, 0:1024].rearrange("p (h e) -> p h e", e=64)
            pr.tt("dve", xdt[:].rearrange("p (h e) -> p h e", e=64), xs3,
                  dtd.unsqueeze(2).broadcast_to([128, 16, 64]), ALU.mult, reads=[xk_, dtk], writes=[xdtk])
            pr.copy("act", bcT[:], ptb[:, 0:512].rearrange("p (k n) -> p k n", k=4), reads=["ptb"], writes=[bcTk])
            pr.copy("dve", sm[:, 16:48], pmA[:, 256:288], reads=["pmA"], writes=[smk + "at"])
            pr.copy("dve", acsT[:], pmA[0:16, 320:448], reads=["pmA"], writes=[acsTk])
            pr.ts("dve", nacsT[:], acsT[:], -1.0, None, ALU.mult, reads=[acsTk], writes=[nacsTk])
            Bm, Bmk = R(BmR, "Bm", c)
            pr.tt("pool", Bm[:].rearrange("k (h q) -> k h q", h=16), sel[:].rearrange("k (h q) -> k h q", h=16),
                  acsT[:].unsqueeze(1).broadcast_to([16, 16, 128]), ALU.mult, reads=["sel", acsTk], writes=[Bmk])

        def s1b(c):
            t = order[c]
            t0 = t * 128
            xbc, xk_ = R(xbcR, "xbc", c)
            dt, dtk = R(dtR, "dt", c)
            sm, smk = R(smR, "sm", c)
            bcT, bcTk = R(bcTR, "bcT", c)
            cbt, cbtk = R(cbtR, "cbt", c)
            acsT, acsTk = R(acsTR, "acsT", c)
            nacsT, nacsTk = R(nacsTR, "nacsT", c)
            Bm, Bmk = R(BmR, "Bm", c)
            xw, xwk = R(xwR, "xw", c)
            mt, mtk = R(mtR, "mt", c)
            la, acs, tot, eacs, etot, wdec, dtw = smv(sm)
            dtd = dt[:, 16 * d:16 * d + 16]
            if d:
                b = c % 2
                pr.dma(yfb[b][:], g.YF[t0:t0 + 128, :], writes=["yfb%d" % b])
                pr.dma(zb[b][:], g.P[t0:t0 + 128, C_Z:C_Z + 1024], writes=["zb%d" % b])
            for gi in range(2):
                pr.mm(pyd1[:, gi * 128:(gi + 1) * 128], bcT[:, gi, :], bcT[:, 2 + gi, :], inc=(gi == 1),
                      reads=[bcTk], writes=["pyd"])
            def seg_mm(hb):
                ps_ = pseg[hb % 2]
                pk = "pseg%d" % (hb % 2)
                pr.mm(ps_[:], identb[:], maskb[:], start=True, stop=False, inc=False,
                      reads=["ident_b", CK[4]], writes=[pk])
                pr.mm(ps_[:], ones[0:16, :], Bm[:, hb * 512:(hb + 1) * 512], start=False, stop=False, inc=False,
                      reads=["ones", Bmk], writes=[pk])
                pr.mm(ps_[:], nacsT[:], sel[:, hb * 512:(hb + 1) * 512], start=False, stop=True, inc=True,
                      reads=["sel", nacsTk], writes=[pk])

            def seg_ev(hb):
                ps_ = pseg[hb % 2]
                pk = "pseg%d" % (hb % 2)
                gi = hb // 2
                e_ = ebf[hb % 2]
                ek = "ebf%d" % (hb % 2)
                pr.act(e_[:], ps_[:], AF.Exp, reads=[pk], writes=[ek])
                pr.tt("dve", mt[:, hb * 512:(hb + 1) * 512].rearrange("p (j q) -> p j q", j=4),
                      e_[:].rearrange("p (j q) -> p j q", j=4),
                      cbt[:, gi, :].unsqueeze(1).broadcast_to([128, 4, 128]), ALU.mult,
                      reads=[ek, cbtk], writes=[mtk])

            seg_mm(0)
            pr.copy("dve", cbt[:], pyd1[:, 0:256].rearrange("p (k n) -> p k n", k=2), reads=["pyd"], writes=[cbtk])
            pr.tt("dve", sm[:, 48:64], tot, acs, ALU.subtract, reads=[smk + "at"], writes=[smk + "wd"])
            seg_mm(1)
            seg_ev(0)
            seg_mm(2)
            seg_ev(1)
            seg_mm(3)
            seg_ev(2)
            seg_ev(3)
            pr.act(sm[:, 64:112], sm[:, 16:64], AF.Exp, reads=[smk + "at", smk + "wd"], writes=[smk + "ex"])
            xs3 = xbc[:, 0:1024].rearrange("p (h e) -> p h e", e=64)
            pr.tt("dve", dtw, dtd, wdec, ALU.mult, reads=[dtk, smk + "ex"], writes=[smk + "dtw"])
            pr.tt("pool", xw[:].rearrange("p (h e) -> p h e", e=64), xs3,
                  dtw.unsqueeze(2).broadcast_to([128, 16, 64]), ALU.mult, reads=[xk_, smk + "dtw"], writes=[xwk])

        def s2(c):
            t = order[c]
            b = c % 2
            B = str(b)
            t0 = t * 128
            xbc, xk_ = R(xbcR, "xbc", c)
            sm, smk = R(smR, "sm", c)
            bcbf, bcbfk = R(bcbfR, "bcbf", c)
            bcT, bcTk = R(bcTR, "bcT", c)
            xdt, xdtk = R(xdtR, "xdt", c)
            xw, xwk = R(xwR, "xw", c)
            mt, mtk = R(mtR, "mt", c)
            la, acs, tot, eacs, etot, wdec, dtw = smv(sm)
            xs3 = xbc[:, 0:1024].rearrange("p (h e) -> p h e", e=64)
            Sc, Sck = SR[c % 2], "S%d" % (c % 2)
            Sn, Snk = SR[(c + 1) % 2], "S%d" % ((c + 1) % 2)
            Sbf, Sbfk = SbfR[c % 2], "Sbf%d" % (c % 2)
            Sbn, Sbnk = SbfR[(c + 1) % 2], "Sbf%d" % ((c + 1) % 2)
            for gi in range(2):
                pr.mm(psd[gi][:], bcbf[:, gi * 128:(gi + 1) * 128], xw[:, gi * 512:(gi + 1) * 512],
                      reads=[bcbfk, xwk], writes=["psd%d" % gi])
            pr.tt("dve", Sn[:].rearrange("p (h e) -> p h e", e=64), Sc[:].rearrange("p (h e) -> p h e", e=64),
                  etot.unsqueeze(2).broadcast_to([128, 16, 64]), ALU.mult, reads=[Sck, smk + "ex"], writes=[Snk])
            for gi in range(2):
                sl = slice(gi * 512, (gi + 1) * 512)
                pr.tt("dve", Sn[:, sl], psd[gi][:], Sn[:, sl], ALU.add, reads=["psd%d" % gi, Snk], writes=[Snk])
            pr.copy("act", Sbn[:], Sn[:], reads=[Snk], writes=[Sbnk])
            y = ysb[b]
            yk = "ysb" + B
            for gi in range(2):
                pr.mm(pyo1[:], bcT[:, 2 + gi, :], Sbf[:, gi * 512:(gi + 1) * 512],
                      reads=[bcTk, Sbfk], writes=["pyo"])
                for h in range(gi * 8, gi * 8 + 8):
                    pr.mm(pyd1[:, (h % 8) * 64:(h % 8 + 1) * 64], mt[:, h * 128:(h + 1) * 128],
                          xdt[:, h * 64:(h + 1) * 64], inc=(h % 8 == 7), reads=[mtk, xdtk], writes=["pyd"])
                sl = slice(gi * 512, (gi + 1) * 512)
                pr.tt("dve", ytmp[:, sl].rearrange("p (h e) -> p h e", e=64),
                      pyo1[:].rearrange("p (h e) -> p h e", e=64),
                      eacs[:, gi * 8:(gi + 1) * 8].unsqueeze(2).broadcast_to([128, 8, 64]), ALU.mult,
                      reads=["pyo", smk + "ex"], writes=["ytmp"])
                pr.tt("dve", y[:, sl], ytmp[:, sl], pyd1[:], ALU.add, reads=["ytmp", "pyd"], writes=[yk])
            if d == 0:
                pr.dma(g.YF[t0:t0 + 128, :], y[:], reads=[yk], eng="pool")
            else:
                yf, z = yfb[b], zb[b]
                yfk, zk = "yfb" + B, "zb" + B
                pr.tt("dve", y[:], y[:], yf[:], ALU.add, reads=[yk, yfk], writes=[yk])
                pr.tt("pool", ytmp[:].rearrange("p (h e) -> p h e", e=64), xs3,
                      dsum[:].unsqueeze(2).broadcast_to([128, 16, 64]), ALU.mult, reads=[xk_, "dsum"], writes=["ytmp"])
                pr.tt("dve", y[:], y[:], ytmp[:], ALU.add, reads=[yk, "ytmp"], writes=[yk])
                pr.act(z[:], z[:], AF.Silu, reads=[zk], writes=[zk])
                pr.tt("dve", ub[:], y[:], z[:], ALU.mult, reads=[yk, zk], writes=["ub"])
                for gi in range(2):
                    sl = slice(gi * 512, (gi + 1) * 512)
                    pr.act(ytmp[:, sl], ub[:, sl], AF.Square, reads=["ub"], writes=["ytmp", "ms"],
                           accum_out=ms[:, gi:gi + 1])
                pr.ts("dve", ms[:, 2:4], ms[:, 0:2], 1.0 / 512, EPS, ALU.mult, ALU.add, reads=["ms"], writes=["ms2"])
                pr.act(ms[:, 2:4], ms[:, 2:4], AF.Sqrt, reads=["ms2"], writes=["ms2"])
                pr.op("dve", lambda e: e.reciprocal(ms[:, 2:4], ms[:, 2:4]), reads=["ms2"], writes=["ms2"])
                for gi in range(2):
                    sl = slice(gi * 512, (gi + 1) * 512)
                    pr.stt("dve", y[:, sl], ub[:, sl], ms[:, 2 + gi:3 + gi], ssdw[:, sl], ALU.mult, ALU.mult,
                           reads=["ub", "ms2", "ssdw"], writes=[yk])
                pr.dma(g.YS[t0:t0 + 128, :], y[:], reads=[yk], eng="pool")

        en = ("0", "1a", "1b", "2")
        for i in range(-3, NT):
            if 0 <= i + 3 < NT and "0" in en:
                s0(i + 3)
            if 0 <= i + 2 < NT and "1a" in en:
                s1a(i + 2)
            if 0 <= i + 1 < NT and "1b" in en:
                s1b(i + 1)
            if i >= 0 and "2" in en:
                s2(i)


def phase_NP(g, l):
    with ExitStack() as st:
        cst = load_consts(g, st, ["ident_b"])
        np_tile = np_setup(g, st, l, cst["ident_b"])
        for t in range(NT):
            np_tile(t)


def np_setup(g, st, l, identb):
    pr, I, nc = g.pr, g.I, g.nc
    if True:
        wq = sbt(g, st, "wq", [128, 64], F32)
        wk = sbt(g, st, "wk", [128, 64], F32)
        pr.dma(wq[:], rowb(I["q_norm_w"][l:l + 1, :], 64), writes=["wq"])
        pr.dma(wk[:], rowb(I["k_norm_w"][l:l + 1, :], 64), writes=["wk"])
        pr.ts("dve", wq[:], wq[:], 0.125, None, ALU.mult, reads=["wq"], writes=["wq"])
        rc = [sbt(g, st, "rc%d" % i, [128, 64], F32) for i in range(2)]
        rsn = [sbt(g, st, "rsn%d" % i, [128, 64], F32) for i in range(2)]
        xin = [sbt(g, st, "xin%d" % i, [128, 1024], F32) for i in range(2)]
        junk = sbt(g, st, "njunk", [128, 1024], F32)
        xn = sbt(g, st, "xn", [128, 1024], F32)
        t1 = sbt(g, st, "t1", [128, 1024], F32)
        t2 = sbt(g, st, "t2", [128, 1024], F32)
        xr = sbt(g, st, "xr", [128, 1024], BF16)
        ss = sbt(g, st, "ss", [128, 32], F32)
        xT = [sbt(g, st, "xT%d" % i, [128, 8, 128], BF16) for i in range(2)]
        ptr = [pst(g, st, "ptrn%d" % i, [128, 1024], BF16) for i in range(2)]
        nn = [0]

        def np_tile(t):
            n = nn[0]
            t0 = t * 128
            lat = t >= 2
            if lat:
                pr.dma(rc[t % 2][:], I["ropec"][(t - 2) * 128:(t - 1) * 128, :], writes=["rc%d" % (t % 2)])
                pr.dma(rsn[t % 2][:], I["ropes"][(t - 2) * 128:(t - 1) * 128, :], writes=["rsn%d" % (t % 2)])
            for wi, (c0, w, wn, dst) in enumerate([(C_Q, wq, "wq", g.QT), (C_K, wk, "wk", g.KT)]):
                x = xin[n % 2]
                xk = "xin%d" % (n % 2)
                pr.dma(x[:], g.P[t0:t0 + 128, c0:c0 + 1024], writes=[xk])
                pr.act(junk[:], x[:], AF.Square, reads=[xk], writes=["njunk"])
                pr.op("dve", lambda e, junk=junk, ss=ss: e.reduce_sum(
                    ss[:, 0:16], junk[:].rearrange("p (h e) -> p h e", e=64), AX.X),
                    reads=["njunk"], writes=["ss"])
                pr.ts("dve", ss[:, 16:32], ss[:, 0:16], 1.0 / 64, EPS, ALU.mult, ALU.add, reads=["ss"], writes=["ss2"])
                pr.act(ss[:, 16:32], ss[:, 16:32], AF.Sqrt, reads=["ss2"], writes=["ss2"])
                pr.op("dve", lambda e, ss=ss: e.reciprocal(ss[:, 16:32], ss[:, 16:32]), reads=["ss2"], writes=["ss2"])
                x3 = x[:].rearrange("p (h e) -> p h e", e=64)
                xn3 = xn[:].rearrange("p (h e) -> p h e", e=64)
                pr.tt("dve", xn3, x3, ss[:, 16:32].unsqueeze(2).broadcast_to([128, 16, 64]), ALU.mult,
                      reads=[xk, "ss2"], writes=["xn"])
                pr.tt("pool", xn3, xn3, w[:].unsqueeze(1).broadcast_to([128, 16, 64]), ALU.mult,
                      reads=["xn", wn], writes=["xn"])
                if lat:
                    c_ = rc[t % 2]
                    s_ = rsn[t % 2]
                    pr.tt("dve", t1[:].rearrange("p (h e) -> p h e", e=64), xn3,
                          c_[:].unsqueeze(1).broadcast_to([128, 16, 64]), ALU.mult,
                          reads=["xn", "rc%d" % (t % 2)], writes=["t1"])
                    xn4 = xn[:].rearrange("p (h b e) -> p h b e", b=2, e=32)
                    t24 = t2[:].rearrange("p (h b e) -> p h b e", b=2, e=32)
                    s4 = s_[:].rearrange("p (b e) -> p b e", e=32)
                    pr.tt("pool", t24[:, :, :, 0:16], xn4[:, :, :, 16:32],
                          s4[:, :, 0:16].unsqueeze(1).broadcast_to([128, 16, 2, 16]), ALU.mult,
                          reads=["xn", "rsn%d" % (t % 2)], writes=["t2a"])
                    pr.tt("pool", t24[:, :, :, 16:32], xn4[:, :, :, 0:16],
                          s4[:, :, 16:32].unsqueeze(1).broadcast_to([128, 16, 2, 16]), ALU.mult,
                          reads=["xn", "rsn%d" % (t % 2)], writes=["t2b"])
                    pr.tt("dve", xr[:], t1[:], t2[:], ALU.add, reads=["t1", "t2a", "t2b"], writes=["xr"])
                else:
                    pr.copy("dve", xr[:], xn[:], reads=["xn"], writes=["xr"])
                p = ptr[n % 2]
                pk = "ptrn%d" % (n % 2)
                for k in range(8):
                    pr.tr(p[:, k * 128:(k + 1) * 128], xr[:, k * 128:(k + 1) * 128], identb[:], inc=(k == 7),
                          reads=["xr", "ident_b"], writes=[pk])
                o = xT[n % 2]
                ok = "xT%d" % (n % 2)
                pr.copy("act", o[:], p[:].rearrange("p (k n) -> p k n", k=8), reads=[pk], writes=[ok])
                pr.dma(dst[t], o[:], reads=[ok], writes=["%s%d" % ("QT" if wi == 0 else "KT", t)], eng="pool")
                n += 1
            nn[0] = n

        return np_tile


def phase_NA(g, l, fuse_np=False):
    pr, I, nc = g.pr, g.I, g.nc
    LOOK = 6
    with ExitStack() as st:
        cst = load_consts(g, st, ["ident_b"])
        identb = cst["ident_b"]
        np_done = [NT]
        if fuse_np:
            np_tile = np_setup(g, st, l, identb)
            np_done[0] = 0

        def np_upto(t):
            while np_done[0] <= min(t, NT - 1):
                np_tile(np_done[0])
                np_done[0] += 1

        np_upto(LOOK - 1)
        tab0 = sbt(g, st, "tab0", [128, 16, 640], BF16)
        tabE = sbt(g, st, "tabE", [128, 16, 640], BF16)
        for h in range(16):
            pr.dma(tab0[:, h, :], I["nab"][l, 0, h], writes=["tab0"], eng="pool")
        for h4 in range(4):
            pr.act(tab0[:, 4 * h4:4 * h4 + 4, :], tab0[:, 4 * h4:4 * h4 + 4, :], AF.Exp, reads=["tab0"], writes=["tab0"])
        kr = [sbt(g, st, "kr%d" % i, [128, 8, 128], BF16) for i in range(6)]
        vr = [sbt(g, st, "vr%d" % i, [128, 16, 65], BF16) for i in range(6)]
        kc = [sbt(g, st, "kc%d" % i, [128, 8, 128], BF16) for i in range(2)]
        vc = [sbt(g, st, "vc%d" % i, [128, 16, 65], BF16) for i in range(2)]
        for i in range(6):
            pr.memset("dve", vr[i][:, :, 64:65], 1.0, writes=["vr1_%d" % i])
        for i in range(2):
            pr.memset("dve", vc[i][:, :, 64:65], 1.0, writes=["vc1_%d" % i])
            pr.dma(kc[i][:], g.KT[i], reads=["KT%d" % i], writes=["kc%d" % i])
            pr.dma(vc[i][:, :, 0:64], g.P[i * 128:(i + 1) * 128, C_V:C_V + 1024].rearrange("p (h e) -> p h e", e=64),
                   writes=["vc%d" % i], eng="pool")
        qb = [sbt(g, st, "qb%d" % i, [128, 8, 128], BF16) for i in range(2)]
        et = [sbt(g, st, "et%d" % i, [128, 7, 128], BF16) for i in range(2)]
        yb = [sbt(g, st, "ynb%d" % i, [128, 1024], F32) for i in range(2)]
        rec = sbt(g, st, "rec", [128, 8], F32)
        pA = [pst(g, st, "pA%d" % i, [128, 512], F32) for i in range(2)]
        pB = [pst(g, st, "pB%d" % i, [128, 512], F32) for i in range(2)]
        pO = [pst(g, st, "pO%d" % i, [128, 512], F32) for i in range(2)]
        loaded = [0]

        def load_chunks(upto):
            while loaded[0] <= min(upto, 63):
                c = loaded[0]
                sl = c % 6
                pr.dma(kr[sl][:], g.KT[c + 2], reads=["KT%d" % (c + 2)], writes=["kr%d" % sl])
                pr.dma(vr[sl][:, :, 0:64],
                       g.P[(c + 2) * 128:(c + 3) * 128, C_V:C_V + 1024].rearrange("p (h e) -> p h e", e=64),
                       writes=["vr%d" % sl], eng="pool")
                loaded[0] += 1

        def cbase(jq):
            return min(max(jq - 2, 0), 59)

        hcount_box = [0]
        pr.dma(qb[0][:], g.QT[0], reads=["QT0"], writes=["qb0"])
        for t in range(NT):
            np_upto(t + LOOK)
            jq = t - 2
            q = qb[t % 2]
            qk = "qb%d" % (t % 2)
            if t + 1 < NT:
                pr.dma(qb[(t + 1) % 2][:], g.QT[t + 1], reads=["QT%d" % (t + 1)], writes=["qb%d" % ((t + 1) % 2)])
            if jq >= 0:
                load_chunks(cbase(min(jq + 1, 63)) + 4)
                cls = {0: 1, 1: 2, 62: 3, 63: 4}.get(jq, 0)
                if cls:
                    for h in range(16):
                        pr.dma(tabE[:, h, :], I["nab"][l, cls, h], writes=["tabE"], eng="pool")
                    for h4 in range(4):
                        pr.act(tabE[:, 4 * h4:4 * h4 + 4, :], tabE[:, 4 * h4:4 * h4 + 4, :], AF.Exp,
                               reads=["tabE"], writes=["tabE"])
                tab, tabk = (tabE, "tabE") if cls else (tab0, "tab0")
                cb = cbase(jq)
                chunks = [(kr[(cb + i) % 6], "kr%d" % ((cb + i) % 6), vr[(cb + i) % 6],
                           ["vr%d" % ((cb + i) % 6), "vr1_%d" % ((cb + i) % 6)]) for i in range(5)]
            else:
                chunks = []
                tab = None
            chunks = chunks + [(kc[i], "kc%d" % i, vc[i], ["vc%d" % i, "vc1_%d" % i]) for i in range(2)]
            first = 0 if jq >= 0 else 5
            y = yb[t % 2]
            yk = "ynb%d" % (t % 2)
            hinfo = {}

            def s_stage(h):
                p_, bp = h // 2, (h % 2) * 64
                hc = hcount_box[0]
                hcount_box[0] += 1
                a_, b_ = pA[hc % 2], pB[hc % 2]
                ak, bk = "pA%d" % (hc % 2), "pB%d" % (hc % 2)
                e_ = et[hc % 2]
                ek = "et%d" % (hc % 2)
                hinfo[h] = (e_, ek)
                qv = q[bp:bp + 64, p_, :]
                if jq >= 0:
                    for ci in range(4):
                        kt, kk = chunks[ci][0], chunks[ci][1]
                        pr.mm(a_[:, ci * 128:(ci + 1) * 128], kt[bp:bp + 64, p_, :], qv, start=True, stop=True,
                              inc=(ci == 3), reads=[kk, qk], writes=[ak])
                    pr.mm(b_[:, 0:128], chunks[4][0][bp:bp + 64, p_, :], qv, start=True, stop=True, inc=False,
                          reads=[chunks[4][1], qk], writes=[bk])
                for i in range(2):
                    kt, kk = chunks[-2 + i][0], chunks[-2 + i][1]
                    pr.mm(b_[:, 128 + i * 128:256 + i * 128], kt[bp:bp + 64, p_, :], qv, start=True, stop=True,
                          inc=(i == 1), reads=[kk, qk], writes=[bk])
                if jq >= 0:
                    pr.act(e_[:, 0:4, :], a_[:].rearrange("p (c q) -> p c q", c=4), AF.Exp, reads=[ak], writes=[ek])
                    pr.act(e_[:, 4:7, :], b_[:, 0:384].rearrange("p (c q) -> p c q", c=3), AF.Exp,
                           reads=[bk], writes=[ek])
                    pr.tt("dve", e_[:, 0:5, :], e_[:, 0:5, :], tab[:, h, :].rearrange("p (c q) -> p c q", c=5),
                          ALU.mult, reads=[ek, tabk], writes=[ek])
                else:
                    pr.act(e_[:, 5:7, :], b_[:, 128:384].rearrange("p (c q) -> p c q", c=2), AF.Exp,
                           reads=[bk], writes=[ek])

            def pv_stage(h):
                hg, j = h // 4, h % 4
                po = pO[hg % 2]
                pok = "pO%d" % (hg % 2)
                po3 = po[:].rearrange("p (j e) -> p j e", j=4)
                e_, ek = hinfo[h]
                for ci in range(first, 7):
                    vt, vks = chunks[ci - first][2], chunks[ci - first][3]
                    pr.mm(po3[:, j, 0:65], e_[:, ci, :], vt[:, h, :], start=(ci == first), stop=(ci == 6),
                          inc=(ci == 6), reads=[ek] + vks, writes=[pok])
                if j == 3:
                    pr.op("dve", lambda e, po3=po3, rec=rec, hg=hg: e.reciprocal(
                        rec[:, (hg % 2) * 4:(hg % 2) * 4 + 4].unsqueeze(2), po3[:, :, 64:65]),
                        reads=[pok], writes=["rec%d" % (hg % 2)])
                    pr.tt("dve", y[:, hg * 256:(hg + 1) * 256].rearrange("p (j e) -> p j e", j=4), po3[:, :, 0:64],
                          rec[:, (hg % 2) * 4:(hg % 2) * 4 + 4].unsqueeze(2).broadcast_to([128, 4, 64]), ALU.mult,
                          reads=[pok, "rec%d" % (hg % 2)], writes=[yk])

            s_stage(0)
            for h in range(16):
                if h + 1 < 16:
                    s_stage(h + 1)
                pv_stage(h)
            pr.dma(g.YN[t * 128:(t + 1) * 128, :], y[:], reads=[yk], eng="pool")


def load_w_bf16(g, t, src, nk, key):
    for k in range(nk):
        g.pr.dma(t[:, k, :], src[k * 128:(k + 1) * 128, :], writes=[key], eng="pool")


def mod_row(g, l, v, which):
    return rowb(g.MOD[l, v:v + 1, which * 1024:(which + 1) * 1024], 1024)


def phase_C1(g, l):
    pr, I, nc = g.pr, g.I, g.nc
    with ExitStack() as st:
        cst = load_consts(g, st, ["ident_b"])
        identb = cst["ident_b"]
        wbs = sbt(g, st, "wbs", [128, 8, 1024], BF16)
        wbn = sbt(g, st, "wbn", [128, 8, 1024], BF16)
        wo = sbt(g, st, "wo", [128, 8, 1024], BF16)
        load_w_bf16(g, wbs, I["w_br_ssd"][l], 8, "wbs")
        load_w_bf16(g, wbn, I["w_br_na"][l], 8, "wbn")
        load_w_bf16(g, wo, I["w_out"][l], 8, "wo")
        gate = sbt(g, st, "gate1", [128, 1024], F32)
        ysb = [sbt(g, st, "c1ys%d" % i, [128, 1024], BF16) for i in range(2)]
        ynb = [sbt(g, st, "c1yn%d" % i, [128, 1024], BF16) for i in range(2)]
        gb = [sbt(g, st, "c1g%d" % i, [128, 2048], F32) for i in range(2)]
        hb = [sbt(g, st, "c1h%d" % i, [128, 1024], F32) for i in range(2)]
        ysT = sbt(g, st, "ysT", [128, 8, 128], BF16)
        ynT = sbt(g, st, "ynT", [128, 8, 128], BF16)
        mixT = sbt(g, st, "mixT", [128, 8, 128], BF16)
        tmp = sbt(g, st, "c1tmp", [128, 1024], F32)
        tmp2 = sbt(g, st, "c1tmp2", [128, 1024], F32)
        mixb = sbt(g, st, "mixb", [128, 1024], BF16)
        hn = [sbt(g, st, "c1hn%d" % i, [128, 1024], F32) for i in range(2)]
        ptr = [pst(g, st, "c1ptr%d" % i, [128, 1024], BF16) for i in range(2)]
        pb1 = [pst(g, st, "pb1%d" % i, [128, 512], F32) for i in range(2)]
        pb2 = [pst(g, st, "pb2%d" % i, [128, 512], F32) for i in range(2)]
        pmo = [pst(g, st, "pmo%d" % i, [128, 512], F32) for i in range(2)]

        def loads(t):
            i = t % 2
            r = slice(t * 128, (t + 1) * 128)
            pr.dma(ysb[i][:], g.YS[r, :], writes=["c1ys%d" % i], eng="pool")
            pr.dma(ynb[i][:], g.YN[r, :], writes=["c1yn%d" % i], eng="pool")
            pr.dma(gb[i][:], g.P[r, 0:2048], writes=["c1g%d" % i])
            pr.dma(hb[i][:], src_rows(g, l, t), writes=["c1h%d" % i])

        hb.append(sbt(g, st, "c1h2", [128, 1024], F32))
        hb.append(sbt(g, st, "c1h3", [128, 1024], F32))
        mixb2 = [mixb, sbt(g, st, "mixb1", [128, 1024], BF16)]

        def loads3(t):
            i = t % 2
            r = slice(t * 128, (t + 1) * 128)
            pr.dma(ysb[i][:], g.YS[r, :], writes=["c1ys%d" % i], eng="pool")
            pr.dma(ynb[i][:], g.YN[r, :], writes=["c1yn%d" % i], eng="pool")
            pr.dma(gb[i][:], g.P[r, 0:2048], writes=["c1g%d" % i])
            pr.dma(hb[t % 4][:], src_rows(g, l, t), writes=["c1h%d" % (t % 4)])

        def stA(t):
            i = t % 2
            for k in range(8):
                pr.tr(ptr[0][:, k * 128:(k + 1) * 128], ysb[i][:, k * 128:(k + 1) * 128], identb[:], inc=(k == 7),
                      reads=["c1ys%d" % i, "ident_b"], writes=["c1ptr0"])
            pr.copy("act", ysT[:], ptr[0][:].rearrange("p (k n) -> p k n", k=8), reads=["c1ptr0"], writes=["ysT"])
            for k in range(8):
                pr.tr(ptr[1][:, k * 128:(k + 1) * 128], ynb[i][:, k * 128:(k + 1) * 128], identb[:], inc=(k == 7),
                      reads=["c1yn%d" % i, "ident_b"], writes=["c1ptr1"])
            pr.copy("dve", ynT[:], ptr[1][:].rearrange("p (k n) -> p k n", k=8), reads=["c1ptr1"], writes=["ynT"])
            for cg in range(2):
                for k in range(8):
                    pr.mm(pb1[cg][:], ysT[:, k, :], wbs[:, k, cg * 512:(cg + 1) * 512], start=(k == 0), stop=(k == 7),
                          inc=(k == 7), reads=["ysT", "wbs"], writes=["pb1%d" % cg])
            for cg in range(2):
                for k in range(8):
                    pr.mm(pb2[cg][:], ynT[:, k, :], wbn[:, k, cg * 512:(cg + 1) * 512], start=(k == 0), stop=(k == 7),
                          inc=(k == 7), reads=["ynT", "wbn"], writes=["pb2%d" % cg])
            gk = "c1g%d" % i
            pr.act(gb[i][:], gb[i][:], AF.Sigmoid, reads=[gk], writes=[gk])
            for cg in range(2):
                sl = slice(cg * 512, (cg + 1) * 512)
                pr.tt("dve", tmp[:, sl], pb1[cg][:], gb[i][:, sl], ALU.mult, reads=["pb1%d" % cg, gk], writes=["c1tmp"])
                pr.tt("dve", tmp2[:, sl], pb2[cg][:], gb[i][:, 1024 + cg * 512:1024 + (cg + 1) * 512], ALU.mult,
                      reads=["pb2%d" % cg, gk], writes=["c1tmp2"])
            pr.tt("pool", mixb2[i][:], tmp[:], tmp2[:], ALU.add, reads=["c1tmp", "c1tmp2"], writes=["mixb%d" % i])
            if t + 2 < NT:
                loads3(t + 2)

        def stB(t):
            i = t % 2
            if t == 0 or t == 2:
                pr.dma(gate[:], mod_row(g, l, 0 if t == 0 else 1, 2), writes=["gate1"])
            for k in range(8):
                pr.tr(ptr[0][:, k * 128:(k + 1) * 128], mixb2[i][:, k * 128:(k + 1) * 128], identb[:], inc=(k == 7),
                      reads=["mixb%d" % i, "ident_b"], writes=["c1ptr0"])
            pr.copy("act", mixT[:], ptr[0][:].rearrange("p (k n) -> p k n", k=8), reads=["c1ptr0"], writes=["mixT"])
            for cg in range(2):
                for k in range(8):
                    pr.mm(pmo[cg][:], mixT[:, k, :], wo[:, k, cg * 512:(cg + 1) * 512], start=(k == 0), stop=(k == 7),
                          inc=(k == 7), reads=["mixT", "wo"], writes=["pmo%d" % cg])
            o = hn[i]
            ok = "c1hn%d" % i
            for cg in range(2):
                sl = slice(cg * 512, (cg + 1) * 512)
                pr.tt("dve", o[:, sl], pmo[cg][:], gate[:, sl], ALU.mult, reads=["pmo%d" % cg, "gate1"], writes=[ok])
            pr.tt("pool", o[:], o[:], hb[t % 4][:], ALU.add, reads=[ok, "c1h%d" % (t % 4)], writes=[ok])
            pr.dma(g.H[t * 128:(t + 1) * 128, :], o[:], reads=[ok], eng="pool")

        loads3(0)
        loads3(1)
        stA(0)
        for t in range(NT):
            if t + 1 < NT:
                stA(t + 1)
            stB(t)


def norm2_setup(g, st, l):
    nw = sbt(g, st, "n2w", [128, 1024], F32)
    g.pr.dma(nw[:], rowb(g.I["norm2_w"][l:l + 1, :], 1024), writes=["n2w"])
    G2 = sbt(g, st, "G2", [128, 1024], F32)
    SH2 = sbt(g, st, "SH2", [128, 1024], F32)
    GT2 = sbt(g, st, "GT2", [128, 1024], F32)
    return nw, G2, SH2, GT2


def norm2_variant(g, l, v, nw, G2, SH2, GT2):
    pr = g.pr
    pr.dma(G2[:], mod_row(g, l, v, 4), writes=["G2"])
    pr.stt("dve", G2[:], G2[:], 1.0, nw[:], ALU.add, ALU.mult, reads=["G2", "n2w"], writes=["G2"])
    pr.dma(SH2[:], mod_row(g, l, v, 3), writes=["SH2"])
    pr.dma(GT2[:], mod_row(g, l, v, 5), writes=["GT2"])


def phase_C2(g, l):
    pr, I, nc = g.pr, g.I, g.nc
    with ExitStack() as st:
        cst = load_consts(g, st, ["ident_b"])
        identb = cst["ident_b"]
        w1 = sbt(g, st, "w1", [128, 8, D_FF], BF16)
        w3 = sbt(g, st, "w3", [128, 8, D_FF], BF16)
        w2 = sbt(g, st, "w2", [128, 22, 1024], BF16)
        load_w_bf16(g, w1, I["w_ff1"][0], 8, "w1")
        load_w_bf16(g, w3, I["w_ff3"][0], 8, "w3")
        load_w_bf16(g, w2, I["w_ff2"][0], 22, "w2")
        nw, G2, SH2, GT2 = norm2_setup(g, st, l)
        hb = [sbt(g, st, "c2h%d" % i, [128, 1024], F32) for i in range(2)]
        tmp = sbt(g, st, "c2tmp", [128, 1024], F32)
        fbf = sbt(g, st, "fbf", [128, 1024], BF16)
        fT = sbt(g, st, "fT", [128, 8, 128], BF16)
        ssq = sbt(g, st, "c2ssq", [128, 2], F32)
        stmp = [sbt(g, st, "stmp%d" % i, [128, 512], F32) for i in range(2)]
        actb2 = [sbt(g, st, "actb%d" % i, [128, D_FF], BF16) for i in range(2)]
        actT2 = [sbt(g, st, "actT%d" % i, [128, 22, 128], BF16) for i in range(2)]
        ho = [sbt(g, st, "c2ho%d" % i, [128, 1024], F32) for i in range(2)]
        ptr = [pst(g, st, "c2ptr%d" % i, [128, 1024], BF16) for i in range(2)]
        pu1 = [pst(g, st, "pu1%d" % i, [128, 512], F32) for i in range(2)]
        pu3 = [pst(g, st, "pu3%d" % i, [128, 512], F32) for i in range(2)]
        po = [pst(g, st, "po%d" % i, [128, 512], F32) for i in range(2)]
        pr.dma(hb[0][:], g.H[0:128, :], writes=["c2h0"])
        pr.dma(hb[1][:], g.H[128:256, :], writes=["c2h1"])

        def stA(t):
            i = t % 2
            hk = "c2h%d" % i
            h = hb[i]
            actb = actb2[i]
            abk = "actb%d" % i
            if t == 0 or t == 2:
                v = 0 if t == 0 else 1
                pr.dma(G2[:], mod_row(g, l, v, 4), writes=["G2"])
                pr.stt("dve", G2[:], G2[:], 1.0, nw[:], ALU.add, ALU.mult, reads=["G2", "n2w"], writes=["G2"])
                pr.dma(SH2[:], mod_row(g, l, v, 3), writes=["SH2"])
            rms_rstd(g, tmp[:], h[:], ssq[:, 0:1], ssq[:, 1:2], 1024, [hk], "c2ssq", "c2rs", "c2tmp")
            pr.stt("dve", tmp[:], h[:], ssq[:, 1:2], G2[:], ALU.mult, ALU.mult, reads=[hk, "c2rs", "G2"], writes=["c2tmp"])
            pr.tt("dve", fbf[:], tmp[:], SH2[:], ALU.add, reads=["c2tmp", "SH2"], writes=["fbf"])
            for k in range(8):
                pr.tr(ptr[0][:, k * 128:(k + 1) * 128], fbf[:, k * 128:(k + 1) * 128], identb[:], inc=(k == 7),
                      reads=["fbf", "ident_b"], writes=["c2ptr0"])
            pr.copy("act", fT[:], ptr[0][:].rearrange("p (k n) -> p k n", k=8), reads=["c2ptr0"], writes=["fT"])
            for cg in range(6):
                c0 = cg * 512
                n = min(512, D_FF - c0)
                a1, a3 = pu1[cg % 2], pu3[cg % 2]
                k1, k3 = "pu1%d" % (cg % 2), "pu3%d" % (cg % 2)
                for k in range(8):
                    pr.mm(a1[:, :n], fT[:, k, :], w1[:, k, c0:c0 + n], start=(k == 0), stop=(k == 7), inc=(k == 7),
                          reads=["fT", "w1"], writes=[k1])
                for k in range(8):
                    pr.mm(a3[:, :n], fT[:, k, :], w3[:, k, c0:c0 + n], start=(k == 0), stop=(k == 7), inc=(k == 7),
                          reads=["fT", "w3"], writes=[k3])
                s_ = stmp[cg % 2]
                sk = "stmp%d" % (cg % 2)
                pr.act(s_[:, :n], a1[:, :n], AF.Silu, reads=[k1], writes=[sk])
                pr.tt("dve", actb[:, c0:c0 + n], s_[:, :n], a3[:, :n], ALU.mult, reads=[sk, k3], writes=[abk])

        def stB1(t):
            i = t % 2
            actb, abk = actb2[i], "actb%d" % i
            actT, atk = actT2[i], "actT%d" % i
            for gi in range(3):
                k0 = gi * 8
                nk = min(8, 22 - k0)
                p = ptr[(gi + 1) % 2]
                pk = "c2ptr%d" % ((gi + 1) % 2)
                for k in range(nk):
                    pr.tr(p[:, k * 128:(k + 1) * 128], actb[:, (k0 + k) * 128:(k0 + k + 1) * 128], identb[:],
                          inc=(k == nk - 1), reads=[abk, "ident_b"], writes=[pk])
                pr.copy("act" if gi % 2 == 0 else "dve", actT[:, k0:k0 + nk, :],
                        p[:, 0:nk * 128].rearrange("p (k n) -> p k n", k=nk), reads=[pk], writes=[atk])

        def stB2(t):
            i = t % 2
            hk = "c2h%d" % i
            h = hb[i]
            actT, atk = actT2[i], "actT%d" % i
            if t == 0 or t == 2:
                pr.dma(GT2[:], mod_row(g, l, 0 if t == 0 else 1, 5), writes=["GT2"])
            for cg in range(2):
                for k in range(22):
                    pr.mm(po[cg][:], actT[:, k, :], w2[:, k, cg * 512:(cg + 1) * 512], start=(k == 0), stop=(k == 21),
                          inc=(k == 21), reads=[atk, "w2"], writes=["po%d" % cg])
            o = ho[i]
            ok = "c2ho%d" % i
            for cg in range(2):
                sl = slice(cg * 512, (cg + 1) * 512)
                pr.tt("dve", o[:, sl], po[cg][:], GT2[:, sl], ALU.mult, reads=["po%d" % cg, "GT2"], writes=[ok])
            pr.tt("pool", o[:], o[:], h[:], ALU.add, reads=[ok, hk], writes=[ok])
            pr.dma(g.H[t * 128:(t + 1) * 128, :], o[:], reads=[ok], eng="pool")
            if t + 2 < NT:
                pr.dma(h[:], g.H[(t + 2) * 128:(t + 3) * 128, :], writes=[hk])

        stA(0)
        for t in range(NT):
            stB1(t)
            if t + 1 < NT:
                stA(t + 1)
            stB2(t)


def phase_R(g, l):
    pr, I, nc = g.pr, g.I, g.nc
    with ExitStack() as st:
        cst = load_consts(g, st, ["ident_b", "ident"])
        identb, identf = cst["ident_b"], cst["ident"]
        wr = sbt(g, st, "wr", [128, 8, 8], F32)
        pr.dma(wr[:], I["w_router"][0].rearrange("(k p) e -> p k e", p=128), writes=["wr"])
        nw, G2, SH2, GT2 = norm2_setup(g, st, l)
        norm2_variant(g, l, 1, nw, G2, SH2, GT2)
        hb = [sbt(g, st, "rh%d" % i, [128, 1024], F32) for i in range(2)]
        junk = sbt(g, st, "rjunk", [128, 1024], F32)
        tmp = sbt(g, st, "rtmp", [128, 1024], F32)
        ffp = sbt(g, st, "ffp", [128, 1024], F32)
        fbf = sbt(g, st, "rfbf", [128, 1024], BF16)
        fT32 = sbt(g, st, "fT32", [128, 8, 128], F32)
        fT = [sbt(g, st, "rfT%d" % i, [128, 8, 128], BF16) for i in range(2)]
        sm = sbt(g, st, "rsm", [128, 64], F32)
        gt = [sbt(g, st, "rgt%d" % i, [128, 8], F32) for i in range(2)]
        ptf = pst(g, st, "ptf", [128, 1024], F32)
        ptr = pst(g, st, "rptr", [128, 1024], BF16)
        pl = pst(g, st, "pl", [128, 512], F32)
        lg, mk1, lg2, mk2 = sm[:, 0:8], sm[:, 8:16], sm[:, 16:24], sm[:, 24:32]
        ssq, rs, m1, m2, dl, g1, g2 = (sm[:, 32:33], sm[:, 33:34], sm[:, 34:35], sm[:, 35:36], sm[:, 36:37],
                                       sm[:, 37:38], sm[:, 38:39])
        pr.dma(hb[0][:], g.H[256:384, :], writes=["rh0"])
        for jq in range(64):
            t = jq + 2
            i = jq % 2
            h = hb[i]
            hk = "rh%d" % i
            if jq + 1 < 64:
                pr.dma(hb[(jq + 1) % 2][:], g.H[(t + 1) * 128:(t + 2) * 128, :], writes=["rh%d" % ((jq + 1) % 2)])
            rms_rstd(g, junk[:], h[:], ssq, rs, 1024, [hk], "rssq", "rrs", "rjunk")
            pr.stt("dve", tmp[:], h[:], rs, G2[:], ALU.mult, ALU.mult, reads=[hk, "rrs", "G2"], writes=["rtmp"])
            pr.tt("dve", ffp[:], tmp[:], SH2[:], ALU.add, reads=["rtmp", "SH2"], writes=["ffp"])
            pr.copy("pool", fbf[:], ffp[:], reads=["ffp"], writes=["rfbf"])
            for k in range(8):
                pr.tr(ptf[:, k * 128:(k + 1) * 128], ffp[:, k * 128:(k + 1) * 128], identf[:], inc=(k == 7),
                      reads=["ffp", "ident"], writes=["ptf"])
            pr.copy("act", fT32[:], ptf[:].rearrange("p (k n) -> p k n", k=8), reads=["ptf"], writes=["fT32"])
            for k in range(8):
                pr.mm(pl[:, 0:8], fT32[:, k, :], wr[:, k, :], start=(k == 0), stop=(k == 7), inc=(k == 7),
                      reads=["fT32", "wr"], writes=["pl"])
            pr.copy("dve", lg, pl[:, 0:8], reads=["pl"], writes=["lg"])
            pr.op("dve", lambda e: e.reduce_max(m1, lg, AX.X), reads=["lg"], writes=["m1"])
            pr.ts("dve", mk1, lg, m1, None, ALU.is_equal, reads=["lg", "m1"], writes=["mk1"])
            pr.stt("dve", lg2, mk1, -1e30, lg, ALU.mult, ALU.add, reads=["mk1", "lg"], writes=["lg2"])
            pr.op("dve", lambda e: e.reduce_max(m2, lg2, AX.X), reads=["lg2"], writes=["m2"])
            pr.ts("dve", mk2, lg2, m2, None, ALU.is_equal, reads=["lg2", "m2"], writes=["mk2"])
            pr.tt("dve", dl, m2, m1, ALU.subtract, reads=["m1", "m2"], writes=["dl"])
            pr.act(dl, dl, AF.Exp, reads=["dl"], writes=["dl"])
            pr.ts("dve", g1, dl, 1.0, None, ALU.add, reads=["dl"], writes=["g1"])
            pr.op("dve", lambda e: e.reciprocal(g1, g1), reads=["g1"], writes=["g1"])
            pr.tt("dve", g2, dl, g1, ALU.mult, reads=["dl", "g1"], writes=["g2"])
            go = gt[i]
            gk = "rgt%d" % i
            pr.ts("dve", go[:], mk1, g1, None, ALU.mult, reads=["mk1", "g1"], writes=[gk])
            pr.stt("dve", go[:], mk2, g2, go[:], ALU.mult, ALU.add, reads=["mk2", "g2", gk], writes=[gk])
            pr.dma(g.GT[jq], go[:], reads=[gk], eng="pool")
            for k in range(8):
                pr.tr(ptr[:, k * 128:(k + 1) * 128], fbf[:, k * 128:(k + 1) * 128], identb[:], inc=(k == 7),
                      reads=["rfbf", "ident_b"], writes=["rptr"])
            fo = fT[i]
            fk = "rfT%d" % i
            pr.copy("act", fo[:], ptr[:].rearrange("p (k n) -> p k n", k=8), reads=["rptr"], writes=[fk])
            pr.dma(g.FT[jq], fo[:], reads=[fk], eng="pool")


def phase_E(g, l):
    pr, I, nc = g.pr, g.I, g.nc
    NS = 8
    FQ = 896
    with ExitStack() as st:
        cst = load_consts(g, st, ["ident_b"])
        identb = cst["ident_b"]
        w1 = [sbt(g, st, "ew1_%d" % i, [128, 8, FQ], BF16) for i in range(2)]
        w3 = [sbt(g, st, "ew3_%d" % i, [128, 8, FQ], BF16) for i in range(2)]
        w2 = [sbt(g, st, "ew2_%d" % i, [128, 7, 1024], BF16) for i in range(2)]
        gate = sbt(g, st, "egate", [128, 1024], F32)
        pr.dma(gate[:], mod_row(g, l, 1, 5), writes=["egate"])
        fT = sbt(g, st, "efT", [128, NS, 8, 128], BF16)
        gt = sbt(g, st, "egt", [128, NS, 8], F32)
        acc = sbt(g, st, "eacc", [128, NS, 1024], F32)
        stmp = [sbt(g, st, "estmp%d" % i, [128, 512], F32) for i in range(2)]
        actb = sbt(g, st, "eactb", [128, FQ], BF16)
        actT = sbt(g, st, "eactT", [128, 7, 128], BF16)
        hb = [sbt(g, st, "eh%d" % i, [128, 1024], F32) for i in range(2)]
        ptr = pst(g, st, "eptr", [128, 1024], BF16)
        pu1 = [pst(g, st, "epu1%d" % i, [128, 512], F32) for i in range(2)]
        pu3 = [pst(g, st, "epu3%d" % i, [128, 512], F32) for i in range(2)]
        po = [pst(g, st, "epo%d" % i, [128, 512], F32) for i in range(2)]

        def load_unit(u):
            e, qd = u // 4, u % 4
            i = un_idx[0] % 2
            un_idx[0] += 1
            f0 = qd * FQ
            for k in range(8):
                pr.dma(w1[i][:, k, :], I["w_e1"][0, e, k * 128:(k + 1) * 128, f0:f0 + FQ], writes=["ew1_%d" % i], eng="pool")
                pr.dma(w3[i][:, k, :], I["w_e3"][0, e, k * 128:(k + 1) * 128, f0:f0 + FQ], writes=["ew3_%d" % i], eng="pool")
            for k in range(7):
                pr.dma(w2[i][:, k, :], I["w_e2"][0, e, f0 + k * 128:f0 + (k + 1) * 128, :], writes=["ew2_%d" % i], eng="pool")
            return i

        un_idx = [0]
        for stl in range(64 // NS):
            pr.dma(fT[:].rearrange("p t k n -> p t (k n)"),
                   g.FT[stl * NS:(stl + 1) * NS].rearrange("t p k n -> p t (k n)"), writes=["efT"])
            pr.dma(gt[:], g.GT[stl * NS:(stl + 1) * NS].rearrange("t p e -> p t e"), writes=["egt"])
            pr.memset("pool", acc[:], 0.0, writes=["eacc"])
            nxt = load_unit(0)
            for u in range(32):
                e = u // 4
                wi = nxt
                if u + 1 < 32:
                    nxt = load_unit(u + 1)
                for ti in range(NS):
                    for cg in range(2):
                        c0 = cg * 512
                        n = min(512, FQ - c0)
                        for k in range(8):
                            pr.mm(pu1[cg][:, :n], fT[:, ti, k, :], w1[wi][:, k, c0:c0 + n], start=(k == 0), stop=(k == 7),
                                  inc=(k == 7), reads=["efT", "ew1_%d" % wi], writes=["epu1%d" % cg])
                        for k in range(8):
                            pr.mm(pu3[cg][:, :n], fT[:, ti, k, :], w3[wi][:, k, c0:c0 + n], start=(k == 0), stop=(k == 7),
                                  inc=(k == 7), reads=["efT", "ew3_%d" % wi], writes=["epu3%d" % cg])
                        pr.act(stmp[cg][:, :n], pu1[cg][:, :n], AF.Silu, reads=["epu1%d" % cg], writes=["estmp%d" % cg])
                        pr.stt("dve", actb[:, c0:c0 + n], stmp[cg][:, :n], gt[:, ti, e:e + 1], pu3[cg][:, :n],
                               ALU.mult, ALU.mult, reads=["estmp%d" % cg, "egt", "epu3%d" % cg], writes=["eactb"])
                    for k in range(7):
                        pr.tr(ptr[:, k * 128:(k + 1) * 128], actb[:, k * 128:(k + 1) * 128], identb[:], inc=(k == 6),
                              reads=["eactb", "ident_b"], writes=["eptr"])
                    pr.copy("act", actT[:], ptr[:, 0:896].rearrange("p (k n) -> p k n", k=7), reads=["eptr"], writes=["eactT"])
                    for cg in range(2):
                        for k in range(7):
                            pr.mm(po[cg][:], actT[:, k, :], w2[wi][:, k, cg * 512:(cg + 1) * 512], start=(k == 0),
                                  stop=(k == 6), inc=(k == 6), reads=["eactT", "ew2_%d" % wi], writes=["epo%d" % cg])
                        sl = slice(cg * 512, (cg + 1) * 512)
                        pr.tt("dve", acc[:, ti, sl], po[cg][:], acc[:, ti, sl], ALU.add,
                              reads=["epo%d" % cg, "eacc"], writes=["eacc"])
            for ti in range(NS):
                jq = stl * NS + ti
                t = jq + 2
                h = hb[ti % 2]
                hk = "eh%d" % (ti % 2)
                pr.dma(h[:], g.H[t * 128:(t + 1) * 128, :], writes=[hk])
                pr.tt("dve", acc[:, ti, :], acc[:, ti, :], gate[:], ALU.mult, reads=["eacc", "egate"], writes=["eacc"])
                pr.tt("dve", h[:], h[:], acc[:, ti, :], ALU.add, reads=[hk, "eacc"], writes=[hk])
                pr.dma(g.out[jq * 128:(jq + 1) * 128, :], h[:], reads=[hk], eng="pool")


I32 = mybir.dt.int32


def phase_R2(g, l):
    pr, I, nc = g.pr, g.I, g.nc
    with ExitStack() as st:
        cst = load_consts(g, st, ["ident", "ones", "stri", "thr", "thr2"])
        identf, ones, stri, thr, thr2 = cst["ident"], cst["ones"], cst["stri"], cst["thr"], cst["thr2"]
        wr = sbt(g, st, "wr", [128, 8, 8], F32)
        pr.dma(wr[:], I["w_router"][0].rearrange("(k p) e -> p k e", p=128), writes=["wr"])
        nw, G2, SH2, GT2 = norm2_setup(g, st, l)
        norm2_variant(g, l, 1, nw, G2, SH2, GT2)
        hb = [sbt(g, st, "rh%d" % i, [128, 1024], F32) for i in range(2)]
        junk = sbt(g, st, "rjunk", [128, 1024], F32)
        tmp = sbt(g, st, "rtmp", [128, 1024], F32)
        ffp = sbt(g, st, "ffp", [128, 1024], F32)
        fbf = [sbt(g, st, "rfbf%d" % i, [128, 1024], BF16) for i in range(2)]
        fT32 = sbt(g, st, "fT32", [128, 8, 128], F32)
        sm = sbt(g, st, "rsm", [128, 64], F32)
        MK = sbt(g, st, "MK", [128, 64, 16], F32)
        GG = sbt(g, st, "GGs", [128, 128], F32)
        DF = sbt(g, st, "DF", [128, 128], F32)
        DI = sbt(g, st, "DI", [128, 128], I32)
        cm = sbt(g, st, "cm", [128, 192], F32)
        cnt = sbt(g, st, "cnt", [128, 64], F32)
        ptf = pst(g, st, "ptf", [128, 1024], F32)
        pl = pst(g, st, "pl", [128, 512], F32)
        pc = pst(g, st, "pc", [128, 512], F32)
        pk = [pst(g, st, "pk%d" % i, [128, 512], F32) for i in range(2)]
        lg, lg2, mk12 = sm[:, 0:8], sm[:, 16:24], sm[:, 8:16]
        ssq, rs, m1, m2, dl, g1 = (sm[:, 32:33], sm[:, 33:34], sm[:, 34:35], sm[:, 35:36], sm[:, 36:37],
                                   sm[:, 37:38])
        pr.dma(hb[0][:], g.H[256:384, :], writes=["rh0"])
        for jq in range(64):
            t = jq + 2
            i = jq % 2
            h = hb[i]
            hk = "rh%d" % i
            if jq + 1 < 64:
                pr.dma(hb[(jq + 1) % 2][:], g.H[(t + 1) * 128:(t + 2) * 128, :], writes=["rh%d" % ((jq + 1) % 2)])
            rms_rstd(g, junk[:], h[:], ssq, rs, 1024, [hk], "rssq", "rrs", "rjunk")
            pr.stt("dve", tmp[:], h[:], rs, G2[:], ALU.mult, ALU.mult, reads=[hk, "rrs", "G2"], writes=["rtmp"])
            pr.tt("dve", ffp[:], tmp[:], SH2[:], ALU.add, reads=["rtmp", "SH2"], writes=["ffp"])
            fo = fbf[i]
            fk = "rfbf%d" % i
            pr.copy("pool", fo[:], ffp[:], reads=["ffp"], writes=[fk])
            pr.dma(g.F[jq * 128:(jq + 1) * 128, :], fo[:], reads=[fk], writes=["F%d" % jq], eng="pool")
            for k in range(8):
                pr.tr(ptf[:, k * 128:(k + 1) * 128], ffp[:, k * 128:(k + 1) * 128], identf[:], inc=(k == 7),
                      reads=["ffp", "ident"], writes=["ptf"])
            pr.copy("act", fT32[:], ptf[:].rearrange("p (k n) -> p k n", k=8), reads=["ptf"], writes=["fT32"])
            for k in range(8):
                pr.mm(pl[:, 0:8], fT32[:, k, :], wr[:, k, :], start=(k == 0), stop=(k == 7), inc=(k == 7),
                      reads=["fT32", "wr"], writes=["pl"])
            mk1, mk2 = MK[:, jq, 0:8], MK[:, jq, 8:16]
            mkk = "MK%d" % jq
            pr.copy("dve", lg, pl[:, 0:8], reads=["pl"], writes=["lg"])
            pr.op("dve", lambda e, m1=m1, lg=lg: e.reduce_max(m1, lg, AX.X), reads=["lg"], writes=["m1"])
            pr.ts("dve", mk1, lg, m1, None, ALU.is_equal, reads=["lg", "m1"], writes=[mkk])
            pr.stt("dve", lg2, mk1, -1e30, lg, ALU.mult, ALU.add, reads=[mkk, "lg"], writes=["lg2"])
            pr.op("dve", lambda e, m2=m2, lg2=lg2: e.reduce_max(m2, lg2, AX.X), reads=["lg2"], writes=["m2"])
            pr.ts("dve", mk2, lg2, m2, None, ALU.is_equal, reads=["lg2", "m2"], writes=[mkk])
            pr.tt("dve", dl, m2, m1, ALU.subtract, reads=["m1", "m2"], writes=["dl"])
            pr.act(dl, dl, AF.Exp, reads=["dl"], writes=["dl"])
            pr.ts("dve", g1, dl, 1.0, None, ALU.add, reads=["dl"], writes=["g1"])
            pr.op("dve", lambda e, GG=GG, g1=g1, jq=jq: e.reciprocal(GG[:, 2 * jq:2 * jq + 1], g1),
                  reads=["g1"], writes=["GGs"])
            pr.tt("dve", GG[:, 2 * jq + 1:2 * jq + 2], dl, GG[:, 2 * jq:2 * jq + 1], ALU.mult,
                  reads=["dl", "GGs"], writes=["GGs"])
            pr.tt("dve", mk12, mk1, mk2, ALU.add, reads=[mkk], writes=["mk12"])
            pr.mm(pc[:, 0:8], ones[:], mk12, start=(jq == 0), stop=(jq == 63), reads=["ones", "mk12"], writes=["pc"])
        pr.dma(g.GG[:, :], GG[:], reads=["GGs"], eng="pool")
        c_cnt, c_nb, c_pe, c_base = cnt[:, 0:8], cnt[:, 8:16], cnt[:, 16:24], cnt[:, 24:32]
        pr.copy("dve", c_cnt, pc[:, 0:8], reads=["pc"], writes=["c_cnt"])
        pr.tt("dve", cm[:, 0:128].rearrange("p (e j) -> p e j", j=16), c_cnt.unsqueeze(2).broadcast_to([128, 8, 16]),
              thr[:].rearrange("p (e j) -> p e j", j=16), ALU.is_gt, reads=["c_cnt", "thr"], writes=["cm"])
        pr.op("dve", lambda e: e.reduce_sum(c_nb, cm[:, 0:128].rearrange("p (e j) -> p e j", j=16), AX.X),
              reads=["cm"], writes=["c_nb"])
        pr.ts("dve", c_nb, c_nb, 1024.0, None, ALU.mult, reads=["c_nb"], writes=["c_nb"])
        pr.copy("dve", c_pe[:, 0:1], c_nb[:, 0:1], reads=["c_nb"], writes=["c_pe"])
        for e_ in range(1, 8):
            pr.tt("dve", c_pe[:, e_:e_ + 1], c_pe[:, e_ - 1:e_], c_nb[:, e_:e_ + 1], ALU.add,
                  reads=["c_pe", "c_nb"], writes=["c_pe"])
        pr.tt("dve", c_base, c_pe, c_nb, ALU.subtract, reads=["c_pe", "c_nb"], writes=["c_base"])
        pr.tt("dve", cm[:].rearrange("p (s e) -> p s e", e=8), c_pe.unsqueeze(1).broadcast_to([128, 24, 8]),
              thr2[:].rearrange("p (s e) -> p s e", e=8), ALU.is_le, reads=["c_pe", "thr2", "cm"], writes=["cm"])
        be = cnt[:, 32:56]
        pr.op("dve", lambda e: e.reduce_sum(be, cm[:].rearrange("p (s e) -> p s e", e=8), AX.X),
              reads=["cm"], writes=["be"])
        pr.ts("dve", be, be, 7.0, None, ALU.min, reads=["be"], writes=["be"])
        pr.dma(g.BED[:, :], be, reads=["be"], eng="pool")
        for jq in range(64):
            for s_ in range(2):
                c = 2 * jq + s_
                mk = MK[:, jq, 8 * s_:8 * s_ + 8]
                p_ = pk[c % 2]
                pkk = "pk%d" % (c % 2)
                pr.mm(p_[:, 0:8], stri[:], mk, reads=["stri", "MK%d" % jq], writes=[pkk], inc=False)
                pr.mm(p_[:, 8:16], ones[:], mk, reads=["ones", "MK%d" % jq], writes=[pkk])
                pos, prod = sm[:, 40:48], sm[:, 48:56]
                pr.tt("dve", pos, p_[:, 0:8], c_base, ALU.add, reads=[pkk, "c_base"], writes=["pos"])
                pr.tt("dve", prod, pos, mk, ALU.mult, reads=["pos", "MK%d" % jq], writes=["prod"])
                pr.op("dve", lambda e, DF=DF, prod=prod, c=c: e.reduce_sum(DF[:, c:c + 1], prod, AX.X),
                      reads=["prod"], writes=["DF"])
                pr.tt("dve", c_base, p_[:, 8:16], c_base, ALU.add, reads=[pkk, "c_base"], writes=["c_base"])
        pr.copy("dve", DI[:], DF[:], reads=["DF"], writes=["DI"])
        pr.dma(g.DEST[:, :], DI[:], reads=["DI"], eng="pool")
        for jq in range(64):
            i = jq % 2
            fk = "rfbf%d" % i
            pr.dma(fbf[i][:], g.F[jq * 128:(jq + 1) * 128, :], reads=["F%d" % jq], writes=[fk])
            for s_ in range(2):
                c = 2 * jq + s_
                pr.idma(g.ROWS[:, :], DI[:, c:c + 1], fbf[i][:, :], None, reads=[fk, "DI"])


def phase_ES(g, l):
    pr, I, nc = g.pr, g.I, g.nc
    NS, FQ, NSB = 8, 896, 24
    with ExitStack() as st:
        cst = load_consts(g, st, ["ident_b", "kpq", "kp2"])
        identb, kpq, kp2 = cst["ident_b"], cst["kpq"], cst["kp2"]
        W1v = I["w_e1"].rearrange("a e d (q f) -> (a e d q) f", q=4)
        W3v = I["w_e3"].rearrange("a e d (q f) -> (a e d q) f", q=4)
        W2v = I["w_e2"].rearrange("a e f n -> (a e f) n")
        be = sbt(g, st, "esbe", [128, NSB], F32)
        pr.dma(be[:], g.BED[:, :], writes=["esbe"])
        i1f = sbt(g, st, "i1f", [128, NSB, 32], F32)
        i2f = sbt(g, st, "i2f", [128, NSB, 28], F32)
        i1 = sbt(g, st, "i1", [128, NSB * 32], I32)
        i2 = sbt(g, st, "i2", [128, NSB * 28], I32)
        pr.stt("dve", i1f[:], be[:].unsqueeze(2).broadcast_to([128, NSB, 32]), 4096.0,
               kpq[:].unsqueeze(1).broadcast_to([128, NSB, 32]), ALU.mult, ALU.add, reads=["esbe", "kpq"], writes=["i1f"])
        pr.stt("dve", i2f[:], be[:].unsqueeze(2).broadcast_to([128, NSB, 28]), 3584.0,
               kp2[:].unsqueeze(1).broadcast_to([128, NSB, 28]), ALU.mult, ALU.add, reads=["esbe", "kp2"], writes=["i2f"])
        pr.copy("dve", i1[:], i1f[:].rearrange("p s k -> p (s k)"), reads=["i1f"], writes=["i1"])
        pr.copy("dve", i2[:], i2f[:].rearrange("p s k -> p (s k)"), reads=["i2f"], writes=["i2"])
        w1 = [sbt(g, st, "ew1_%d" % i, [128, 8, FQ], BF16) for i in range(2)]
        w3 = [sbt(g, st, "ew3_%d" % i, [128, 8, FQ], BF16) for i in range(2)]
        w2 = [sbt(g, st, "ew2_%d" % i, [128, 7, 1024], BF16) for i in range(2)]
        rt = [sbt(g, st, "ert%d" % i, [128, 1024], BF16) for i in range(2)]
        fT = [sbt(g, st, "efT%d" % i, [128, NS, 8, 128], BF16) for i in range(2)]
        acc = sbt(g, st, "eacc", [128, NS, 1024], F32)
        stmp = [sbt(g, st, "estmp%d" % i, [128, 512], F32) for i in range(2)]
        actb = sbt(g, st, "eactb", [128, FQ], BF16)
        actT = sbt(g, st, "eactT", [128, 7, 128], BF16)
        ptr = pst(g, st, "eptr", [128, 1024], BF16)
        ptr2 = pst(g, st, "eptr2", [128, 1024], BF16)
        pu1 = [pst(g, st, "epu1%d" % i, [128, 512], F32) for i in range(2)]
        pu3 = [pst(g, st, "epu3%d" % i, [128, 512], F32) for i in range(2)]
        po = [pst(g, st, "epo%d" % i, [128, 512], F32) for i in range(2)]
        un_idx = [0]

        def load_unit(sb, qd):
            i = un_idx[0] % 2
            un_idx[0] += 1
            for k in range(8):
                c = sb * 32 + qd * 8 + k
                pr.idma(w1[i][:, k, :], None, W1v[:, :], i1[:, c:c + 1], reads=["i1"], writes=["ew1_%d" % i])
                pr.idma(w3[i][:, k, :], None, W3v[:, :], i1[:, c:c + 1], reads=["i1"], writes=["ew3_%d" % i])
            for k in range(7):
                c = sb * 28 + qd * 7 + k
                pr.idma(w2[i][:, k, :], None, W2v[:, :], i2[:, c:c + 1], reads=["i2"], writes=["ew2_%d" % i])
            return i

        def load_rows(sb):
            f = fT[sb % 2]
            fk = "efT%d" % (sb % 2)
            for ti in range(NS):
                r = rt[ti % 2]
                rk = "ert%d" % (ti % 2)
                r0 = sb * 1024 + ti * 128
                pr.dma(r[:], g.ROWS[r0:r0 + 128, :], writes=[rk])
                for k in range(8):
                    pr.tr(ptr2[:, k * 128:(k + 1) * 128], r[:, k * 128:(k + 1) * 128], identb[:], inc=(k == 7),
                          reads=[rk, "ident_b"], writes=["eptr2"])
                pr.copy("dve", f[:, ti, :, :], ptr2[:].rearrange("p (k n) -> p k n", k=8), reads=["eptr2"], writes=[fk])

        actb2 = [actb, sbt(g, st, "eactb1", [128, FQ], BF16)]
        actT2 = [actT, sbt(g, st, "eactT1", [128, 7, 128], BF16)]
        units = [(sb, qd) for sb in range(NSB) for qd in range(4)]
        items = [(sb, qd, ti) for (sb, qd) in units for ti in range(NS)]
        wof = {}
        load_rows(0)
        wof[units[0]] = load_unit(*units[0])
        wof[units[1]] = load_unit(*units[1])

        def stA(j):
            sb, qd, ti = items[j]
            if ti == 0 and qd == 1 and sb + 1 < NSB:
                load_rows(sb + 1)
            wi = wof[(sb, qd)]
            f = fT[sb % 2]
            fk = "efT%d" % (sb % 2)
            ab, abk = actb2[j % 2], "eactb%d" % (j % 2)
            for cg in range(2):
                c0 = cg * 512
                n = min(512, FQ - c0)
                for k in range(8):
                    pr.mm(pu1[cg][:, :n], f[:, ti, k, :], w1[wi][:, k, c0:c0 + n], start=(k == 0), stop=(k == 7),
                          inc=(k == 7), reads=[fk, "ew1_%d" % wi], writes=["epu1%d" % cg])
                for k in range(8):
                    pr.mm(pu3[cg][:, :n], f[:, ti, k, :], w3[wi][:, k, c0:c0 + n], start=(k == 0), stop=(k == 7),
                          inc=(k == 7), reads=[fk, "ew3_%d" % wi], writes=["epu3%d" % cg])
                pr.act(stmp[cg][:, :n], pu1[cg][:, :n], AF.Silu, reads=["epu1%d" % cg], writes=["estmp%d" % cg])
                pr.tt("dve", ab[:, c0:c0 + n], stmp[cg][:, :n], pu3[cg][:, :n], ALU.mult,
                      reads=["estmp%d" % cg, "epu3%d" % cg], writes=[abk])

        def stB1(j):
            ab, abk = actb2[j % 2], "eactb%d" % (j % 2)
            at, atk = actT2[j % 2], "eactT%d" % (j % 2)
            for k in range(7):
                pr.tr(ptr[:, k * 128:(k + 1) * 128], ab[:, k * 128:(k + 1) * 128], identb[:], inc=(k == 6),
                      reads=[abk, "ident_b"], writes=["eptr"])
            pr.copy("act", at[:], ptr[:, 0:896].rearrange("p (k n) -> p k n", k=7), reads=["eptr"], writes=[atk])

        def stB2(j):
            sb, qd, ti = items[j]
            wi = wof[(sb, qd)]
            at, atk = actT2[j % 2], "eactT%d" % (j % 2)
            for cg in range(2):
                for k in range(7):
                    pr.mm(po[cg][:], at[:, k, :], w2[wi][:, k, cg * 512:(cg + 1) * 512], start=(k == 0),
                          stop=(k == 6), inc=(k == 6), reads=[atk, "ew2_%d" % wi], writes=["epo%d" % cg])
                sl = slice(cg * 512, (cg + 1) * 512)
                ak = "eacc%d" % ti
                if qd == 0:
                    pr.copy("dve", acc[:, ti, sl], po[cg][:], reads=["epo%d" % cg], writes=[ak])
                else:
                    pr.tt("dve", acc[:, ti, sl], po[cg][:], acc[:, ti, sl], ALU.add,
                          reads=["epo%d" % cg, ak], writes=[ak])
            if qd == 3:
                r0 = sb * 1024 + ti * 128
                pr.dma(g.YROWS[r0:r0 + 128, :], acc[:, ti, :], reads=["eacc%d" % ti])

        stA(0)
        for j in range(len(items)):
            stB1(j)
            if j + 1 < len(items):
                stA(j + 1)
            stB2(j)
            sb_, qd_, ti_ = items[j]
            if ti_ == NS - 1:
                ui = sb_ * 4 + qd_
                if ui + 2 < len(units):
                    wof[units[ui + 2]] = load_unit(*units[ui + 2])


def phase_E2(g, l):
    pr, I, nc = g.pr, g.I, g.nc
    with ExitStack() as st:
        gate = sbt(g, st, "egate", [128, 1024], F32)
        pr.dma(gate[:], mod_row(g, l, 1, 5), writes=["egate"])
        GG = sbt(g, st, "e2gg", [128, 128], F32)
        DI = sbt(g, st, "e2di", [128, 128], I32)
        pr.dma(GG[:], g.GG[:, :], writes=["e2gg"])
        pr.dma(DI[:], g.DEST[:, :], writes=["e2di"])
        hb = [sbt(g, st, "e2h%d" % i, [128, 1024], F32) for i in range(3)]
        y1 = [sbt(g, st, "e2y1%d" % i, [128, 1024], F32) for i in range(3)]
        y2 = [sbt(g, st, "e2y2%d" % i, [128, 1024], F32) for i in range(3)]
        def fetch(jq):
            i = jq % 3
            t = jq + 2
            pr.dma(hb[i][:], g.H[t * 128:(t + 1) * 128, :], writes=["e2h%d" % i])
            pr.idma(y1[i][:, :], None, g.YROWS[:, :], DI[:, 2 * jq:2 * jq + 1], reads=["e2di"], writes=["e2y1%d" % i])
            pr.idma(y2[i][:, :], None, g.YROWS[:, :], DI[:, 2 * jq + 1:2 * jq + 2], reads=["e2di"], writes=["e2y2%d" % i])

        fetch(0)
        for jq in range(64):
            i = jq % 3
            t = jq + 2
            hk, k1, k2 = "e2h%d" % i, "e2y1%d" % i, "e2y2%d" % i
            if jq + 1 < 64:
                fetch(jq + 1)
            pr.ts("dve", y1[i][:], y1[i][:], GG[:, 2 * jq:2 * jq + 1], None, ALU.mult, reads=[k1, "e2gg"], writes=[k1])
            pr.stt("dve", y1[i][:], y2[i][:], GG[:, 2 * jq + 1:2 * jq + 2], y1[i][:], ALU.mult, ALU.add,
                   reads=[k1, k2, "e2gg"], writes=[k1])
            pr.tt("dve", y1[i][:], y1[i][:], gate[:], ALU.mult, reads=[k1, "egate"], writes=[k1])
            pr.tt("dve", hb[i][:], hb[i][:], y1[i][:], ALU.add, reads=[hk, k1], writes=[hk])
            pr.dma(g.out[jq * 128:(jq + 1) * 128, :], hb[i][:], reads=[hk])


PHASES = {"NN": lambda g, l: phase_NA(g, l, True), "R2": phase_R2, "ES": phase_ES, "E2": phase_E2, "R": phase_R, "E": phase_E, "C1": phase_C1, "C2": phase_C2, "NP": phase_NP, "NA": phase_NA, "M": phase_M, "A": phase_A, "SF": lambda g, l: phase_SSD(g, l, 0),
          "SB": lambda g, l: phase_SSD(g, l, 1)}


_CACHE = {}


def prep_inputs(inputs):
    consts = host_consts(np.asarray(inputs["rpb"], np.float32))
    maps = []
    c_ctx = np.asarray(inputs["c_ctx"], np.float32)
    for b in range(8):
        m = {}
        m["x"] = np.ascontiguousarray(inputs["x"][b], dtype=np.float32)
        m["ctx"] = np.ascontiguousarray(inputs["ctx"][b], dtype=np.float32)
        ct = np.empty((128, 16), np.float32)
        ct[:, 0:8] = np.asarray(inputs["c"][b], np.float32).reshape(8, 128).T
        ct[:, 8:16] = c_ctx.reshape(8, 128).T
        m["ct"] = ct
        for k in WEIGHT_SHAPES:
            m[k] = np.ascontiguousarray(inputs[k], dtype=np.float32)
        for k in CONST_SHAPES:
            m[k] = consts[k]
        maps.append(m)
    return maps


FULL_STAGES = [(0, "M"), (0, "A"), (0, "SF"), (0, "SB"), (0, "NN"), (0, "C1"), (0, "C2"),
               (1, "M"), (1, "A"), (1, "SF"), (1, "SB"), (1, "NN"), (1, "C1"), (1, "R2"), (1, "ES"), (1, "E2")]


def kernel(**inputs):
    maps = prep_inputs(inputs)
    nc = build(FULL_STAGES)
    res = run_bass_kernel_spmd(nc, maps, core_ids=list(range(8)))
    return np.stack([np.asarray(r["out"], np.float32) for r in res.results], 0)
```

```python
import numpy as np
from contextlib import ExitStack
import concourse.bass as bass
import concourse.mybir as mybir
from concourse.bass_utils import run_bass_kernel_spmd

F32 = mybir.dt.float32
BF16 = mybir.dt.bfloat16
AF = mybir.ActivationFunctionType
ALU = mybir.AluOpType
AX = mybir.AxisListType

D = 1024
SEQ = 8192
CTX = 256
T = SEQ + CTX
NT = T // 128
IN_DIM = 7712
C_Z, C_XBC, C_DT, C_Q, C_K, C_V = 2048, 3072, 4608, 4640, 5664, 6688
D_FF = 2816
D_FFE = 3584
NEXP = 8
EPS = 1e-6
NEGM = -30000.0
ENG = ("pe", "act", "dve", "pool", "sp")


class Prog:
    def __init__(self, nc, ndma=28):
        self.nc = nc
        self.ndma = ndma
        self.nsp = 16
        self.rr = {"sp": 0, "pool": 0}
        self.sems = {}
        self.ops = {e: [] for e in ENG}
        self.cnt = {}
        self.known = {e: {} for e in ENG}
        self.last_w = {}
        self.readers = {}
        self.dma_rr = 0
        self.dma_last = [None] * ndma
        self.nops = 0

    def setup_sems(self, st):
        for e in ENG:
            self.sems[e] = st.enter_context(self.nc.semaphore("s_" + e))
        for i in range(self.ndma):
            self.sems[("d", i)] = st.enter_context(self.nc.semaphore("s_d%d" % i))

    def _wait(self, eng, tok):
        k, v = tok
        if self.known[eng].get(k, 0) >= v:
            return
        self.known[eng][k] = v
        self.ops[eng].append(("wait", k, v))

    def _deps(self, reads, writes):
        toks = []
        for b in reads:
            t = self.last_w.get(b)
            if t:
                toks.append(t)
        for b in writes:
            t = self.last_w.get(b)
            if t:
                toks.append(t)
            toks.extend(self.readers.get(b, {}).items())
        return toks

    def _record(self, tok, reads, writes):
        for b in writes:
            self.last_w[b] = tok
            self.readers[b] = {}
        for b in reads:
            r = self.readers.setdefault(b, {})
            if r.get(tok[0], 0) < tok[1]:
                r[tok[0]] = tok[1]

    def op(self, eng, fn, reads=(), writes=(), inc=True):
        cur = self.cnt.get(eng, 0)
        for t in self._deps(reads, writes):
            if t[0] == eng and (t[1] < cur - 1 or t[1] > cur):
                continue
            self._wait(eng, t)
        self.ops[eng].append(("op", fn, inc))
        self.nops += 1
        if inc:
            cur += 1
            self.cnt[eng] = cur
            tok = (eng, cur)
        else:
            tok = (eng, cur + 1)
        self._record(tok, reads, writes)
        return tok

    def _slot(self, eng):
        if eng == "sp":
            sl = self.rr["sp"] % self.nsp
        else:
            sl = self.nsp + self.rr["pool"] % (self.ndma - self.nsp)
        self.rr["sp" if eng == "sp" else "pool"] += 1
        return sl

    def dma(self, out, in_, reads=(), writes=(), eng="sp"):
        slot = self._slot(eng)
        if self.dma_last[slot]:
            self._wait(eng, self.dma_last[slot])
        for t in self._deps(reads, writes):
            self._wait(eng, t)
        k = ("d", slot)
        v = self.cnt.get(k, 0) + 16
        self.cnt[k] = v
        self.ops[eng].append(("dma", out, in_, k))
        self.nops += 1
        tok = (k, v)
        self.dma_last[slot] = tok
        self._record(tok, reads, writes)
        return tok

    def idma(self, out, out_off, in_, in_off, reads=(), writes=()):
        eng = "pool"
        slot = self._slot(eng)
        if self.dma_last[slot]:
            self._wait(eng, self.dma_last[slot])
        for t in self._deps(reads, writes):
            self._wait(eng, t)
        k = ("d", slot)
        v = self.cnt.get(k, 0) + 16
        self.cnt[k] = v
        self.ops[eng].append(("idma", out, out_off, in_, in_off, k))
        self.nops += 1
        tok = (k, v)
        self.dma_last[slot] = tok
        self._record(tok, reads, writes)
        return tok

    def barrier(self):
        for e in ENG:
            for k, v in self.cnt.items():
                if k != e:
                    self._wait(e, (k, v))

    def check(self):
        if not hasattr(self, "simval"):
            self.simval = {}
        val = self.simval
        pos = {e: 0 for e in ENG}
        prog = True
        while prog:
            prog = False
            for e in ENG:
                lst = self.ops[e]
                while pos[e] < len(lst):
                    it = lst[pos[e]]
                    if it[0] == "wait":
                        if val.get(it[1], 0) < it[2]:
                            break
                    elif it[0] == "op":
                        if it[2]:
                            val[e] = val.get(e, 0) + 1
                    elif it[0] == "dma":
                        val[it[3]] = val.get(it[3], 0) + 16
                    else:
                        val[it[5]] = val.get(it[5], 0) + 16
                    pos[e] += 1
                    prog = True
        for e in ENG:
            if pos[e] < len(self.ops[e]):
                raise RuntimeError("deadlock: engine %s stuck at %d/%d on %r (vals %r)" % (
                    e, pos[e], len(self.ops[e]), self.ops[e][pos[e]][:3], val))

    def flush(self, final=False):
        self.barrier()
        self.check()
        nc = self.nc
        sems = self.sems

        def run(e, name):
            for it in self.ops[name]:
                if it[0] == "wait":
                    e.wait_ge(sems[it[1]], it[2])
                elif it[0] == "op":
                    ins = it[1](e)
                    if it[2]:
                        ins.then_inc(sems[name], 1)
                elif it[0] == "dma":
                    e.dma_start(out=it[1], in_=it[2]).then_inc(sems[it[3]], 16)
                else:
                    oo = bass.IndirectOffsetOnAxis(ap=it[2], axis=0) if it[2] is not None else None
                    io = bass.IndirectOffsetOnAxis(ap=it[4], axis=0) if it[4] is not None else None
                    e.indirect_dma_start(out=it[1], out_offset=oo, in_=it[3], in_offset=io).then_inc(sems[it[5]], 16)
            self.ops[name] = []

        with nc.Block() as block:
            @block.tensor
            def _(e):
                run(e, "pe")

            @block.scalar
            def _(e):
                run(e, "act")

            @block.vector
            def _(e):
                run(e, "dve")

            @block.gpsimd
            def _(e):
                run(e, "pool")

            @block.sync
            def _(e):
                run(e, "sp")

    def mm(self, out, lhsT, rhs, start=True, stop=True, inc=True, reads=(), writes=()):
        return self.op("pe", lambda e: e.matmul(out, lhsT, rhs, start=start, stop=stop),
                       reads, writes, inc)

    def tr(self, out, in_, ident, inc=True, reads=(), writes=()):
        return self.op("pe", lambda e: e.transpose(out, in_, ident), reads, writes, inc)

    def act(self, out, in_, func, reads=(), writes=(), eng="act", **kw):
        return self.op("act", lambda e: e.activation(out, in_, func, **kw), reads, writes)

    def tt(self, eng, out, in0, in1, op, reads=(), writes=()):
        return self.op(eng, lambda e: e.tensor_tensor(out, in0, in1, op), reads, writes)

    def ts(self, eng, out, in0, s1, s2, op0, op1=None, reads=(), writes=()):
        if op1 is None:
            return self.op(eng, lambda e: e.tensor_scalar(out, in0, s1, None, op0), reads, writes)
        return self.op(eng, lambda e: e.tensor_scalar(out, in0, s1, s2, op0, op1), reads, writes)

    def stt(self, eng, out, in0, scalar, in1, op0, op1, reads=(), writes=()):
        return self.op(eng, lambda e: e.scalar_tensor_tensor(out, in0, scalar, in1, op0, op1),
                       reads, writes)

    def copy(self, eng, out, in_, reads=(), writes=()):
        if eng == "act":
            return self.op("act", lambda e: e.copy(out, in_), reads, writes)
        return self.op(eng, lambda e: e.tensor_copy(out, in_), reads, writes)

    def memset(self, eng, ap, val, writes=()):
        return self.op(eng, lambda e: e.memset(ap, val), (), writes)


class Ctx:
    pass


def host_consts(rpb):
    c = {}
    s = np.arange(128)[:, None]
    q = np.arange(128)[None, :]
    c["ident"] = np.eye(128, dtype=np.float32)
    c["trif"] = (s <= q).astype(np.float32)
    c["trib"] = (s >= q).astype(np.float32)
    c["ones"] = np.ones((128, 128), np.float32)
    mf = np.where(q < s, NEGM, 0.0).astype(np.float32)
    mb = np.where(q > s, NEGM, 0.0).astype(np.float32)
    c["maskf"] = np.tile(mf, (1, 4))
    c["maskb"] = np.tile(mb, (1, 4))
    sel = np.zeros((16, 16 * 128), np.float32)
    for h in range(16):
        sel[h, h * 128:(h + 1) * 128] = 1.0
    c["sel"] = sel
    c["stri"] = (s < q).astype(np.float32)
    p = np.arange(128)[:, None]
    c["thr"] = np.tile((np.arange(16) * 1024.0)[None, :], (128, 8)).astype(np.float32)
    c["thr2"] = np.repeat((np.arange(24) * 1024.0), 8)[None, :].repeat(128, 0).astype(np.float32)
    kq = np.zeros((128, 4, 8), np.float32)
    for qd in range(4):
        for k in range(8):
            kq[:, qd, k] = (k * 128 + np.arange(128)) * 4 + qd
    c["kpq"] = kq.reshape(128, 32)
    k2 = np.zeros((128, 4, 7), np.float32)
    for qd in range(4):
        for k in range(7):
            k2[:, qd, k] = qd * 896 + k * 128 + np.arange(128)
    c["kp2"] = k2.reshape(128, 28)
    t = np.arange(SEQ)
    pr = (t // 64).astype(np.float32)
    pc = (t % 64).astype(np.float32)
    inv = (np.float32(10000.0) ** (-np.arange(16, dtype=np.float32) / np.float32(16))).astype(np.float32)
    ar = (pr[:, None] * inv[None, :]).astype(np.float32)
    ac = (pc[:, None] * inv[None, :]).astype(np.float32)
    c["ropec"] = np.concatenate([np.cos(ar), np.cos(ar), np.cos(ac), np.cos(ac)], 1).astype(np.float32)
    c["ropes"] = np.concatenate([-np.sin(ar), np.sin(ar), -np.sin(ac), np.sin(ac)], 1).astype(np.float32)
    L = rpb.shape[0]
    tab = np.empty((L, 5, 16, 128, 5, 128), np.float32)
    for ci, jq in enumerate([2, 0, 1, 62, 63]):
        cb = min(max(jq - 2, 0), 59)
        kc = np.arange(5)[:, None, None]
        kk = np.arange(128)[None, :, None]
        qq = np.arange(128)[None, None, :]
        rk = 2 * (cb + kc) + kk // 64
        ck = kk % 64 + 0 * kc
        r = 2 * jq + qq // 64
        cq = qq % 64
        r0 = np.clip(r - 4, 0, 120)
        cs = np.clip(cq - 8, 0, 48)
        valid = (rk >= r0) & (rk < r0 + 8) & (ck >= cs) & (ck < cs + 16)
        dr = np.clip(rk - r + 7, 0, 14)
        dc = np.clip(ck - cq, -15, 15) + 15
        dr, dc, valid = np.broadcast_arrays(dr, dc, valid)
        vals = rpb[:, :, dr, dc]
        vals = np.where(valid[None, None], vals, np.float32(NEGM))
        tab[:, ci] = vals.transpose(0, 1, 3, 2, 4)
    c["nab"] = tab.reshape(L, 5, 16, 128, 640)
    return c


CONST_SHAPES = {
    "ident": [128, 128], "trif": [128, 128], "trib": [128, 128], "ones": [128, 128],
    "maskf": [128, 512], "maskb": [128, 512], "sel": [16, 2048],
    "stri": [128, 128], "thr": [128, 128], "thr2": [128, 192], "kpq": [128, 32], "kp2": [128, 28],
    "ropec": [SEQ, 64], "ropes": [SEQ, 64], "nab": [2, 5, 16, 128, 640],
}

WEIGHT_SHAPES = {
    "w_mod": [2, 1024, 6144], "b_mod": [2, 6144], "norm1_w": [2, 1024], "norm2_w": [2, 1024],
    "w_in": [2, 1024, 7712], "conv_w": [2, 5, 1536], "conv_b": [2, 1536], "dt_bias": [2, 2, 16],
    "a_log": [2, 2, 16], "d_skip": [2, 2, 16], "ssd_norm_w": [2, 1024], "q_norm_w": [2, 64],
    "k_norm_w": [2, 64], "w_br_ssd": [2, 1024, 1024], "w_br_na": [2, 1024, 1024],
    "w_out": [2, 1024, 1024], "w_ff1": [1, 1024, 2816], "w_ff3": [1, 1024, 2816],
    "w_ff2": [1, 2816, 1024], "w_router": [1, 1024, 8], "w_e1": [1, 8, 1024, 3584],
    "w_e3": [1, 8, 1024, 3584], "w_e2": [1, 8, 3584, 1024],
}


def rowb(ap1d_row, n, parts=128):
    return ap1d_row.broadcast_to([parts, n])


def build(stages, dbg=()):
    nc = bass.Bass("TRN2", target_bir_lowering=False)
    g = Ctx()
    g.nc = nc
    g.dbg = dbg
    I = {}
    I["x"] = nc.dram_tensor("x", [SEQ, D], F32, kind="ExternalInput").ap()
    I["ctx"] = nc.dram_tensor("ctx", [CTX, D], F32, kind="ExternalInput").ap()
    I["ct"] = nc.dram_tensor("ct", [128, 16], F32, kind="ExternalInput").ap()
    for k, s in WEIGHT_SHAPES.items():
        I[k] = nc.dram_tensor(k, s, F32, kind="ExternalInput").ap()
    for k, s in CONST_SHAPES.items():
        I[k] = nc.dram_tensor(k, s, F32, kind="ExternalInput").ap()
    g.I = I
    g.out = nc.dram_tensor("out", [SEQ, D], F32, kind="ExternalOutput").ap()

    def scratch(name, shape, dt):
        kind = "ExternalOutput" if name in dbg else "Internal"
        return nc.dram_tensor(name, shape, dt, kind=kind).ap()

    g.MOD = scratch("MOD", [2, 2, 6144], F32)
    g.P = scratch("P", [T, IN_DIM], F32)
    g.YF = scratch("YF", [T, D], F32)
    g.XS = scratch("XS", [T, 1536], F32)
    g.YS = scratch("YS", [T, D], F32)
    g.YN = scratch("YN", [T, D], F32)
    g.H = scratch("H", [T, D], F32)
    g.QT = scratch("QT", [NT, 128, 8, 128], BF16)
    g.KT = scratch("KT", [NT, 128, 8, 128], BF16)
    g.FT = scratch("FT", [64, 128, 8, 128], BF16)
    g.GT = scratch("GT", [64, 128, 8], F32)
    I32 = mybir.dt.int32
    g.F = scratch("F", [SEQ, D], BF16)
    g.ROWS = scratch("ROWS", [24576, D], BF16)
    g.YROWS = scratch("YROWS", [24576, D], F32)
    g.GG = scratch("GG", [128, 128], F32)
    g.DEST = scratch("DEST", [128, 128], I32)
    g.BED = scratch("BED", [128, 24], F32)

    with ExitStack() as st:
        pr = Prog(nc)
        pr.setup_sems(st)
        g.pr = pr
        for (l, ph) in stages:
            PHASES[ph](g, l)
            pr.flush()
    return nc


_UID = [0]


def sbt(g, st, name, shape, dt):
    _UID[0] += 1
    return st.enter_context(g.nc.sbuf_tensor("sb%d_%s" % (_UID[0], name), shape, dt))


def pst(g, st, name, shape, dt):
    _UID[0] += 1
    return st.enter_context(g.nc.psum_tensor("ps%d_%s" % (_UID[0], name), shape, dt))


def load_consts(g, st, names):
    pr, I = g.pr, g.I
    out = {}
    for n in names:
        base = n[:-2] if n.endswith("_b") else n
        shp = CONST_SHAPES[base]
        if n.endswith("_b"):
            t = sbt(g, st, "c_" + n, shp, BF16)
            pr.dma(t[:], I[base][:, :], writes=[n], eng="pool")
        else:
            t = sbt(g, st, "c_" + n, shp, F32)
            pr.dma(t[:], I[n][:, :], writes=[n])
        out[n] = t
    return out


def src_rows(g, l, t):
    if l == 0:
        if t < 2:
            return g.I["ctx"][t * 128:(t + 1) * 128, :]
        return g.I["x"][(t - 2) * 128:(t - 1) * 128, :]
    return g.H[t * 128:(t + 1) * 128, :]


def phase_M(g, l):
    pr, I, nc = g.pr, g.I, g.nc
    with ExitStack() as st:
        ct = sbt(g, st, "ct", [128, 16], F32)
        cs = sbt(g, st, "cs", [128, 16], F32)
        cb = sbt(g, st, "cb", [128, 16, 128], F32)
        wst = [sbt(g, st, "wst%d" % i, [128, 8, 512], F32) for i in range(2)]
        bst = [sbt(g, st, "bst%d" % i, [128, 512], F32) for i in range(2)]
        res = [sbt(g, st, "res%d" % i, [128, 512], F32) for i in range(2)]
        pp = [pst(g, st, "pp%d" % i, [128, 512], F32) for i in range(4)]
        pr.dma(ct[:], I["ct"][:, :], writes=["ct"])
        pr.act(cs[:], ct[:], AF.Silu, reads=["ct"], writes=["cs"])
        pr.copy("dve", cb[:], cs[:].unsqueeze(2).broadcast_to([128, 16, 128]), reads=["cs"], writes=["cb"])
        i = 0
        for cg in range(12):
            c0 = cg * 512
            w = wst[cg % 2]
            pr.dma(w[:], I["w_mod"][l, :, c0:c0 + 512].rearrange("(k p) n -> p k n", p=128),
                   writes=["wst%d" % (cg % 2)])
            pr.dma(bst[cg % 2][:], rowb(I["b_mod"][l:l + 1, c0:c0 + 512], 512), writes=["bst%d" % (cg % 2)])
            for v in range(2):
                p = pp[i % 4]
                pk = "pp%d" % (i % 4)
                for k in range(8):
                    col = (8 + k) if v == 0 else k
                    pr.mm(p[:], cb[:, col, :], w[:, k, :], start=(k == 0), stop=(k == 7), inc=(k == 7),
                          reads=["cb", "wst%d" % (cg % 2)], writes=[pk])
                r = res[i % 2]
                rk = "res%d" % (i % 2)
                pr.tt("dve", r[:], p[:], bst[cg % 2][:], ALU.add, reads=[pk, "bst%d" % (cg % 2)], writes=[rk])
                pr.dma(g.MOD[l, v:v + 1, c0:c0 + 512], r[0:1, :], reads=[rk], eng="pool")
                i += 1


def load_mod(g, st, l, which, name):
    pr = g.pr
    out = []
    for v in range(2):
        t = sbt(g, st, "%s%d" % (name, v), [128, 1024], F32)
        pr.dma(t[:], rowb(g.MOD[l, v:v + 1, which * 1024:(which + 1) * 1024], 1024), writes=["%s%d" % (name, v)])
        out.append(t)
    return out


def make_scale(g, st, l, which, wname, name):
    pr, I = g.pr, g.I
    sc = load_mod(g, st, l, which, name)
    nw = sbt(g, st, name + "nw", [128, 1024], F32)
    pr.dma(nw[:], rowb(I[wname][l:l + 1, :], 1024), writes=[name + "nw"])
    for v in range(2):
        k = "%s%d" % (name, v)
        pr.stt("dve", sc[v][:], sc[v][:], 1.0, nw[:], ALU.add, ALU.mult, reads=[k, name + "nw"], writes=[k])
    return sc


def rms_rstd(g, sq_junk, x_ap, ssq, rs, n, keys_r, key_ssq, key_rs, junk_key):
    pr = g.pr
    pr.act(sq_junk, x_ap, AF.Square, reads=keys_r, writes=[junk_key, key_ssq], accum_out=ssq)
    pr.ts("dve", rs, ssq, 1.0 / n, EPS, ALU.mult, ALU.add, reads=[key_ssq], writes=[key_rs])
    pr.act(rs, rs, AF.Sqrt, reads=[key_rs], writes=[key_rs])
    pr.op("dve", lambda e: e.reciprocal(rs, rs), reads=[key_rs], writes=[key_rs])


def phase_A(g, l):
    pr, I, nc = g.pr, g.I, g.nc
    with ExitStack() as st:
        W = sbt(g, st, "winb", [128, 8, IN_DIM], BF16)
        for k in range(8):
            for pc in range(4):
                c0 = pc * 2048
                c1 = min(IN_DIM, c0 + 2048)
                pr.dma(W[:, k, c0:c1], I["w_in"][l, k * 128:(k + 1) * 128, c0:c1], writes=["W"], eng="pool")
        cst = load_consts(g, st, ["ident_b"])
        G1 = make_scale(g, st, l, 1, "norm1_w", "G1")
        SH1 = load_mod(g, st, l, 0, "SH1")
        hb = [sbt(g, st, "hb%d" % i, [128, 1024], F32) for i in range(2)]
        junk = sbt(g, st, "junk", [128, 1024], F32)
        tmp = sbt(g, st, "tmpa", [128, 1024], F32)
        abf = sbt(g, st, "abf", [128, 1024], BF16)
        aT = [sbt(g, st, "aT%d" % i, [128, 8, 128], BF16) for i in range(2)]
        ssq = sbt(g, st, "ssq", [128, 2], F32)
        rs = sbt(g, st, "rs", [128, 2], F32)
        ostg = [sbt(g, st, "ostg%d" % i, [128, 2048], F32) for i in range(2)]
        ptr = pst(g, st, "ptr", [128, 1024], BF16)
        pp = [pst(g, st, "ppa%d" % i, [128, 512], F32) for i in range(5)]
        abf2 = [abf, sbt(g, st, "abf1", [128, 1024], BF16)]
        pr.dma(hb[0][:], src_rows(g, l, 0), writes=["hb0"])
        pr.dma(hb[1][:], src_rows(g, l, 1), writes=["hb1"])
        oi = 0
        ei = 0

        def normA(t):
            v = 0 if t < 2 else 1
            h = hb[t % 2]
            hk = "hb%d" % (t % 2)
            ab, abk = abf2[t % 2], "abf%d" % (t % 2)
            rms_rstd(g, junk[:], h[:], ssq[:, 0:1], rs[:, 0:1], 1024, [hk], "ssq", "rs", "junk")
            pr.stt("dve", tmp[:], h[:], rs[:, 0:1], G1[v][:], ALU.mult, ALU.mult,
                   reads=[hk, "rs", "G1%d" % v], writes=["tmpa"])
            pr.tt("dve", ab[:], tmp[:], SH1[v][:], ALU.add, reads=["tmpa", "SH1%d" % v], writes=[abk])
            if t + 2 < NT:
                pr.dma(h[:], src_rows(g, l, t + 2), reads=[], writes=[hk])

        def transA(t):
            ab, abk = abf2[t % 2], "abf%d" % (t % 2)
            for k in range(8):
                pr.tr(ptr[:, k * 128:(k + 1) * 128], ab[:, k * 128:(k + 1) * 128], cst["ident_b"][:],
                      inc=(k == 7), reads=[abk, "ident_b"], writes=["ptr"])
            a = aT[t % 2]
            ak = "aT%d" % (t % 2)
            pr.copy("act", a[:], ptr[:].rearrange("p (k n) -> p k n", k=8), reads=["ptr"], writes=[ak])

        normA(0)
        transA(0)
        for t in range(NT):
            a = aT[t % 2]
            ak = "aT%d" % (t % 2)
            if t + 1 < NT:
                normA(t + 1)
            for cg in range(16):
                c0 = cg * 512
                n = min(512, IN_DIM - c0)
                p = pp[cg % 5]
                pk = "ppa%d" % (cg % 5)
                for k in range(8):
                    pr.mm(p[:, :n], a[:, k, :], W[:, k, c0:c0 + n], start=(k == 0), stop=(k == 7),
                          inc=(k == 7), reads=[ak, "W"], writes=[pk])
                o = ostg[oi % 2]
                ok = "ostg%d" % (oi % 2)
                oc = (cg % 4) * 512
                pr.copy("act" if ei % 2 == 0 else "dve", o[:, oc:oc + n], p[:, :n], reads=[pk], writes=[ok])
                ei += 1
                if cg % 4 == 3:
                    b0 = (cg // 4) * 2048
                    b1 = min(IN_DIM, b0 + 2048)
                    pr.dma(g.P[t * 128:(t + 1) * 128, b0:b1], o[:, :b1 - b0], reads=[ok], eng="pool")
                    oi += 1
                if cg == 11 and t + 1 < NT:
                    transA(t + 1)


def phase_SSD(g, l, d):
    pr, I, nc = g.pr, g.I, g.nc
    with ExitStack() as st:
        CK = ["ident_b", "trib" if d else "trif", "ones", "sel", "maskb_b" if d else "maskf_b"]
        cst = load_consts(g, st, CK)
        identb, tri, ones, sel, maskb = [cst[k] for k in CK]
        if d == 0:
            convw = sbt(g, st, "convw", [128, 5, 1536], F32)
            pr.dma(convw[:].rearrange("p k c -> p (k c)"),
                   rowb(I["conv_w"][l:l + 1].rearrange("a k c -> a (k c)"), 7680), writes=["convw"])
            convb = sbt(g, st, "convb", [128, 1536], F32)
            pr.dma(convb[:], rowb(I["conv_b"][l:l + 1, :], 1536), writes=["convb"])
            xb = [sbt(g, st, "xb%d" % i, [128, 1536], F32) for i in range(3)]
            ctmp = [sbt(g, st, "ctmp%d" % i, [128, 1536], F32) for i in range(2)]
            acc = sbt(g, st, "cacc", [128, 1536], F32)
        dtb = sbt(g, st, "dtb", [128, 32], F32)
        pr.dma(dtb[:], rowb(I["dt_bias"][l:l + 1].rearrange("a x h -> a (x h)"), 32), writes=["dtb"])
        aneg = sbt(g, st, "aneg", [128, 32], F32)
        pr.dma(aneg[:], rowb(I["a_log"][l:l + 1].rearrange("a x h -> a (x h)"), 32), writes=["aneg"])
        pr.act(aneg[:], aneg[:], AF.Exp, reads=["aneg"], writes=["aneg"])
        pr.ts("dve", aneg[:], aneg[:], -1.0, None, ALU.mult, reads=["aneg"], writes=["aneg"])
        if d:
            dsk = sbt(g, st, "dsk", [128, 32], F32)
            pr.dma(dsk[:], rowb(I["d_skip"][l:l + 1].rearrange("a x h -> a (x h)"), 32), writes=["dsk"])
            dsum = sbt(g, st, "dsum", [128, 16], F32)
            pr.tt("dve", dsum[:], dsk[:, 0:16], dsk[:, 16:32], ALU.add, reads=["dsk"], writes=["dsum"])
            ssdw = sbt(g, st, "ssdw", [128, 1024], F32)
            pr.dma(ssdw[:], rowb(I["ssd_norm_w"][l:l + 1, :], 1024), writes=["ssdw"])
            yfb = [sbt(g, st, "yfb%d" % i, [128, 1024], F32) for i in range(2)]
            zb = [sbt(g, st, "zb%d" % i, [128, 1024], F32) for i in range(2)]
            ub = sbt(g, st, "ub", [128, 1024], F32)
            ms = sbt(g, st, "ms", [128, 4], F32)
        SR = [sbt(g, st, "S%d" % i, [128, 1024], F32) for i in range(2)]
        SbfR = [sbt(g, st, "Sbf%d" % i, [128, 1024], BF16) for i in range(2)]
        pr.memset("dve", SR[0][:], 0.0, writes=["S0"])
        pr.memset("dve", SbfR[0][:], 0.0, writes=["Sbf0"])
        def ring(name, n, shape, dt_):
            return [sbt(g, st, "%s%d" % (name, i), shape, dt_) for i in range(n)]

        xbcR = ring("xbc", 4, [128, 1536], F32)
        bcbfR = ring("bcbf", 3, [128, 512], BF16)
        bcTR = ring("bcT", 3, [128, 4, 128], BF16)
        cbtR = ring("cbt", 2, [128, 2, 128], BF16)
        dtrR = ring("dtr", 2, [128, 32], F32)
        dtR = ring("dt", 3, [128, 32], F32)
        smR = ring("sm", 3, [128, 128], F32)
        acsTR = ring("acsT", 2, [16, 128], F32)
        nacsTR = ring("nacsT", 2, [16, 128], F32)
        BmR = ring("Bm", 2, [16, 2048], F32)
        xdtR = ring("xdt", 3, [128, 1024], BF16)
        xwR = ring("xw", 3, [128, 1024], BF16)
        mtR = ring("mt", 2, [128, 2048], BF16)
        ebf = ring("ebf", 2, [128, 512], BF16)
        ytmp = sbt(g, st, "ytmp", [128, 1024], F32)
        ysb = ring("ysb", 2, [128, 1024], F32)
        pmA = pst(g, st, "pmA", [128, 512], F32)
        ptb = pst(g, st, "ptb", [128, 1024], BF16)
        pseg = [pst(g, st, "pseg%d" % i, [128, 512], F32) for i in range(2)]
        pyd1 = pst(g, st, "pyd", [128, 512], F32)
        pyo1 = pst(g, st, "pyo", [128, 512], F32)
        psd = [pst(g, st, "psd%d" % i, [128, 512], F32) for i in range(2)]
        order = list(range(NT)) if d == 0 else [1, 0] + list(range(NT - 1, 1, -1))
        xi = [0]
        pend = []

        def R(ringl, name, c):
            i = c % len(ringl)
            return ringl[i], "%s%d" % (name, i)

        def smv(sm):
            return (sm[:, 0:16], sm[:, 16:32], sm[:, 32:48], sm[:, 64:80], sm[:, 80:96], sm[:, 96:112], sm[:, 112:128])

        def s0(c):
            t = order[c]
            t0 = t * 128
            lo, hi = (0, 256) if t < 2 else (256, T)
            xbc, xk_ = R(xbcR, "xbc", c)
            dtr, dtrk = R(dtrR, "dtr", c)
            dt, dtk = R(dtR, "dt", c)
            pr.dma(dtr[:], g.P[t0:t0 + 128, C_DT:C_DT + 32], writes=[dtrk])
            if d:
                pr.dma(xbc[:], g.XS[t0:t0 + 128, :], writes=[xk_])
            else:
                for k in range(5):
                    x = xb[xi[0] % 3]
                    xk = "xb%d" % (xi[0] % 3)
                    xi[0] += 1
                    r0 = t0 + k - 2
                    i_lo = max(0, lo - r0)
                    i_hi = min(128, hi - r0)
                    if i_lo > 0 or i_hi < 128:
                        pr.memset("dve", x[:], 0.0, writes=[xk])
                    pr.dma(x[i_lo:i_hi, :], g.P[r0 + i_lo:r0 + i_hi, C_XBC:C_XBC + 1536], writes=[xk],
                           eng=("pool" if k in (1, 3) else "sp"))
                    ct_ = ctmp[k % 2]
                    ck = "ctmp%d" % (k % 2)
                    pr.tt("pool" if k in (0, 2) else "dve", ct_[:], x[:], convw[:, k, :], ALU.mult,
                          reads=[xk, "convw"], writes=[ck])
                    pr.tt("dve", acc[:], ct_[:], convb[:] if k == 0 else acc[:], ALU.add,
                          reads=[ck, "convb", "cacc"], writes=["cacc"])
                pr.act(xbc[:], acc[:], AF.Silu, reads=["cacc"], writes=[xk_])
                pr.dma(g.XS[t0:t0 + 128, :], xbc[:], reads=[xk_], eng="pool")
            pr.tt("dve", dt[:], dtr[:], dtb[:], ALU.add, reads=[dtrk, "dtb"], writes=[dtk])
            pr.act(dt[:], dt[:], AF.Exp, reads=[dtk], writes=[dtk])
            pr.act(dt[:], dt[:], AF.Ln, reads=[dtk], writes=[dtk], bias=1.0)

        def s1a(c):
            xbc, xk_ = R(xbcR, "xbc", c)
            dt, dtk = R(dtR, "dt", c)
            sm, smk = R(smR, "sm", c)
            bcbf, bcbfk = R(bcbfR, "bcbf", c)
            bcT, bcTk = R(bcTR, "bcT", c)
            acsT, acsTk = R(acsTR, "acsT", c)
            nacsT, nacsTk = R(nacsTR, "nacsT", c)
            xdt, xdtk = R(xdtR, "xdt", c)
            la, acs, tot, eacs, etot, wdec, dtw = smv(sm)
            dtd = dt[:, 16 * d:16 * d + 16]
            pr.copy("dve", bcbf[:], xbc[:, 1024:1536], reads=[xk_], writes=[bcbfk])
            pr.tt("dve", la, dtd, aneg[:, 16 * d:16 * d + 16], ALU.mult, reads=[dtk, "aneg"], writes=[smk + "la"])
            for j in range(4):
                pr.tr(ptb[:, j * 128:(j + 1) * 128], bcbf[:, j * 128:(j + 1) * 128], identb[:], inc=(j == 3),
                      reads=[bcbfk, "ident_b"], writes=["ptb"])
            tk = CK[1]
            pr.mm(pmA[:, 256:272], tri[:], la, reads=[tk, smk + "la"], writes=["pmA"], inc=False)
            pr.mm(pmA[:, 272:288], ones[:], la, reads=["ones", smk + "la"], writes=["pmA"], inc=False)
            pr.mm(pmA[0:16, 320:448], la, tri[:], reads=[tk, smk + "la"], writes=["pmA"])
            xs3 = xbc[:, 0:1024].rearrange("p (h e) -> p h e", e=64)
            pr.tt("dve", xdt[:].rearrange("p (h e) -> p h e", e=64), xs3,
                  dtd.unsqueeze(2).broadcast_to([128, 16, 64]), ALU.mult, reads=[xk_, dtk], writes=[xdtk])
            pr.copy("act", bcT[:], ptb[:, 0:512].rearrange("p (k n) -> p k n", k=4), reads=["ptb"], writes=[bcTk])
            pr.copy("dve", sm[:, 16:48], pmA[:, 256:288], reads=["pmA"], writes=[smk + "at"])
            pr.copy("dve", acsT[:], pmA[0:16, 320:448], reads=["pmA"], writes=[acsTk])
            pr.ts("dve", nacsT[:], acsT[:], -1.0, None, ALU.mult, reads=[acsTk], writes=[nacsTk])
            Bm, Bmk = R(BmR, "Bm", c)
            pr.tt("pool", Bm[:].rearrange("k (h q) -> k h q", h=16), sel[:].rearrange("k (h q) -> k h q", h=16),
                  acsT[:].unsqueeze(1).broadcast_to([16, 16, 128]), ALU.mult, reads=["sel", acsTk], writes=[Bmk])

        def s1b(c):
            t = order[c]
            t0 = t * 128
            xbc, xk_ = R(xbcR, "xbc", c)
            dt, dtk = R(dtR, "dt", c)
            sm, smk = R(smR, "sm", c)
            bcT, bcTk = R(bcTR, "bcT", c)
            cbt, cbtk = R(cbtR, "cbt", c)
            acsT, acsTk = R(acsTR, "acsT", c)
            nacsT, nacsTk = R(nacsTR, "nacsT", c)
            Bm, Bmk = R(BmR, "Bm", c)
            xw, xwk = R(xwR, "xw", c)
            mt, mtk = R(mtR, "mt", c)
            la, acs, tot, eacs, etot, wdec, dtw = smv(sm)
            dtd = dt[:, 16 * d:16 * d + 16]
            if d:
                b = c % 2
                pr.dma(yfb[b][:], g.YF[t0:t0 + 128, :], writes=["yfb%d" % b])
                pr.dma(zb[b][:], g.P[t0:t0 + 128, C_Z:C_Z + 1024], writes=["zb%d" % b])
            for gi in range(2):
                pr.mm(pyd1[:, gi * 128:(gi + 1) * 128], bcT[:, gi, :], bcT[:, 2 + gi, :], inc=(gi == 1),
                      reads=[bcTk], writes=["pyd"])
            def seg_mm(hb):
                ps_ = pseg[hb % 2]
                pk = "pseg%d" % (hb % 2)
                pr.mm(ps_[:], identb[:], maskb[:], start=True, stop=False, inc=False,
                      reads=["ident_b", CK[4]], writes=[pk])
                pr.mm(ps_[:], ones[0:16, :], Bm[:, hb * 512:(hb + 1) * 512], start=False, stop=False, inc=False,
                      reads=["ones", Bmk], writes=[pk])
                pr.mm(ps_[:], nacsT[:], sel[:, hb * 512:(hb + 1) * 512], start=False, stop=True, inc=True,
                      reads=["sel", nacsTk], writes=[pk])

            def seg_ev(hb):
                ps_ = pseg[hb % 2]
                pk = "pseg%d" % (hb % 2)
                gi = hb // 2
                e_ = ebf[hb % 2]
                ek = "ebf%d" % (hb % 2)
                pr.act(e_[:], ps_[:], AF.Exp, reads=[pk], writes=[ek])
                pr.tt("dve", mt[:, hb * 512:(hb + 1) * 512].rearrange("p (j q) -> p j q", j=4),
                      e_[:].rearrange("p (j q) -> p j q", j=4),
                      cbt[:, gi, :].unsqueeze(1).broadcast_to([128, 4, 128]), ALU.mult,
                      reads=[ek, cbtk], writes=[mtk])

            seg_mm(0)
            pr.copy("dve", cbt[:], pyd1[:, 0:256].rearrange("p (k n) -> p k n", k=2), reads=["pyd"], writes=[cbtk])
            pr.tt("dve", sm[:, 48:64], tot, acs, ALU.subtract, reads=[smk + "at"], writes=[smk + "wd"])
            seg_mm(1)
            seg_ev(0)
            seg_mm(2)
            seg_ev(1)
            seg_mm(3)
            seg_ev(2)
            seg_ev(3)
            pr.act(sm[:, 64:112], sm[:, 16:64], AF.Exp, reads=[smk + "at", smk + "wd"], writes=[smk + "ex"])
            xs3 = xbc[:, 0:1024].rearrange("p (h e) -> p h e", e=64)
            pr.tt("dve", dtw, dtd, wdec, ALU.mult, reads=[dtk, smk + "ex"], writes=[smk + "dtw"])
            pr.tt("pool", xw[:].rearrange("p (h e) -> p h e", e=64), xs3,
                  dtw.unsqueeze(2).broadcast_to([128, 16, 64]), ALU.mult, reads=[xk_, smk + "dtw"], writes=[xwk])

        def s2(c):
            t = order[c]
            b = c % 2
            B = str(b)
            t0 = t * 128
            xbc, xk_ = R(xbcR, "xbc", c)
            sm, smk = R(smR, "sm", c)
            bcbf, bcbfk = R(bcbfR, "bcbf", c)
            bcT, bcTk = R(bcTR, "bcT", c)
            xdt, xdtk = R(xdtR, "xdt", c)
            xw, xwk = R(xwR, "xw", c)
            mt, mtk = R(mtR, "mt", c)
            la, acs, tot, eacs, etot, wdec, dtw = smv(sm)
            xs3 = xbc[:, 0:1024].rearrange("p (h e) -> p h e", e=64)
            Sc, Sck = SR[c % 2], "S%d" % (c % 2)
            Sn, Snk = SR[(c + 1) % 2], "S%d" % ((c + 1) % 2)
            Sbf, Sbfk = SbfR[c % 2], "Sbf%d" % (c % 2)
            Sbn, Sbnk = SbfR[(c + 1) % 2], "Sbf%d" % ((c + 1) % 2)
            for gi in range(2):
                pr.mm(psd[gi][:], bcbf[:, gi * 128:(gi + 1) * 128], xw[:, gi * 512:(gi + 1) * 512],
                      reads=[bcbfk, xwk], writes=["psd%d" % gi])
            pr.tt("dve", Sn[:].rearrange("p (h e) -> p h e", e=64), Sc[:].rearrange("p (h e) -> p h e", e=64),
                  etot.unsqueeze(2).broadcast_to([128, 16, 64]), ALU.mult, reads=[Sck, smk + "ex"], writes=[Snk])
            for gi in range(2):
                sl = slice(gi * 512, (gi + 1) * 512)
                pr.tt("dve", Sn[:, sl], psd[gi][:], Sn[:, sl], ALU.add, reads=["psd%d" % gi, Snk], writes=[Snk])
            pr.copy("act", Sbn[:], Sn[:], reads=[Snk], writes=[Sbnk])
            y = ysb[b]
            yk = "ysb" + B
            for gi in range(2):
                pr.mm(pyo1[:], bcT[:, 2 + gi, :], Sbf[:, gi * 512:(gi + 1) * 512],
                      reads=[bcTk, Sbfk], writes=["pyo"])
                for h in range(gi * 8, gi * 8 + 8):
                    pr.mm(pyd1[:, (h % 8) * 64:(h % 8 + 1) * 64], mt[:, h * 128:(h + 1) * 128],
                          xdt[:, h * 64:(h + 1) * 64], inc=(h % 8 == 7), reads=[mtk, xdtk], writes=["pyd"])
                sl = slice(gi * 512, (gi + 1) * 512)
                pr.tt("dve", ytmp[:, sl].rearrange("p (h e) -> p h e", e=64),
                      pyo1[:].rearrange("p (h e) -> p h e", e=64),
                      eacs[:, gi * 8:(gi + 1) * 8].unsqueeze(2).broadcast_to([128, 8, 64]), ALU.mult,
                      reads=["pyo", smk + "ex"], writes=["ytmp"])
                pr.tt("dve", y[:, sl], ytmp[:, sl], pyd1[:], ALU.add, reads=["ytmp", "pyd"], writes=[yk])
            if d == 0:
                pend.append(lambda y=y, yk=yk, t0=t0: pr.dma(g.YF[t0:t0 + 128, :], y[:], reads=[yk], eng="pool"))
            else:
                yf, z = yfb[b], zb[b]
                yfk, zk = "yfb" + B, "zb" + B
                pr.tt("dve", y[:], y[:], yf[:], ALU.add, reads=[yk, yfk], writes=[yk])
                pr.tt("pool", ytmp[:].rearrange("p (h e) -> p h e", e=64), xs3,
                      dsum[:].unsqueeze(2).broadcast_to([128, 16, 64]), ALU.mult, reads=[xk_, "dsum"], writes=["ytmp"])
                pr.tt("dve", y[:], y[:], ytmp[:], ALU.add, reads=[yk, "ytmp"], writes=[yk])
                pr.act(z[:], z[:], AF.Silu, reads=[zk], writes=[zk])
                pr.tt("dve", ub[:], y[:], z[:], ALU.mult, reads=[yk, zk], writes=["ub"])
                for gi in range(2):
                    sl = slice(gi * 512, (gi + 1) * 512)
                    pr.act(ytmp[:, sl], ub[:, sl], AF.Square, reads=["ub"], writes=["ytmp", "ms"],
                           accum_out=ms[:, gi:gi + 1])
                pr.ts("dve", ms[:, 2:4], ms[:, 0:2], 1.0 / 512, EPS, ALU.mult, ALU.add, reads=["ms"], writes=["ms2"])
                pr.act(ms[:, 2:4], ms[:, 2:4], AF.Sqrt, reads=["ms2"], writes=["ms2"])
                pr.op("dve", lambda e: e.reciprocal(ms[:, 2:4], ms[:, 2:4]), reads=["ms2"], writes=["ms2"])
                for gi in range(2):
                    sl = slice(gi * 512, (gi + 1) * 512)
                    pr.stt("dve", y[:, sl], ub[:, sl], ms[:, 2 + gi:3 + gi], ssdw[:, sl], ALU.mult, ALU.mult,
                           reads=["ub", "ms2", "ssdw"], writes=[yk])
                pend.append(lambda y=y, yk=yk, t0=t0: pr.dma(g.YS[t0:t0 + 128, :], y[:], reads=[yk], eng="pool"))

        en = ("0", "1a", "1b", "2")
        for i in range(-3, NT + 1):
            if 0 <= i + 3 < NT and "0" in en:
                s0(i + 3)
            while pend:
                pend.pop(0)()
            if i >= NT:
                break
            if 0 <= i + 2 < NT and "1a" in en:
                s1a(i + 2)
            if 0 <= i + 1 < NT and "1b" in en:
                s1b(i + 1)
            if i >= 0 and "2" in en:
                s2(i)


def phase_NP(g, l):
    with ExitStack() as st:
        cst = load_consts(g, st, ["ident_b"])
        np_tile = np_setup(g, st, l, cst["ident_b"])
        for t in range(NT):
            np_tile(t)


def np_setup(g, st, l, identb):
    pr, I, nc = g.pr, g.I, g.nc
    if True:
        wq = sbt(g, st, "wq", [128, 64], F32)
        wk = sbt(g, st, "wk", [128, 64], F32)
        pr.dma(wq[:], rowb(I["q_norm_w"][l:l + 1, :], 64), writes=["wq"])
        pr.dma(wk[:], rowb(I["k_norm_w"][l:l + 1, :], 64), writes=["wk"])
        pr.ts("dve", wq[:], wq[:], 0.125, None, ALU.mult, reads=["wq"], writes=["wq"])
        rc = [sbt(g, st, "rc%d" % i, [128, 64], F32) for i in range(2)]
        rsn = [sbt(g, st, "rsn%d" % i, [128, 64], F32) for i in range(2)]
        xin = [sbt(g, st, "xin%d" % i, [128, 1024], F32) for i in range(2)]
        junk = sbt(g, st, "njunk", [128, 1024], F32)
        xn = sbt(g, st, "xn", [128, 1024], F32)
        t1 = sbt(g, st, "t1", [128, 1024], F32)
        t2 = sbt(g, st, "t2", [128, 1024], F32)
        xr = sbt(g, st, "xr", [128, 1024], BF16)
        ss = sbt(g, st, "ss", [128, 32], F32)
        xT = [sbt(g, st, "xT%d" % i, [128, 8, 128], BF16) for i in range(2)]
        ptr = [pst(g, st, "ptrn%d" % i, [128, 1024], BF16) for i in range(2)]
        nn = [0]

        def np_tile(t):
            n = nn[0]
            t0 = t * 128
            lat = t >= 2
            if lat:
                pr.dma(rc[t % 2][:], I["ropec"][(t - 2) * 128:(t - 1) * 128, :], writes=["rc%d" % (t % 2)])
                pr.dma(rsn[t % 2][:], I["ropes"][(t - 2) * 128:(t - 1) * 128, :], writes=["rsn%d" % (t % 2)])
            for wi, (c0, w, wn, dst) in enumerate([(C_Q, wq, "wq", g.QT), (C_K, wk, "wk", g.KT)]):
                x = xin[n % 2]
                xk = "xin%d" % (n % 2)
                pr.dma(x[:], g.P[t0:t0 + 128, c0:c0 + 1024], writes=[xk])
                pr.act(junk[:], x[:], AF.Square, reads=[xk], writes=["njunk"])
                pr.op("dve", lambda e, junk=junk, ss=ss: e.reduce_sum(
                    ss[:, 0:16], junk[:].rearrange("p (h e) -> p h e", e=64), AX.X),
                    reads=["njunk"], writes=["ss"])
                pr.ts("dve", ss[:, 16:32], ss[:, 0:16], 1.0 / 64, EPS, ALU.mult, ALU.add, reads=["ss"], writes=["ss2"])
                pr.act(ss[:, 16:32], ss[:, 16:32], AF.Sqrt, reads=["ss2"], writes=["ss2"])
                pr.op("dve", lambda e, ss=ss: e.reciprocal(ss[:, 16:32], ss[:, 16:32]), reads=["ss2"], writes=["ss2"])
                x3 = x[:].rearrange("p (h e) -> p h e", e=64)
                xn3 = xn[:].rearrange("p (h e) -> p h e", e=64)
                pr.tt("dve", xn3, x3, ss[:, 16:32].unsqueeze(2).broadcast_to([128, 16, 64]), ALU.mult,
                      reads=[xk, "ss2"], writes=["xn"])
                pr.tt("pool", xn3, xn3, w[:].unsqueeze(1).broadcast_to([128, 16, 64]), ALU.mult,
                      reads=["xn", wn], writes=["xn"])
                if lat:
                    c_ = rc[t % 2]
                    s_ = rsn[t % 2]
                    pr.tt("dve", t1[:].rearrange("p (h e) -> p h e", e=64), xn3,
                          c_[:].unsqueeze(1).broadcast_to([128, 16, 64]), ALU.mult,
                          reads=["xn", "rc%d" % (t % 2)], writes=["t1"])
                    xn4 = xn[:].rearrange("p (h b e) -> p h b e", b=2, e=32)
                    t24 = t2[:].rearrange("p (h b e) -> p h b e", b=2, e=32)
                    s4 = s_[:].rearrange("p (b e) -> p b e", e=32)
                    pr.tt("pool", t24[:, :, :, 0:16], xn4[:, :, :, 16:32],
                          s4[:, :, 0:16].unsqueeze(1).broadcast_to([128, 16, 2, 16]), ALU.mult,
                          reads=["xn", "rsn%d" % (t % 2)], writes=["t2a"])
                    pr.tt("pool", t24[:, :, :, 16:32], xn4[:, :, :, 0:16],
                          s4[:, :, 16:32].unsqueeze(1).broadcast_to([128, 16, 2, 16]), ALU.mult,
                          reads=["xn", "rsn%d" % (t % 2)], writes=["t2b"])
                    pr.tt("dve", xr[:], t1[:], t2[:], ALU.add, reads=["t1", "t2a", "t2b"], writes=["xr"])
                else:
                    pr.copy("dve", xr[:], xn[:], reads=["xn"], writes=["xr"])
                p = ptr[n % 2]
                pk = "ptrn%d" % (n % 2)
                for k in range(8):
                    pr.tr(p[:, k * 128:(k + 1) * 128], xr[:, k * 128:(k + 1) * 128], identb[:], inc=(k == 7),
                          reads=["xr", "ident_b"], writes=[pk])
                o = xT[n % 2]
                ok = "xT%d" % (n % 2)
                pr.copy("act", o[:], p[:].rearrange("p (k n) -> p k n", k=8), reads=[pk], writes=[ok])
                pr.dma(dst[t], o[:], reads=[ok], writes=["%s%d" % ("QT" if wi == 0 else "KT", t)], eng="pool")
                n += 1
            nn[0] = n

        return np_tile


def phase_NA(g, l, fuse_np=False):
    pr, I, nc = g.pr, g.I, g.nc
    LOOK = 6
    with ExitStack() as st:
        cst = load_consts(g, st, ["ident_b"])
        identb = cst["ident_b"]
        np_done = [NT]
        if fuse_np:
            np_tile = np_setup(g, st, l, identb)
            np_done[0] = 0

        def np_upto(t):
            while np_done[0] <= min(t, NT - 1):
                np_tile(np_done[0])
                np_done[0] += 1

        np_upto(LOOK - 1)
        tab0 = sbt(g, st, "tab0", [128, 16, 640], BF16)
        tabE = sbt(g, st, "tabE", [128, 16, 640], BF16)
        for h in range(16):
            pr.dma(tab0[:, h, :], I["nab"][l, 0, h], writes=["tab0"], eng="pool")
        for h4 in range(4):
            pr.act(tab0[:, 4 * h4:4 * h4 + 4, :], tab0[:, 4 * h4:4 * h4 + 4, :], AF.Exp, reads=["tab0"], writes=["tab0"])
        kr = [sbt(g, st, "kr%d" % i, [128, 8, 128], BF16) for i in range(6)]
        vr = [sbt(g, st, "vr%d" % i, [128, 16, 65], BF16) for i in range(6)]
        kc = [sbt(g, st, "kc%d" % i, [128, 8, 128], BF16) for i in range(2)]
        vc = [sbt(g, st, "vc%d" % i, [128, 16, 65], BF16) for i in range(2)]
        for i in range(6):
            pr.memset("dve", vr[i][:, :, 64:65], 1.0, writes=["vr1_%d" % i])
        for i in range(2):
            pr.memset("dve", vc[i][:, :, 64:65], 1.0, writes=["vc1_%d" % i])
            pr.dma(kc[i][:], g.KT[i], reads=["KT%d" % i], writes=["kc%d" % i])
            pr.dma(vc[i][:, :, 0:64], g.P[i * 128:(i + 1) * 128, C_V:C_V + 1024].rearrange("p (h e) -> p h e", e=64),
                   writes=["vc%d" % i], eng="pool")
        qb = [sbt(g, st, "qb%d" % i, [128, 8, 128], BF16) for i in range(2)]
        et = [sbt(g, st, "et%d" % i, [128, 7, 128], BF16) for i in range(2)]
        yb = [sbt(g, st, "ynb%d" % i, [128, 1024], F32) for i in range(2)]
        rec = sbt(g, st, "rec", [128, 8], F32)
        pA = [pst(g, st, "pA%d" % i, [128, 512], F32) for i in range(2)]
        pB = [pst(g, st, "pB%d" % i, [128, 512], F32) for i in range(2)]
        pO = [pst(g, st, "pO%d" % i, [128, 512], F32) for i in range(2)]
        loaded = [0]

        def load_chunks(upto):
            while loaded[0] <= min(upto, 63):
                c = loaded[0]
                sl = c % 6
                pr.dma(kr[sl][:], g.KT[c + 2], reads=["KT%d" % (c + 2)], writes=["kr%d" % sl])
                pr.dma(vr[sl][:, :, 0:64],
                       g.P[(c + 2) * 128:(c + 3) * 128, C_V:C_V + 1024].rearrange("p (h e) -> p h e", e=64),
                       writes=["vr%d" % sl], eng="pool")
                loaded[0] += 1

        def cbase(jq):
            return min(max(jq - 2, 0), 59)

        hcount_box = [0]
        pr.dma(qb[0][:], g.QT[0], reads=["QT0"], writes=["qb0"])
        for t in range(NT):
            np_upto(t + LOOK)
            jq = t - 2
            q = qb[t % 2]
            qk = "qb%d" % (t % 2)
            if t + 1 < NT:
                pr.dma(qb[(t + 1) % 2][:], g.QT[t + 1], reads=["QT%d" % (t + 1)], writes=["qb%d" % ((t + 1) % 2)])
            if jq >= 0:
                load_chunks(cbase(min(jq + 1, 63)) + 4)
                cls = {0: 1, 1: 2, 62: 3, 63: 4}.get(jq, 0)
                if cls:
                    for h in range(16):
                        pr.dma(tabE[:, h, :], I["nab"][l, cls, h], writes=["tabE"], eng="pool")
                    for h4 in range(4):
                        pr.act(tabE[:, 4 * h4:4 * h4 + 4, :], tabE[:, 4 * h4:4 * h4 + 4, :], AF.Exp,
                               reads=["tabE"], writes=["tabE"])
                tab, tabk = (tabE, "tabE") if cls else (tab0, "tab0")
                cb = cbase(jq)
                chunks = [(kr[(cb + i) % 6], "kr%d" % ((cb + i) % 6), vr[(cb + i) % 6],
                           ["vr%d" % ((cb + i) % 6), "vr1_%d" % ((cb + i) % 6)]) for i in range(5)]
            else:
                chunks = []
                tab = None
            chunks = chunks + [(kc[i], "kc%d" % i, vc[i], ["vc%d" % i, "vc1_%d" % i]) for i in range(2)]
            first = 0 if jq >= 0 else 5
            y = yb[t % 2]
            yk = "ynb%d" % (t % 2)
            hinfo = {}

            def s_stage(h):
                p_, bp = h // 2, (h % 2) * 64
                hc = hcount_box[0]
                hcount_box[0] += 1
                a_, b_ = pA[hc % 2], pB[hc % 2]
                ak, bk = "pA%d" % (hc % 2), "pB%d" % (hc % 2)
                e_ = et[hc % 2]
                ek = "et%d" % (hc % 2)
                hinfo[h] = (e_, ek)
                qv = q[bp:bp + 64, p_, :]
                if jq >= 0:
                    for ci in range(4):
                        kt, kk = chunks[ci][0], chunks[ci][1]
                        pr.mm(a_[:, ci * 128:(ci + 1) * 128], kt[bp:bp + 64, p_, :], qv, start=True, stop=True,
                              inc=(ci == 3), reads=[kk, qk], writes=[ak])
                    pr.mm(b_[:, 0:128], chunks[4][0][bp:bp + 64, p_, :], qv, start=True, stop=True, inc=False,
                          reads=[chunks[4][1], qk], writes=[bk])
                for i in range(2):
                    kt, kk = chunks[-2 + i][0], chunks[-2 + i][1]
                    pr.mm(b_[:, 128 + i * 128:256 + i * 128], kt[bp:bp + 64, p_, :], qv, start=True, stop=True,
                          inc=(i == 1), reads=[kk, qk], writes=[bk])
                if jq >= 0:
                    pr.act(e_[:, 0:4, :], a_[:].rearrange("p (c q) -> p c q", c=4), AF.Exp, reads=[ak], writes=[ek])
                    pr.act(e_[:, 4:7, :], b_[:, 0:384].rearrange("p (c q) -> p c q", c=3), AF.Exp,
                           reads=[bk], writes=[ek])
                    pr.tt("dve", e_[:, 0:5, :], e_[:, 0:5, :], tab[:, h, :].rearrange("p (c q) -> p c q", c=5),
                          ALU.mult, reads=[ek, tabk], writes=[ek])
                else:
                    pr.act(e_[:, 5:7, :], b_[:, 128:384].rearrange("p (c q) -> p c q", c=2), AF.Exp,
                           reads=[bk], writes=[ek])

            def pv_stage(h):
                hg, j = h // 4, h % 4
                po = pO[hg % 2]
                pok = "pO%d" % (hg % 2)
                po3 = po[:].rearrange("p (j e) -> p j e", j=4)
                e_, ek = hinfo[h]
                for ci in range(first, 7):
                    vt, vks = chunks[ci - first][2], chunks[ci - first][3]
                    pr.mm(po3[:, j, 0:65], e_[:, ci, :], vt[:, h, :], start=(ci == first), stop=(ci == 6),
                          inc=(ci == 6), reads=[ek] + vks, writes=[pok])
                if j == 3:
                    pr.op("dve", lambda e, po3=po3, rec=rec, hg=hg: e.reciprocal(
                        rec[:, (hg % 2) * 4:(hg % 2) * 4 + 4].unsqueeze(2), po3[:, :, 64:65]),
                        reads=[pok], writes=["rec%d" % (hg % 2)])
                    pr.tt("dve", y[:, hg * 256:(hg + 1) * 256].rearrange("p (j e) -> p j e", j=4), po3[:, :, 0:64],
                          rec[:, (hg % 2) * 4:(hg % 2) * 4 + 4].unsqueeze(2).broadcast_to([128, 4, 64]), ALU.mult,
                          reads=[pok, "rec%d" % (hg % 2)], writes=[yk])

            s_stage(0)
            for h in range(16):
                if h + 1 < 16:
                    s_stage(h + 1)
                pv_stage(h)
            pr.dma(g.YN[t * 128:(t + 1) * 128, :], y[:], reads=[yk], eng="pool")


def load_w_bf16(g, t, src, nk, key):
    for k in range(nk):
        g.pr.dma(t[:, k, :], src[k * 128:(k + 1) * 128, :], writes=[key], eng="pool")


def mod_row(g, l, v, which):
    return rowb(g.MOD[l, v:v + 1, which * 1024:(which + 1) * 1024], 1024)


def phase_C1(g, l):
    pr, I, nc = g.pr, g.I, g.nc
    with ExitStack() as st:
        cst = load_consts(g, st, ["ident_b"])
        identb = cst["ident_b"]
        wbs = sbt(g, st, "wbs", [128, 8, 1024], BF16)
        wbn = sbt(g, st, "wbn", [128, 8, 1024], BF16)
        wo = sbt(g, st, "wo", [128, 8, 1024], BF16)
        load_w_bf16(g, wbs, I["w_br_ssd"][l], 8, "wbs")
        load_w_bf16(g, wbn, I["w_br_na"][l], 8, "wbn")
        load_w_bf16(g, wo, I["w_out"][l], 8, "wo")
        gate = sbt(g, st, "gate1", [128, 1024], F32)
        ysb = [sbt(g, st, "c1ys%d" % i, [128, 1024], BF16) for i in range(2)]
        ynb = [sbt(g, st, "c1yn%d" % i, [128, 1024], BF16) for i in range(2)]
        gb = [sbt(g, st, "c1g%d" % i, [128, 2048], F32) for i in range(2)]
        hb = [sbt(g, st, "c1h%d" % i, [128, 1024], F32) for i in range(2)]
        ysT = sbt(g, st, "ysT", [128, 8, 128], BF16)
        ynT = sbt(g, st, "ynT", [128, 8, 128], BF16)
        mixT = sbt(g, st, "mixT", [128, 8, 128], BF16)
        tmp = sbt(g, st, "c1tmp", [128, 1024], F32)
        tmp2 = sbt(g, st, "c1tmp2", [128, 1024], F32)
        mixb = sbt(g, st, "mixb", [128, 1024], BF16)
        hn = [sbt(g, st, "c1hn%d" % i, [128, 1024], F32) for i in range(2)]
        ptr = [pst(g, st, "c1ptr%d" % i, [128, 1024], BF16) for i in range(2)]
        pb1 = [pst(g, st, "pb1%d" % i, [128, 512], F32) for i in range(2)]
        pb2 = [pst(g, st, "pb2%d" % i, [128, 512], F32) for i in range(2)]
        pmo = [pst(g, st, "pmo%d" % i, [128, 512], F32) for i in range(2)]

        def loads(t):
            i = t % 2
            r = slice(t * 128, (t + 1) * 128)
            pr.dma(ysb[i][:], g.YS[r, :], writes=["c1ys%d" % i], eng="pool")
            pr.dma(ynb[i][:], g.YN[r, :], writes=["c1yn%d" % i], eng="pool")
            pr.dma(gb[i][:], g.P[r, 0:2048], writes=["c1g%d" % i])
            pr.dma(hb[i][:], src_rows(g, l, t), writes=["c1h%d" % i])

        hb.append(sbt(g, st, "c1h2", [128, 1024], F32))
        hb.append(sbt(g, st, "c1h3", [128, 1024], F32))
        mixb2 = [mixb, sbt(g, st, "mixb1", [128, 1024], BF16)]

        def loads3(t):
            i = t % 2
            r = slice(t * 128, (t + 1) * 128)
            pr.dma(ysb[i][:], g.YS[r, :], writes=["c1ys%d" % i], eng="pool")
            pr.dma(ynb[i][:], g.YN[r, :], writes=["c1yn%d" % i], eng="pool")
            pr.dma(gb[i][:], g.P[r, 0:2048], writes=["c1g%d" % i])
            pr.dma(hb[t % 4][:], src_rows(g, l, t), writes=["c1h%d" % (t % 4)])

        def stA(t):
            i = t % 2
            for k in range(8):
                pr.tr(ptr[0][:, k * 128:(k + 1) * 128], ysb[i][:, k * 128:(k + 1) * 128], identb[:], inc=(k == 7),
                      reads=["c1ys%d" % i, "ident_b"], writes=["c1ptr0"])
            pr.copy("act", ysT[:], ptr[0][:].rearrange("p (k n) -> p k n", k=8), reads=["c1ptr0"], writes=["ysT"])
            for k in range(8):
                pr.tr(ptr[1][:, k * 128:(k + 1) * 128], ynb[i][:, k * 128:(k + 1) * 128], identb[:], inc=(k == 7),
                      reads=["c1yn%d" % i, "ident_b"], writes=["c1ptr1"])
            pr.copy("dve", ynT[:], ptr[1][:].rearrange("p (k n) -> p k n", k=8), reads=["c1ptr1"], writes=["ynT"])
            for cg in range(2):
                for k in range(8):
                    pr.mm(pb1[cg][:], ysT[:, k, :], wbs[:, k, cg * 512:(cg + 1) * 512], start=(k == 0), stop=(k == 7),
                          inc=(k == 7), reads=["ysT", "wbs"], writes=["pb1%d" % cg])
            for cg in range(2):
                for k in range(8):
                    pr.mm(pb2[cg][:], ynT[:, k, :], wbn[:, k, cg * 512:(cg + 1) * 512], start=(k == 0), stop=(k == 7),
                          inc=(k == 7), reads=["ynT", "wbn"], writes=["pb2%d" % cg])
            gk = "c1g%d" % i
            pr.act(gb[i][:], gb[i][:], AF.Sigmoid, reads=[gk], writes=[gk])
            for cg in range(2):
                sl = slice(cg * 512, (cg + 1) * 512)
                pr.tt("dve", tmp[:, sl], pb1[cg][:], gb[i][:, sl], ALU.mult, reads=["pb1%d" % cg, gk], writes=["c1tmp"])
                pr.tt("dve", tmp2[:, sl], pb2[cg][:], gb[i][:, 1024 + cg * 512:1024 + (cg + 1) * 512], ALU.mult,
                      reads=["pb2%d" % cg, gk], writes=["c1tmp2"])
            pr.tt("pool", mixb2[i][:], tmp[:], tmp2[:], ALU.add, reads=["c1tmp", "c1tmp2"], writes=["mixb%d" % i])
            if t + 2 < NT:
                loads3(t + 2)

        def stB(t):
            i = t % 2
            if t == 0 or t == 2:
                pr.dma(gate[:], mod_row(g, l, 0 if t == 0 else 1, 2), writes=["gate1"])
            for k in range(8):
                pr.tr(ptr[0][:, k * 128:(k + 1) * 128], mixb2[i][:, k * 128:(k + 1) * 128], identb[:], inc=(k == 7),
                      reads=["mixb%d" % i, "ident_b"], writes=["c1ptr0"])
            pr.copy("act", mixT[:], ptr[0][:].rearrange("p (k n) -> p k n", k=8), reads=["c1ptr0"], writes=["mixT"])
            for cg in range(2):
                for k in range(8):
                    pr.mm(pmo[cg][:], mixT[:, k, :], wo[:, k, cg * 512:(cg + 1) * 512], start=(k == 0), stop=(k == 7),
                          inc=(k == 7), reads=["mixT", "wo"], writes=["pmo%d" % cg])
            o = hn[i]
            ok = "c1hn%d" % i
            for cg in range(2):
                sl = slice(cg * 512, (cg + 1) * 512)
                pr.tt("dve", o[:, sl], pmo[cg][:], gate[:, sl], ALU.mult, reads=["pmo%d" % cg, "gate1"], writes=[ok])
            pr.tt("pool", o[:], o[:], hb[t % 4][:], ALU.add, reads=[ok, "c1h%d" % (t % 4)], writes=[ok])
            pr.dma(g.H[t * 128:(t + 1) * 128, :], o[:], reads=[ok], eng="pool")

        loads3(0)
        loads3(1)
        stA(0)
        for t in range(NT):
            if t + 1 < NT:
                stA(t + 1)
            stB(t)


def norm2_setup(g, st, l):
    nw = sbt(g, st, "n2w", [128, 1024], F32)
    g.pr.dma(nw[:], rowb(g.I["norm2_w"][l:l + 1, :], 1024), writes=["n2w"])
    G2 = sbt(g, st, "G2", [128, 1024], F32)
    SH2 = sbt(g, st, "SH2", [128, 1024], F32)
    GT2 = sbt(g, st, "GT2", [128, 1024], F32)
    return nw, G2, SH2, GT2


def norm2_variant(g, l, v, nw, G2, SH2, GT2):
    pr = g.pr
    pr.dma(G2[:], mod_row(g, l, v, 4), writes=["G2"])
    pr.stt("dve", G2[:], G2[:], 1.0, nw[:], ALU.add, ALU.mult, reads=["G2", "n2w"], writes=["G2"])
    pr.dma(SH2[:], mod_row(g, l, v, 3), writes=["SH2"])
    pr.dma(GT2[:], mod_row(g, l, v, 5), writes=["GT2"])


def phase_C2(g, l):
    pr, I, nc = g.pr, g.I, g.nc
    with ExitStack() as st:
        cst = load_consts(g, st, ["ident_b"])
        identb = cst["ident_b"]
        w1 = sbt(g, st, "w1", [128, 8, D_FF], BF16)
        w3 = sbt(g, st, "w3", [128, 8, D_FF], BF16)
        w2 = sbt(g, st, "w2", [128, 22, 1024], BF16)
        load_w_bf16(g, w1, I["w_ff1"][0], 8, "w1")
        load_w_bf16(g, w3, I["w_ff3"][0], 8, "w3")
        load_w_bf16(g, w2, I["w_ff2"][0], 22, "w2")
        nw, G2, SH2, GT2 = norm2_setup(g, st, l)
        hb = [sbt(g, st, "c2h%d" % i, [128, 1024], F32) for i in range(2)]
        tmp = sbt(g, st, "c2tmp", [128, 1024], F32)
        fbf = sbt(g, st, "fbf", [128, 1024], BF16)
        fT = sbt(g, st, "fT", [128, 8, 128], BF16)
        ssq = sbt(g, st, "c2ssq", [128, 2], F32)
        stmp = [sbt(g, st, "stmp%d" % i, [128, 512], F32) for i in range(2)]
        actb2 = [sbt(g, st, "actb%d" % i, [128, D_FF], BF16) for i in range(2)]
        actT2 = [sbt(g, st, "actT%d" % i, [128, 22, 128], BF16) for i in range(2)]
        ho = [sbt(g, st, "c2ho%d" % i, [128, 1024], F32) for i in range(2)]
        ptr = [pst(g, st, "c2ptr%d" % i, [128, 1024], BF16) for i in range(2)]
        pu1 = [pst(g, st, "pu1%d" % i, [128, 512], F32) for i in range(2)]
        pu3 = [pst(g, st, "pu3%d" % i, [128, 512], F32) for i in range(2)]
        po = [pst(g, st, "po%d" % i, [128, 512], F32) for i in range(2)]
        pr.dma(hb[0][:], g.H[0:128, :], writes=["c2h0"])
        pr.dma(hb[1][:], g.H[128:256, :], writes=["c2h1"])

        def stA(t):
            i = t % 2
            hk = "c2h%d" % i
            h = hb[i]
            actb = actb2[i]
            abk = "actb%d" % i
            if t == 0 or t == 2:
                v = 0 if t == 0 else 1
                pr.dma(G2[:], mod_row(g, l, v, 4), writes=["G2"])
                pr.stt("dve", G2[:], G2[:], 1.0, nw[:], ALU.add, ALU.mult, reads=["G2", "n2w"], writes=["G2"])
                pr.dma(SH2[:], mod_row(g, l, v, 3), writes=["SH2"])
            rms_rstd(g, tmp[:], h[:], ssq[:, 0:1], ssq[:, 1:2], 1024, [hk], "c2ssq", "c2rs", "c2tmp")
            pr.stt("dve", tmp[:], h[:], ssq[:, 1:2], G2[:], ALU.mult, ALU.mult, reads=[hk, "c2rs", "G2"], writes=["c2tmp"])
            pr.tt("dve", fbf[:], tmp[:], SH2[:], ALU.add, reads=["c2tmp", "SH2"], writes=["fbf"])
            for k in range(8):
                pr.tr(ptr[0][:, k * 128:(k + 1) * 128], fbf[:, k * 128:(k + 1) * 128], identb[:], inc=(k == 7),
                      reads=["fbf", "ident_b"], writes=["c2ptr0"])
            pr.copy("act", fT[:], ptr[0][:].rearrange("p (k n) -> p k n", k=8), reads=["c2ptr0"], writes=["fT"])
            for cg in range(6):
                c0 = cg * 512
                n = min(512, D_FF - c0)
                a1, a3 = pu1[cg % 2], pu3[cg % 2]
                k1, k3 = "pu1%d" % (cg % 2), "pu3%d" % (cg % 2)
                for k in range(8):
                    pr.mm(a1[:, :n], fT[:, k, :], w1[:, k, c0:c0 + n], start=(k == 0), stop=(k == 7), inc=(k == 7),
                          reads=["fT", "w1"], writes=[k1])
                for k in range(8):
                    pr.mm(a3[:, :n], fT[:, k, :], w3[:, k, c0:c0 + n], start=(k == 0), stop=(k == 7), inc=(k == 7),
                          reads=["fT", "w3"], writes=[k3])
                s_ = stmp[cg % 2]
                sk = "stmp%d" % (cg % 2)
                pr.act(s_[:, :n], a1[:, :n], AF.Silu, reads=[k1], writes=[sk])
                pr.tt("dve", actb[:, c0:c0 + n], s_[:, :n], a3[:, :n], ALU.mult, reads=[sk, k3], writes=[abk])

        def stB1(t):
            i = t % 2
            actb, abk = actb2[i], "actb%d" % i
            actT, atk = actT2[i], "actT%d" % i
            for gi in range(3):
                k0 = gi * 8
                nk = min(8, 22 - k0)
                p = ptr[(gi + 1) % 2]
                pk = "c2ptr%d" % ((gi + 1) % 2)
                for k in range(nk):
                    pr.tr(p[:, k * 128:(k + 1) * 128], actb[:, (k0 + k) * 128:(k0 + k + 1) * 128], identb[:],
                          inc=(k == nk - 1), reads=[abk, "ident_b"], writes=[pk])
                pr.copy("act" if gi % 2 == 0 else "dve", actT[:, k0:k0 + nk, :],
                        p[:, 0:nk * 128].rearrange("p (k n) -> p k n", k=nk), reads=[pk], writes=[atk])

        def stB2(t):
            i = t % 2
            hk = "c2h%d" % i
            h = hb[i]
            actT, atk = actT2[i], "actT%d" % i
            if t == 0 or t == 2:
                pr.dma(GT2[:], mod_row(g, l, 0 if t == 0 else 1, 5), writes=["GT2"])
            for cg in range(2):
                for k in range(22):
                    pr.mm(po[cg][:], actT[:, k, :], w2[:, k, cg * 512:(cg + 1) * 512], start=(k == 0), stop=(k == 21),
                          inc=(k == 21), reads=[atk, "w2"], writes=["po%d" % cg])
            o = ho[i]
            ok = "c2ho%d" % i
            for cg in range(2):
                sl = slice(cg * 512, (cg + 1) * 512)
                pr.tt("dve", o[:, sl], po[cg][:], GT2[:, sl], ALU.mult, reads=["po%d" % cg, "GT2"], writes=[ok])
            pr.tt("pool", o[:], o[:], h[:], ALU.add, reads=[ok, hk], writes=[ok])
            pr.dma(g.H[t * 128:(t + 1) * 128, :], o[:], reads=[ok], eng="pool")
            if t + 2 < NT:
                pr.dma(h[:], g.H[(t + 2) * 128:(t + 3) * 128, :], writes=[hk])

        stA(0)
        for t in range(NT):
            stB1(t)
            if t + 1 < NT:
                stA(t + 1)
            stB2(t)


def phase_R(g, l):
    pr, I, nc = g.pr, g.I, g.nc
    with ExitStack() as st:
        cst = load_consts(g, st, ["ident_b", "ident"])
        identb, identf = cst["ident_b"], cst["ident"]
        wr = sbt(g, st, "wr", [128, 8, 8], F32)
        pr.dma(wr[:], I["w_router"][0].rearrange("(k p) e -> p k e", p=128), writes=["wr"])
        nw, G2, SH2, GT2 = norm2_setup(g, st, l)
        norm2_variant(g, l, 1, nw, G2, SH2, GT2)
        hb = [sbt(g, st, "rh%d" % i, [128, 1024], F32) for i in range(2)]
        junk = sbt(g, st, "rjunk", [128, 1024], F32)
        tmp = sbt(g, st, "rtmp", [128, 1024], F32)
        ffp = sbt(g, st, "ffp", [128, 1024], F32)
        fbf = sbt(g, st, "rfbf", [128, 1024], BF16)
        fT32 = sbt(g, st, "fT32", [128, 8, 128], F32)
        fT = [sbt(g, st, "rfT%d" % i, [128, 8, 128], BF16) for i in range(2)]
        sm = sbt(g, st, "rsm", [128, 64], F32)
        gt = [sbt(g, st, "rgt%d" % i, [128, 8], F32) for i in range(2)]
        ptf = pst(g, st, "ptf", [128, 1024], F32)
        ptr = pst(g, st, "rptr", [128, 1024], BF16)
        pl = pst(g, st, "pl", [128, 512], F32)
        lg, mk1, lg2, mk2 = sm[:, 0:8], sm[:, 8:16], sm[:, 16:24], sm[:, 24:32]
        ssq, rs, m1, m2, dl, g1, g2 = (sm[:, 32:33], sm[:, 33:34], sm[:, 34:35], sm[:, 35:36], sm[:, 36:37],
                                       sm[:, 37:38], sm[:, 38:39])
        pr.dma(hb[0][:], g.H[256:384, :], writes=["rh0"])
        for jq in range(64):
            t = jq + 2
            i = jq % 2
            h = hb[i]
            hk = "rh%d" % i
            if jq + 1 < 64:
                pr.dma(hb[(jq + 1) % 2][:], g.H[(t + 1) * 128:(t + 2) * 128, :], writes=["rh%d" % ((jq + 1) % 2)])
            rms_rstd(g, junk[:], h[:], ssq, rs, 1024, [hk], "rssq", "rrs", "rjunk")
            pr.stt("dve", tmp[:], h[:], rs, G2[:], ALU.mult, ALU.mult, reads=[hk, "rrs", "G2"], writes=["rtmp"])
            pr.tt("dve", ffp[:], tmp[:], SH2[:], ALU.add, reads=["rtmp", "SH2"], writes=["ffp"])
            pr.copy("pool", fbf[:], ffp[:], reads=["ffp"], writes=["rfbf"])
            for k in range(8):
                pr.tr(ptf[:, k * 128:(k + 1) * 128], ffp[:, k * 128:(k + 1) * 128], identf[:], inc=(k == 7),
                      reads=["ffp", "ident"], writes=["ptf"])
            pr.copy("act", fT32[:], ptf[:].rearrange("p (k n) -> p k n", k=8), reads=["ptf"], writes=["fT32"])
            for k in range(8):
                pr.mm(pl[:, 0:8], fT32[:, k, :], wr[:, k, :], start=(k == 0), stop=(k == 7), inc=(k == 7),
                      reads=["fT32", "wr"], writes=["pl"])
            pr.copy("dve", lg, pl[:, 0:8], reads=["pl"], writes=["lg"])
            pr.op("dve", lambda e: e.reduce_max(m1, lg, AX.X), reads=["lg"], writes=["m1"])
            pr.ts("dve", mk1, lg, m1, None, ALU.is_equal, reads=["lg", "m1"], writes=["mk1"])
            pr.stt("dve", lg2, mk1, -1e30, lg, ALU.mult, ALU.add, reads=["mk1", "lg"], writes=["lg2"])
            pr.op("dve", lambda e: e.reduce_max(m2, lg2, AX.X), reads=["lg2"], writes=["m2"])
            pr.ts("dve", mk2, lg2, m2, None, ALU.is_equal, reads=["lg2", "m2"], writes=["mk2"])
            pr.tt("dve", dl, m2, m1, ALU.subtract, reads=["m1", "m2"], writes=["dl"])
            pr.act(dl, dl, AF.Exp, reads=["dl"], writes=["dl"])
            pr.ts("dve", g1, dl, 1.0, None, ALU.add, reads=["dl"], writes=["g1"])
            pr.op("dve", lambda e: e.reciprocal(g1, g1), reads=["g1"], writes=["g1"])
            pr.tt("dve", g2, dl, g1, ALU.mult, reads=["dl", "g1"], writes=["g2"])
            go = gt[i]
            gk = "rgt%d" % i
            pr.ts("dve", go[:], mk1, g1, None, ALU.mult, reads=["mk1", "g1"], writes=[gk])
            pr.stt("dve", go[:], mk2, g2, go[:], ALU.mult, ALU.add, reads=["mk2", "g2", gk], writes=[gk])
            pr.dma(g.GT[jq], go[:], reads=[gk], eng="pool")
            for k in range(8):
                pr.tr(ptr[:, k * 128:(k + 1) * 128], fbf[:, k * 128:(k + 1) * 128], identb[:], inc=(k == 7),
                      reads=["rfbf", "ident_b"], writes=["rptr"])
            fo = fT[i]
            fk = "rfT%d" % i
            pr.copy("act", fo[:], ptr[:].rearrange("p (k n) -> p k n", k=8), reads=["rptr"], writes=[fk])
            pr.dma(g.FT[jq], fo[:], reads=[fk], eng="pool")


def phase_E(g, l):
    pr, I, nc = g.pr, g.I, g.nc
    NS = 8
    FQ = 896
    with ExitStack() as st:
        cst = load_consts(g, st, ["ident_b"])
        identb = cst["ident_b"]
        w1 = [sbt(g, st, "ew1_%d" % i, [128, 8, FQ], BF16) for i in range(2)]
        w3 = [sbt(g, st, "ew3_%d" % i, [128, 8, FQ], BF16) for i in range(2)]
        w2 = [sbt(g, st, "ew2_%d" % i, [128, 7, 1024], BF16) for i in range(2)]
        gate = sbt(g, st, "egate", [128, 1024], F32)
        pr.dma(gate[:], mod_row(g, l, 1, 5), writes=["egate"])
        fT = sbt(g, st, "efT", [128, NS, 8, 128], BF16)
        gt = sbt(g, st, "egt", [128, NS, 8], F32)
        acc = sbt(g, st, "eacc", [128, NS, 1024], F32)
        stmp = [sbt(g, st, "estmp%d" % i, [128, 512], F32) for i in range(2)]
        actb = sbt(g, st, "eactb", [128, FQ], BF16)
        actT = sbt(g, st, "eactT", [128, 7, 128], BF16)
        hb = [sbt(g, st, "eh%d" % i, [128, 1024], F32) for i in range(2)]
        ptr = pst(g, st, "eptr", [128, 1024], BF16)
        pu1 = [pst(g, st, "epu1%d" % i, [128, 512], F32) for i in range(2)]
        pu3 = [pst(g, st, "epu3%d" % i, [128, 512], F32) for i in range(2)]
        po = [pst(g, st, "epo%d" % i, [128, 512], F32) for i in range(2)]

        def load_unit(u):
            e, qd = u // 4, u % 4
            i = un_idx[0] % 2
            un_idx[0] += 1
            f0 = qd * FQ
            for k in range(8):
                pr.dma(w1[i][:, k, :], I["w_e1"][0, e, k * 128:(k + 1) * 128, f0:f0 + FQ], writes=["ew1_%d" % i], eng="pool")
                pr.dma(w3[i][:, k, :], I["w_e3"][0, e, k * 128:(k + 1) * 128, f0:f0 + FQ], writes=["ew3_%d" % i], eng="pool")
            for k in range(7):
                pr.dma(w2[i][:, k, :], I["w_e2"][0, e, f0 + k * 128:f0 + (k + 1) * 128, :], writes=["ew2_%d" % i], eng="pool")
            return i

        un_idx = [0]
        for stl in range(64 // NS):
            pr.dma(fT[:].rearrange("p t k n -> p t (k n)"),
                   g.FT[stl * NS:(stl + 1) * NS].rearrange("t p k n -> p t (k n)"), writes=["efT"])
            pr.dma(gt[:], g.GT[stl * NS:(stl + 1) * NS].rearrange("t p e -> p t e"), writes=["egt"])
            pr.memset("pool", acc[:], 0.0, writes=["eacc"])
            nxt = load_unit(0)
            for u in range(32):
                e = u // 4
                wi = nxt
                if u + 1 < 32:
                    nxt = load_unit(u + 1)
                for ti in range(NS):
                    for cg in range(2):
                        c0 = cg * 512
                        n = min(512, FQ - c0)
                        for k in range(8):
                            pr.mm(pu1[cg][:, :n], fT[:, ti, k, :], w1[wi][:, k, c0:c0 + n], start=(k == 0), stop=(k == 7),
                                  inc=(k == 7), reads=["efT", "ew1_%d" % wi], writes=["epu1%d" % cg])
                        for k in range(8):
                            pr.mm(pu3[cg][:, :n], fT[:, ti, k, :], w3[wi][:, k, c0:c0 + n], start=(k == 0), stop=(k == 7),
                                  inc=(k == 7), reads=["efT", "ew3_%d" % wi], writes=["epu3%d" % cg])
                        pr.act(stmp[cg][:, :n], pu1[cg][:, :n], AF.Silu, reads=["epu1%d" % cg], writes=["estmp%d" % cg])
                        pr.stt("dve", actb[:, c0:c0 + n], stmp[cg][:, :n], gt[:, ti, e:e + 1], pu3[cg][:, :n],
                               ALU.mult, ALU.mult, reads=["estmp%d" % cg, "egt", "epu3%d" % cg], writes=["eactb"])
                    for k in range(7):
                        pr.tr(ptr[:, k * 128:(k + 1) * 128], actb[:, k * 128:(k + 1) * 128], identb[:], inc=(k == 6),
                              reads=["eactb", "ident_b"], writes=["eptr"])
                    pr.copy("act", actT[:], ptr[:, 0:896].rearrange("p (k n) -> p k n", k=7), reads=["eptr"], writes=["eactT"])
                    for cg in range(2):
                        for k in range(7):
                            pr.mm(po[cg][:], actT[:, k, :], w2[wi][:, k, cg * 512:(cg + 1) * 512], start=(k == 0),
                                  stop=(k == 6), inc=(k == 6), reads=["eactT", "ew2_%d" % wi], writes=["epo%d" % cg])
                        sl = slice(cg * 512, (cg + 1) * 512)
                        pr.tt("dve", acc[:, ti, sl], po[cg][:], acc[:, ti, sl], ALU.add,
                              reads=["epo%d" % cg, "eacc"], writes=["eacc"])
            for ti in range(NS):
                jq = stl * NS + ti
                t = jq + 2
                h = hb[ti % 2]
                hk = "eh%d" % (ti % 2)
                pr.dma(h[:], g.H[t * 128:(t + 1) * 128, :], writes=[hk])
                pr.tt("dve", acc[:, ti, :], acc[:, ti, :], gate[:], ALU.mult, reads=["eacc", "egate"], writes=["eacc"])
                pr.tt("dve", h[:], h[:], acc[:, ti, :], ALU.add, reads=[hk, "eacc"], writes=[hk])
                pr.dma(g.out[jq * 128:(jq + 1) * 128, :], h[:], reads=[hk], eng="pool")


I32 = mybir.dt.int32


def phase_R2(g, l):
    pr, I, nc = g.pr, g.I, g.nc
    with ExitStack() as st:
        cst = load_consts(g, st, ["ident", "ones", "stri", "thr", "thr2"])
        identf, ones, stri, thr, thr2 = cst["ident"], cst["ones"], cst["stri"], cst["thr"], cst["thr2"]
        wr = sbt(g, st, "wr", [128, 8, 8], F32)
        pr.dma(wr[:], I["w_router"][0].rearrange("(k p) e -> p k e", p=128), writes=["wr"])
        nw, G2, SH2, GT2 = norm2_setup(g, st, l)
        norm2_variant(g, l, 1, nw, G2, SH2, GT2)
        hb = [sbt(g, st, "rh%d" % i, [128, 1024], F32) for i in range(2)]
        junk = sbt(g, st, "rjunk", [128, 1024], F32)
        tmp = sbt(g, st, "rtmp", [128, 1024], F32)
        ffp = sbt(g, st, "ffp", [128, 1024], F32)
        fbf = [sbt(g, st, "rfbf%d" % i, [128, 1024], BF16) for i in range(2)]
        fT32 = sbt(g, st, "fT32", [128, 8, 128], F32)
        sm = sbt(g, st, "rsm", [128, 64], F32)
        MK = sbt(g, st, "MK", [128, 64, 16], F32)
        GG = sbt(g, st, "GGs", [128, 128], F32)
        DF = sbt(g, st, "DF", [128, 128], F32)
        DI = sbt(g, st, "DI", [128, 128], I32)
        cm = sbt(g, st, "cm", [128, 192], F32)
        cnt = sbt(g, st, "cnt", [128, 64], F32)
        ptf = pst(g, st, "ptf", [128, 1024], F32)
        pl = pst(g, st, "pl", [128, 512], F32)
        pc = pst(g, st, "pc", [128, 512], F32)
        pk = [pst(g, st, "pk%d" % i, [128, 512], F32) for i in range(2)]
        lg, lg2, mk12 = sm[:, 0:8], sm[:, 16:24], sm[:, 8:16]
        ssq, rs, m1, m2, dl, g1 = (sm[:, 32:33], sm[:, 33:34], sm[:, 34:35], sm[:, 35:36], sm[:, 36:37],
                                   sm[:, 37:38])
        pr.dma(hb[0][:], g.H[256:384, :], writes=["rh0"])
        for jq in range(64):
            t = jq + 2
            i = jq % 2
            h = hb[i]
            hk = "rh%d" % i
            if jq + 1 < 64:
                pr.dma(hb[(jq + 1) % 2][:], g.H[(t + 1) * 128:(t + 2) * 128, :], writes=["rh%d" % ((jq + 1) % 2)])
            rms_rstd(g, junk[:], h[:], ssq, rs, 1024, [hk], "rssq", "rrs", "rjunk")
            pr.stt("dve", tmp[:], h[:], rs, G2[:], ALU.mult, ALU.mult, reads=[hk, "rrs", "G2"], writes=["rtmp"])
            pr.tt("dve", ffp[:], tmp[:], SH2[:], ALU.add, reads=["rtmp", "SH2"], writes=["ffp"])
            fo = fbf[i]
            fk = "rfbf%d" % i
            pr.copy("pool", fo[:], ffp[:], reads=["ffp"], writes=[fk])
            pr.dma(g.F[jq * 128:(jq + 1) * 128, :], fo[:], reads=[fk], writes=["F%d" % jq], eng="pool")
            for k in range(8):
                pr.tr(ptf[:, k * 128:(k + 1) * 128], ffp[:, k * 128:(k + 1) * 128], identf[:], inc=(k == 7),
                      reads=["ffp", "ident"], writes=["ptf"])
            pr.copy("act", fT32[:], ptf[:].rearrange("p (k n) -> p k n", k=8), reads=["ptf"], writes=["fT32"])
            for k in range(8):
                pr.mm(pl[:, 0:8], fT32[:, k, :], wr[:, k, :], start=(k == 0), stop=(k == 7), inc=(k == 7),
                      reads=["fT32", "wr"], writes=["pl"])
            mk1, mk2 = MK[:, jq, 0:8], MK[:, jq, 8:16]
            mkk = "MK%d" % jq
            pr.copy("dve", lg, pl[:, 0:8], reads=["pl"], writes=["lg"])
            pr.op("dve", lambda e, m1=m1, lg=lg: e.reduce_max(m1, lg, AX.X), reads=["lg"], writes=["m1"])
            pr.ts("dve", mk1, lg, m1, None, ALU.is_equal, reads=["lg", "m1"], writes=[mkk])
            pr.stt("dve", lg2, mk1, -1e30, lg, ALU.mult, ALU.add, reads=[mkk, "lg"], writes=["lg2"])
            pr.op("dve", lambda e, m2=m2, lg2=lg2: e.reduce_max(m2, lg2, AX.X), reads=["lg2"], writes=["m2"])
            pr.ts("dve", mk2, lg2, m2, None, ALU.is_equal, reads=["lg2", "m2"], writes=[mkk])
            pr.tt("dve", dl, m2, m1, ALU.subtract, reads=["m1", "m2"], writes=["dl"])
            pr.act(dl, dl, AF.Exp, reads=["dl"], writes=["dl"])
            pr.ts("dve", g1, dl, 1.0, None, ALU.add, reads=["dl"], writes=["g1"])
            pr.op("dve", lambda e, GG=GG, g1=g1, jq=jq: e.reciprocal(GG[:, 2 * jq:2 * jq + 1], g1),
                  reads=["g1"], writes=["GGs"])
            pr.tt("dve", GG[:, 2 * jq + 1:2 * jq + 2], dl, GG[:, 2 * jq:2 * jq + 1], ALU.mult,
                  reads=["dl", "GGs"], writes=["GGs"])
            pr.tt("dve", mk12, mk1, mk2, ALU.add, reads=[mkk], writes=["mk12"])
            pr.mm(pc[:, 0:8], ones[:], mk12, start=(jq == 0), stop=(jq == 63), reads=["ones", "mk12"], writes=["pc"])
        pr.dma(g.GG[:, :], GG[:], reads=["GGs"], eng="pool")
        c_cnt, c_nb, c_pe, c_base = cnt[:, 0:8], cnt[:, 8:16], cnt[:, 16:24], cnt[:, 24:32]
        pr.copy("dve", c_cnt, pc[:, 0:8], reads=["pc"], writes=["c_cnt"])
        pr.tt("dve", cm[:, 0:128].rearrange("p (e j) -> p e j", j=16), c_cnt.unsqueeze(2).broadcast_to([128, 8, 16]),
              thr[:].rearrange("p (e j) -> p e j", j=16), ALU.is_gt, reads=["c_cnt", "thr"], writes=["cm"])
        pr.op("dve", lambda e: e.reduce_sum(c_nb, cm[:, 0:128].rearrange("p (e j) -> p e j", j=16), AX.X),
              reads=["cm"], writes=["c_nb"])
        pr.ts("dve", c_nb, c_nb, 1024.0, None, ALU.mult, reads=["c_nb"], writes=["c_nb"])
        pr.copy("dve", c_pe[:, 0:1], c_nb[:, 0:1], reads=["c_nb"], writes=["c_pe"])
        for e_ in range(1, 8):
            pr.tt("dve", c_pe[:, e_:e_ + 1], c_pe[:, e_ - 1:e_], c_nb[:, e_:e_ + 1], ALU.add,
                  reads=["c_pe", "c_nb"], writes=["c_pe"])
        pr.tt("dve", c_base, c_pe, c_nb, ALU.subtract, reads=["c_pe", "c_nb"], writes=["c_base"])
        pr.tt("dve", cm[:].rearrange("p (s e) -> p s e", e=8), c_pe.unsqueeze(1).broadcast_to([128, 24, 8]),
              thr2[:].rearrange("p (s e) -> p s e", e=8), ALU.is_le, reads=["c_pe", "thr2", "cm"], writes=["cm"])
        be = cnt[:, 32:56]
        pr.op("dve", lambda e: e.reduce_sum(be, cm[:].rearrange("p (s e) -> p s e", e=8), AX.X),
              reads=["cm"], writes=["be"])
        pr.ts("dve", be, be, 7.0, None, ALU.min, reads=["be"], writes=["be"])
        pr.dma(g.BED[:, :], be, reads=["be"], eng="pool")
        for jq in range(64):
            for s_ in range(2):
                c = 2 * jq + s_
                mk = MK[:, jq, 8 * s_:8 * s_ + 8]
                p_ = pk[c % 2]
                pkk = "pk%d" % (c % 2)
                pr.mm(p_[:, 0:8], stri[:], mk, reads=["stri", "MK%d" % jq], writes=[pkk], inc=False)
                pr.mm(p_[:, 8:16], ones[:], mk, reads=["ones", "MK%d" % jq], writes=[pkk])
                pos, prod = sm[:, 40:48], sm[:, 48:56]
                pr.tt("dve", pos, p_[:, 0:8], c_base, ALU.add, reads=[pkk, "c_base"], writes=["pos"])
                pr.tt("dve", prod, pos, mk, ALU.mult, reads=["pos", "MK%d" % jq], writes=["prod"])
                pr.op("dve", lambda e, DF=DF, prod=prod, c=c: e.reduce_sum(DF[:, c:c + 1], prod, AX.X),
                      reads=["prod"], writes=["DF"])
                pr.tt("dve", c_base, p_[:, 8:16], c_base, ALU.add, reads=[pkk, "c_base"], writes=["c_base"])
        pr.copy("dve", DI[:], DF[:], reads=["DF"], writes=["DI"])
        pr.dma(g.DEST[:, :], DI[:], reads=["DI"], eng="pool")
        for jq in range(64):
            i = jq % 2
            fk = "rfbf%d" % i
            pr.dma(fbf[i][:], g.F[jq * 128:(jq + 1) * 128, :], reads=["F%d" % jq], writes=[fk])
            for s_ in range(2):
                c = 2 * jq + s_
                pr.idma(g.ROWS[:, :], DI[:, c:c + 1], fbf[i][:, :], None, reads=[fk, "DI"])


def phase_ES(g, l):
    pr, I, nc = g.pr, g.I, g.nc
    NS, FQ, NSB = 8, 896, 24
    with ExitStack() as st:
        cst = load_consts(g, st, ["ident_b", "kpq", "kp2"])
        identb, kpq, kp2 = cst["ident_b"], cst["kpq"], cst["kp2"]
        W1v = I["w_e1"].rearrange("a e d (q f) -> (a e d q) f", q=4)
        W3v = I["w_e3"].rearrange("a e d (q f) -> (a e d q) f", q=4)
        W2v = I["w_e2"].rearrange("a e f n -> (a e f) n")
        be = sbt(g, st, "esbe", [128, NSB], F32)
        pr.dma(be[:], g.BED[:, :], writes=["esbe"])
        i1f = sbt(g, st, "i1f", [128, NSB, 32], F32)
        i2f = sbt(g, st, "i2f", [128, NSB, 28], F32)
        i1 = sbt(g, st, "i1", [128, NSB * 32], I32)
        i2 = sbt(g, st, "i2", [128, NSB * 28], I32)
        pr.stt("dve", i1f[:], be[:].unsqueeze(2).broadcast_to([128, NSB, 32]), 4096.0,
               kpq[:].unsqueeze(1).broadcast_to([128, NSB, 32]), ALU.mult, ALU.add, reads=["esbe", "kpq"], writes=["i1f"])
        pr.stt("dve", i2f[:], be[:].unsqueeze(2).broadcast_to([128, NSB, 28]), 3584.0,
               kp2[:].unsqueeze(1).broadcast_to([128, NSB, 28]), ALU.mult, ALU.add, reads=["esbe", "kp2"], writes=["i2f"])
        pr.copy("dve", i1[:], i1f[:].rearrange("p s k -> p (s k)"), reads=["i1f"], writes=["i1"])
        pr.copy("dve", i2[:], i2f[:].rearrange("p s k -> p (s k)"), reads=["i2f"], writes=["i2"])
        w1 = [sbt(g, st, "ew1_%d" % i, [128, 8, FQ], BF16) for i in range(2)]
        w3 = [sbt(g, st, "ew3_%d" % i, [128, 8, FQ], BF16) for i in range(2)]
        w2 = [sbt(g, st, "ew2_%d" % i, [128, 7, 1024], BF16) for i in range(2)]
        rt = [sbt(g, st, "ert%d" % i, [128, 1024], BF16) for i in range(2)]
        fT = [sbt(g, st, "efT%d" % i, [128, NS, 8, 128], BF16) for i in range(2)]
        acc = sbt(g, st, "eacc", [128, NS, 1024], F32)
        stmp = [sbt(g, st, "estmp%d" % i, [128, 512], F32) for i in range(2)]
        actb = sbt(g, st, "eactb", [128, FQ], BF16)
        actT = sbt(g, st, "eactT", [128, 7, 128], BF16)
        ptr = pst(g, st, "eptr", [128, 1024], BF16)
        ptr2 = pst(g, st, "eptr2", [128, 1024], BF16)
        pu1 = [pst(g, st, "epu1%d" % i, [128, 512], F32) for i in range(2)]
        pu3 = [pst(g, st, "epu3%d" % i, [128, 512], F32) for i in range(2)]
        po = [pst(g, st, "epo%d" % i, [128, 512], F32) for i in range(2)]
        un_idx = [0]

        def load_unit(sb, qd):
            i = un_idx[0] % 2
            un_idx[0] += 1
            for k in range(8):
                c = sb * 32 + qd * 8 + k
                pr.idma(w1[i][:, k, :], None, W1v[:, :], i1[:, c:c + 1], reads=["i1"], writes=["ew1_%d" % i])
                pr.idma(w3[i][:, k, :], None, W3v[:, :], i1[:, c:c + 1], reads=["i1"], writes=["ew3_%d" % i])
            for k in range(7):
                c = sb * 28 + qd * 7 + k
                pr.idma(w2[i][:, k, :], None, W2v[:, :], i2[:, c:c + 1], reads=["i2"], writes=["ew2_%d" % i])
            return i

        def load_rows(sb):
            f = fT[sb % 2]
            fk = "efT%d" % (sb % 2)
            for ti in range(NS):
                r = rt[ti % 2]
                rk = "ert%d" % (ti % 2)
                r0 = sb * 1024 + ti * 128
                pr.dma(r[:], g.ROWS[r0:r0 + 128, :], writes=[rk])
                for k in range(8):
                    pr.tr(ptr2[:, k * 128:(k + 1) * 128], r[:, k * 128:(k + 1) * 128], identb[:], inc=(k == 7),
                          reads=[rk, "ident_b"], writes=["eptr2"])
                pr.copy("dve", f[:, ti, :, :], ptr2[:].rearrange("p (k n) -> p k n", k=8), reads=["eptr2"], writes=[fk])

        actb2 = [actb, sbt(g, st, "eactb1", [128, FQ], BF16)]
        actT2 = [actT, sbt(g, st, "eactT1", [128, 7, 128], BF16)]
        units = [(sb, qd) for sb in range(NSB) for qd in range(4)]
        items = [(sb, qd, ti) for (sb, qd) in units for ti in range(NS)]
        wof = {}
        load_rows(0)
        wof[units[0]] = load_unit(*units[0])
        wof[units[1]] = load_unit(*units[1])

        def stA(j):
            sb, qd, ti = items[j]
            if ti == 0 and qd == 1 and sb + 1 < NSB:
                load_rows(sb + 1)
            wi = wof[(sb, qd)]
            f = fT[sb % 2]
            fk = "efT%d" % (sb % 2)
            ab, abk = actb2[j % 2], "eactb%d" % (j % 2)
            for cg in range(2):
                c0 = cg * 512
                n = min(512, FQ - c0)
                for k in range(8):
                    pr.mm(pu1[cg][:, :n], f[:, ti, k, :], w1[wi][:, k, c0:c0 + n], start=(k == 0), stop=(k == 7),
                          inc=(k == 7), reads=[fk, "ew1_%d" % wi], writes=["epu1%d" % cg])
                for k in range(8):
                    pr.mm(pu3[cg][:, :n], f[:, ti, k, :], w3[wi][:, k, c0:c0 + n], start=(k == 0), stop=(k == 7),
                          inc=(k == 7), reads=[fk, "ew3_%d" % wi], writes=["epu3%d" % cg])
                pr.act(stmp[cg][:, :n], pu1[cg][:, :n], AF.Silu, reads=["epu1%d" % cg], writes=["estmp%d" % cg])
                pr.tt("dve", ab[:, c0:c0 + n], stmp[cg][:, :n], pu3[cg][:, :n], ALU.mult,
                      reads=["estmp%d" % cg, "epu3%d" % cg], writes=[abk])

        def stB1(j):
            ab, abk = actb2[j % 2], "eactb%d" % (j % 2)
            at, atk = actT2[j % 2], "eactT%d" % (j % 2)
            for k in range(7):
                pr.tr(ptr[:, k * 128:(k + 1) * 128], ab[:, k * 128:(k + 1) * 128], identb[:], inc=(k == 6),
                      reads=[abk, "ident_b"], writes=["eptr"])
            pr.copy("act", at[:], ptr[:, 0:896].rearrange("p (k n) -> p k n", k=7), reads=["eptr"], writes=[atk])

        def stB2(j):
            sb, qd, ti = items[j]
            wi = wof[(sb, qd)]
            at, atk = actT2[j % 2], "eactT%d" % (j % 2)
            for cg in range(2):
                for k in range(7):
                    pr.mm(po[cg][:], at[:, k, :], w2[wi][:, k, cg * 512:(cg + 1) * 512], start=(k == 0),
                          stop=(k == 6), inc=(k == 6), reads=[atk, "ew2_%d" % wi], writes=["epo%d" % cg])
                sl = slice(cg * 512, (cg + 1) * 512)
                ak = "eacc%d" % ti
                if qd == 0:
                    pr.copy("dve", acc[:, ti, sl], po[cg][:], reads=["epo%d" % cg], writes=[ak])
                else:
                    pr.tt("dve", acc[:, ti, sl], po[cg][:], acc[:, ti, sl], ALU.add,
                          reads=["epo%d" % cg, ak], writes=[ak])
            if qd == 3:
                r0 = sb * 1024 + ti * 128
                pr.dma(g.YROWS[r0:r0 + 128, :], acc[:, ti, :], reads=["eacc%d" % ti])

        stA(0)
        for j in range(len(items)):
            stB1(j)
            if j + 1 < len(items):
                stA(j + 1)
            stB2(j)
            sb_, qd_, ti_ = items[j]
            if ti_ == NS - 1:
                ui = sb_ * 4 + qd_
                if ui + 2 < len(units):
                    wof[units[ui + 2]] = load_unit(*units[ui + 2])


def phase_E2(g, l):
    pr, I, nc = g.pr, g.I, g.nc
    with ExitStack() as st:
        gate = sbt(g, st, "egate", [128, 1024], F32)
        pr.dma(gate[:], mod_row(g, l, 1, 5), writes=["egate"])
        GG = sbt(g, st, "e2gg", [128, 128], F32)
        DI = sbt(g, st, "e2di", [128, 128], I32)
        pr.dma(GG[:], g.GG[:, :], writes=["e2gg"])
        pr.dma(DI[:], g.DEST[:, :], writes=["e2di"])
        hb = [sbt(g, st, "e2h%d" % i, [128, 1024], F32) for i in range(3)]
        y1 = [sbt(g, st, "e2y1%d" % i, [128, 1024], F32) for i in range(3)]
        y2 = [sbt(g, st, "e2y2%d" % i, [128, 1024], F32) for i in range(3)]
        def fetch(jq):
            i = jq % 3
            t = jq + 2
            pr.dma(hb[i][:], g.H[t * 128:(t + 1) * 128, :], writes=["e2h%d" % i])
            pr.idma(y1[i][:, :], None, g.YROWS[:, :], DI[:, 2 * jq:2 * jq + 1], reads=["e2di"], writes=["e2y1%d" % i])
            pr.idma(y2[i][:, :], None, g.YROWS[:, :], DI[:, 2 * jq + 1:2 * jq + 2], reads=["e2di"], writes=["e2y2%d" % i])

        fetch(0)
        for jq in range(64):
            i = jq % 3
            t = jq + 2
            hk, k1, k2 = "e2h%d" % i, "e2y1%d" % i, "e2y2%d" % i
            if jq + 1 < 64:
                fetch(jq + 1)
            pr.ts("dve", y1[i][:], y1[i][:], GG[:, 2 * jq:2 * jq + 1], None, ALU.mult, reads=[k1, "e2gg"], writes=[k1])
            pr.stt("dve", y1[i][:], y2[i][:], GG[:, 2 * jq + 1:2 * jq + 2], y1[i][:], ALU.mult, ALU.add,
                   reads=[k1, k2, "e2gg"], writes=[k1])
            pr.tt("dve", y1[i][:], y1[i][:], gate[:], ALU.mult, reads=[k1, "egate"], writes=[k1])
            pr.tt("dve", hb[i][:], hb[i][:], y1[i][:], ALU.add, reads=[hk, k1], writes=[hk])
            pr.dma(g.out[jq * 128:(jq + 1) * 128, :], hb[i][:], reads=[hk])


PHASES = {"NN": lambda g, l: phase_NA(g, l, True), "R2": phase_R2, "ES": phase_ES, "E2": phase_E2, "R": phase_R, "E": phase_E, "C1": phase_C1, "C2": phase_C2, "NP": phase_NP, "NA": phase_NA, "M": phase_M, "A": phase_A, "SF": lambda g, l: phase_SSD(g, l, 0),
          "SB": lambda g, l: phase_SSD(g, l, 1)}


_CACHE = {}


def prep_inputs(inputs):
    consts = host_consts(np.asarray(inputs["rpb"], np.float32))
    maps = []
    c_ctx = np.asarray(inputs["c_ctx"], np.float32)
    for b in range(8):
        m = {}
        m["x"] = np.ascontiguousarray(inputs["x"][b], dtype=np.float32)
        m["ctx"] = np.ascontiguousarray(inputs["ctx"][b], dtype=np.float32)
        ct = np.empty((128, 16), np.float32)
        ct[:, 0:8] = np.asarray(inputs["c"][b], np.float32).reshape(8, 128).T
        ct[:, 8:16] = c_ctx.reshape(8, 128).T
        m["ct"] = ct
        for k in WEIGHT_SHAPES:
            m[k] = np.ascontiguousarray(inputs[k], dtype=np.float32)
        for k in CONST_SHAPES:
            m[k] = consts[k]
        maps.append(m)
    return maps


FULL_STAGES = [(0, "M"), (0, "A"), (0, "SF"), (0, "SB"), (0, "NN"), (0, "C1"), (0, "C2"),
               (1, "M"), (1, "A"), (1, "SF"), (1, "SB"), (1, "NN"), (1, "C1"), (1, "R2"), (1, "ES"), (1, "E2")]


def kernel(**inputs):
    maps = prep_inputs(inputs)
    nc = build(FULL_STAGES)
    res = run_bass_kernel_spmd(nc, maps, core_ids=list(range(8)))
    return np.stack([np.asarray(r["out"], np.float32) for r in res.results], 0)
```
